# Optimizing a Trainium2 kernel written in Bass

```python
import jax, jax.numpy as jnp
from jax import lax
import numpy as np


D_MODEL = 2048
BATCH = 4
SEQ = 4096
DEPTH = 2

MEM_LEN = 256
BRANCH_W = D_MODEL // 2
N_BRANCH = 4
MOBA_HEADS = 8
MOBA_HEAD_DIM = BRANCH_W // MOBA_HEADS
MOBA_BLOCK = 256
MOBA_TOPK = 3
MOBA_Q_CHUNK = 16
CONV_WIDTH = 3
DSA_HEADS = 8
DSA_HEAD_DIM = BRANCH_W // DSA_HEADS
IDX_HEADS = 4
IDX_DIM = 64
DSA_TOPK_MAX = 256
DSA_Q_CHUNK = 64
MEM_HEADS = 4
MEM_HEAD_DIM = BRANCH_W // MEM_HEADS
EPS = 1e-6

SPLIT_SIZES = (BRANCH_W,) * 14 + (IDX_HEADS * IDX_DIM, IDX_DIM, IDX_HEADS) + (D_MODEL,) * N_BRANCH
IN_COLS = 14 * BRANCH_W + IDX_HEADS * IDX_DIM + IDX_DIM + IDX_HEADS + N_BRANCH * D_MODEL

kernel_name = 'hybrid_moba_conv_dsa_memory_gated'


def _split_points():
    pts, acc = [], 0
    for s in SPLIT_SIZES[:-1]:
        acc += s
        pts.append(acc)
    return pts


def rms_norm(x, g):
    xf = x.astype(jnp.float32)
    y = xf * lax.rsqrt(jnp.mean(xf * xf, axis=-1, keepdims=True) + EPS)
    return (y * g.astype(jnp.float32)).astype(x.dtype)


def moba_attention(q, k, v):
    b, L, h, d = q.shape
    nb = -(-L // MOBA_BLOCK)
    Lp = nb * MOBA_BLOCK
    pad = ((0, 0), (0, Lp - L), (0, 0), (0, 0))
    q, k, v = jnp.pad(q, pad), jnp.pad(k, pad), jnp.pad(v, pad)
    qh = q.transpose(0, 2, 1, 3)
    kb = k.transpose(0, 2, 1, 3).reshape(b, h, nb, MOBA_BLOCK, d)
    vb = v.transpose(0, 2, 1, 3).reshape(b, h, nb, MOBA_BLOCK, d)
    kmean = jnp.mean(kb.astype(jnp.float32), axis=3)
    qblk = jnp.arange(Lp) // MOBA_BLOCK
    bscore = jnp.einsum('bhtd,bhnd->bhtn', qh.astype(jnp.float32), kmean)
    past = jnp.arange(nb)[None, :] < qblk[:, None]
    bscore = jnp.where(past, bscore, -jnp.inf)
    _, top_idx = lax.top_k(bscore, min(MOBA_TOPK, nb))
    own = jnp.broadcast_to(qblk[None, None, :, None], (b, h, Lp, 1)).astype(top_idx.dtype)
    sel = jnp.concatenate([top_idx, own], axis=-1)
    n_sel = sel.shape[-1]
    C = MOBA_Q_CHUNK
    n_chunks = Lp // C
    qs = qh.reshape(b, h, n_chunks, C, d).transpose(2, 0, 1, 3, 4)
    sels = sel.reshape(b, h, n_chunks, C, n_sel).transpose(2, 0, 1, 3, 4)
    gather = jax.vmap(jax.vmap(lambda blocks, ix: blocks[ix]))
    scale = d ** -0.5
    is_own = (jnp.arange(n_sel) == n_sel - 1)[:, None]

    def chunk(args):
        qc, selc, cid = args
        t = cid * C + jnp.arange(C)
        kg = gather(kb, selc)
        vg = gather(vb, selc)
        s = jnp.einsum('bhcd,bhcnkd->bhcnk', qc, kg).astype(jnp.float32) * scale
        kpos = selc[..., None] * MOBA_BLOCK + jnp.arange(MOBA_BLOCK)
        valid = jnp.where(is_own, kpos <= t[:, None, None],
                          selc[..., None] < (t // MOBA_BLOCK)[:, None, None])
        s = jnp.where(valid, s, -jnp.inf)
        p = jax.nn.softmax(s.reshape(b, h, C, n_sel * MOBA_BLOCK), axis=-1)
        p = p.reshape(b, h, C, n_sel, MOBA_BLOCK).astype(vg.dtype)
        return jnp.einsum('bhcnk,bhcnkd->bhcd', p, vg)

    o = lax.map(chunk, (qs, sels, jnp.arange(n_chunks)))
    o = o.transpose(1, 2, 0, 3, 4).reshape(b, h, Lp, d).transpose(0, 2, 1, 3)
    return o[:, :L]


def short_conv(u, w, bias):
    y = lax.conv_general_dilated(u, w.astype(u.dtype)[:, None, :], window_strides=(1,),
                                 padding=[(CONV_WIDTH - 1, 0)],
                                 dimension_numbers=('NWC', 'WIO', 'NWC'),
                                 feature_group_count=u.shape[-1])
    return y + bias.astype(u.dtype)


def dsa_attention(q, k, v, qi, ki, wi):
    b, L, h, d = q.shape
    topk = min(DSA_TOPK_MAX, L // 4)
    C = DSA_Q_CHUNK
    n_chunks = L // C
    qs = q.reshape(b, n_chunks, C, h, d).transpose(1, 0, 2, 3, 4)
    qis = qi.reshape(b, n_chunks, C, IDX_HEADS, IDX_DIM).transpose(1, 0, 2, 3, 4)
    wis = wi.reshape(b, n_chunks, C, IDX_HEADS).transpose(1, 0, 2, 3)
    kif = ki.astype(jnp.float32)
    gather = jax.vmap(lambda kk, ix: kk[ix])
    scale = d ** -0.5
    idx_scale = (IDX_DIM ** -0.5) * (IDX_HEADS ** -0.5)
    key_pos = jnp.arange(L)

    def chunk(args):
        qc, qic, wic, cid = args
        t = cid * C + jnp.arange(C)
        logits = jnp.einsum('bcji,bsi->bcjs', qic.astype(jnp.float32), kif)
        score = jnp.einsum('bcj,bcjs->bcs', wic.astype(jnp.float32) * idx_scale,
                           jax.nn.relu(logits))
        score = jnp.where(key_pos[None, None, :] <= t[None, :, None], score, -jnp.inf)
        _, idx = lax.top_k(score, topk)
        kg = gather(k, idx)
        vg = gather(v, idx)
        s = jnp.einsum('bchd,bckhd->bhck', qc, kg).astype(jnp.float32) * scale
        valid = (idx <= t[None, :, None])[:, None]
        p = jax.nn.softmax(jnp.where(valid, s, -jnp.inf), axis=-1).astype(vg.dtype)
        return jnp.einsum('bhck,bckhd->bchd', p, vg)

    o = lax.map(chunk, (qs, qis, wis, jnp.arange(n_chunks)))
    return o.transpose(1, 0, 2, 3, 4).reshape(b, L, h, d)


def memory_attention(q, mk, mv):
    s = jnp.einsum('bthd,bmhd->bhtm', q, mk).astype(jnp.float32) * (q.shape[-1] ** -0.5)
    p = jax.nn.softmax(s, axis=-1).astype(mv.dtype)
    return jnp.einsum('bhtm,bmhd->bthd', p, mv)


def hybrid_layer(x, mem, ln_g, w_in, conv_w, conv_b, mem_ln_g, w_mem_kv, w_branch, w_out):
    b, L, _ = x.shape
    xn = rms_norm(x, ln_g)
    cols = jnp.split(w_in, _split_points(), axis=1)
    (a_q, a_k, a_v, a_g, c_b, c_c, c_h, c_g, s_q, s_k, s_v, s_g, m_q, m_g,
     i_q, i_k, i_w, r_a, r_c, r_s, r_m) = [xn @ w for w in cols]

    def heads(t, n, d):
        return t.reshape(b, L, n, d)

    y_a = moba_attention(heads(a_q, MOBA_HEADS, MOBA_HEAD_DIM), heads(a_k, MOBA_HEADS, MOBA_HEAD_DIM),
                         heads(a_v, MOBA_HEADS, MOBA_HEAD_DIM)).reshape(b, L, BRANCH_W)
    y_a = y_a * jax.nn.silu(a_g)
    y_c = c_b * short_conv(c_c * c_h, conv_w, conv_b) * jax.nn.silu(c_g)
    y_s = dsa_attention(heads(s_q, DSA_HEADS, DSA_HEAD_DIM), heads(s_k, DSA_HEADS, DSA_HEAD_DIM),
                        heads(s_v, DSA_HEADS, DSA_HEAD_DIM), heads(i_q, IDX_HEADS, IDX_DIM),
                        i_k, i_w).reshape(b, L, BRANCH_W)
    y_s = y_s * jax.nn.silu(s_g)
    mem_n = rms_norm(mem, mem_ln_g)
    mk, mv = jnp.split(mem_n @ w_mem_kv, 2, axis=-1)
    M = mem.shape[1]
    y_m = memory_attention(heads(m_q, MEM_HEADS, MEM_HEAD_DIM),
                           mk.reshape(b, M, MEM_HEADS, MEM_HEAD_DIM),
                           mv.reshape(b, M, MEM_HEADS, MEM_HEAD_DIM)).reshape(b, L, BRANCH_W)
    y_m = y_m * jax.nn.silu(m_g)
    merged = (jax.nn.sigmoid(r_a) * (y_a @ w_branch[0]) + jax.nn.sigmoid(r_c) * (y_c @ w_branch[1])
              + jax.nn.sigmoid(r_s) * (y_s @ w_branch[2]) + jax.nn.sigmoid(r_m) * (y_m @ w_branch[3]))
    return x + merged @ w_out


def setup_inputs(seed: int = 0) -> dict:
    key = jax.random.key(seed)
    ks = jax.random.split(key, 11)
    f32 = jnp.float32
    x = jax.random.normal(ks[0], (BATCH, SEQ, D_MODEL), f32)
    mem = jax.random.normal(ks[1], (BATCH, MEM_LEN, D_MODEL), f32)
    ln_g = 1.0 + 0.02 * jax.random.normal(ks[2], (DEPTH, D_MODEL), f32)
    w_in = jax.random.normal(ks[3], (DEPTH, D_MODEL, IN_COLS), f32) * D_MODEL ** -0.5
    conv_w = jax.random.normal(ks[4], (DEPTH, CONV_WIDTH, BRANCH_W), f32) * CONV_WIDTH ** -0.5
    conv_b = 0.02 * jax.random.normal(ks[5], (DEPTH, BRANCH_W), f32)
    mem_ln_g = 1.0 + 0.02 * jax.random.normal(ks[6], (DEPTH, D_MODEL), f32)
    w_mem_kv = jax.random.normal(ks[7], (DEPTH, D_MODEL, 2 * BRANCH_W), f32) * D_MODEL ** -0.5
    w_branch = jax.random.normal(ks[8], (DEPTH, N_BRANCH, BRANCH_W, D_MODEL), f32) * BRANCH_W ** -0.5
    w_out = jax.random.normal(ks[9], (DEPTH, D_MODEL, D_MODEL), f32) * D_MODEL ** -0.5
    final_g = 1.0 + 0.02 * jax.random.normal(ks[10], (D_MODEL,), f32)
    return {'x': x, 'mem': mem, 'ln_g': ln_g, 'w_in': w_in, 'conv_w': conv_w, 'conv_b': conv_b,
            'mem_ln_g': mem_ln_g, 'w_mem_kv': w_mem_kv, 'w_branch': w_branch, 'w_out': w_out,
            'final_g': final_g}


def reference(x, mem, ln_g, w_in, conv_w, conv_b, mem_ln_g, w_mem_kv, w_branch, w_out, final_g):
    for layer in range(DEPTH):
        x = hybrid_layer(x, mem, ln_g[layer], w_in[layer], conv_w[layer], conv_b[layer],
                         mem_ln_g[layer], w_mem_kv[layer], w_branch[layer], w_out[layer])
    return rms_norm(x, final_g)
```

```python
import os
import numpy as np
import concourse.bass as bass
import concourse.mybir as mybir
from concourse.bass_utils import run_bass_kernel_spmd
from contextlib import ExitStack

F32 = mybir.dt.float32
BF16 = mybir.dt.bfloat16
AF = mybir.ActivationFunctionType
ALU = mybir.AluOpType
AX = mybir.AxisListType

D = 2048
KC = 16
BW = 1024
INC = 22852
NEG = -30000.0
BIG = 1.0e30
EPS = 1e-6
C_AQ, C_AK, C_AV, C_AG = 0, 1024, 2048, 3072
C_CB, C_CC, C_CH, C_CG = 4096, 5120, 6144, 7168
C_SQ, C_SK, C_SV, C_SG = 8192, 9216, 10240, 11264
C_MQ, C_MG = 12288, 13312
C_IQ, C_IK, C_IW, C_R = 14336, 14592, 14656, 14660
NIT = 16
TOPK = 256


class Tk:
    __slots__ = ("name", "w", "r", "dsem", "dcnt")

    def __init__(self, name):
        self.name = name
        self.w = None
        self.r = []
        self.dsem = None
        self.dcnt = 0


class Sched:
    def __init__(self, nc, es):
        self.nc = nc
        self.es = es
        self.eng = {"pe": nc.tensor, "act": nc.scalar, "dve": nc.vector, "pool": nc.gpsimd, "sp": nc.sync}
        self.sem = {k: es.enter_context(nc.semaphore("sem_" + k)) for k in self.eng}
        self.cnt = {k: 0 for k in self.eng}
        self.waited = {k: {} for k in self.eng}
        self.ninst = 0
        self.owners = []

    def drain(self):
        for k in self.eng:
            if self.cnt[k] > 0 and k != "sp":
                self.eng["sp"].wait_ge(self.sem[k], self.cnt[k])
        for o in self.owners:
            self.eng["sp"].wait_ge(o.dsem, o.dcnt)

    def _deps(self, e, reads, writes):
        deps = {}

        def add(tk, war):
            if tk is None:
                return
            key, sh, val, te = tk
            if te == e and e == "pe":
                return
            if deps.get(key, (None, 0))[1] < val:
                deps[key] = (sh, val)

        for t in reads:
            add(t.w, False)
        for t in writes:
            add(t.w, False)
            for rt in t.r:
                add(rt, True)
        return deps

    def _wait(self, e, deps):
        for key, (sh, val) in deps.items():
            if self.waited[e].get(key, 0) >= val:
                continue
            self.eng[e].wait_ge(sh, val)
            self.waited[e][key] = val

    def _record(self, tk, reads, writes):
        for t in writes:
            t.w = tk
            t.r = []
        for t in reads:
            if t in writes:
                continue
            t.r = [x for x in t.r if x[0] != tk[0]] + [tk]

    def op(self, e, fn, reads=(), writes=()):
        reads = list(reads)
        writes = list(writes)
        self._wait(e, self._deps(e, reads, writes))
        ins = fn(self.eng[e])
        self.cnt[e] += 1
        ins.then_inc(self.sem[e], 1)
        tk = (e, self.sem[e], self.cnt[e], e)
        self._record(tk, reads, writes)
        self.ninst += 1
        return tk

    def dma(self, q, out_ap, in_ap, owner, reads=(), writes=(), **kw):
        reads = list(reads)
        writes = list(writes)
        if owner.dsem is None:
            owner.dsem = self.es.enter_context(self.nc.semaphore("d_" + owner.name))
            self.owners.append(owner)
        self._wait(q, self._deps(q, reads, writes))
        ins = self.eng[q].dma_start(out=out_ap, in_=in_ap, **kw)
        owner.dcnt += 16
        ins.then_inc(owner.dsem, 16)
        tk = ("d_" + owner.name, owner.dsem, owner.dcnt, None)
        self._record(tk, reads, writes)
        self.ninst += 1
        return tk

    def final_wait(self, e, tiles):
        deps = {}
        for t in tiles:
            for tk in [t.w] + t.r:
                if tk is None:
                    continue
                key, sh, val, te = tk
                if deps.get(key, (None, 0))[1] < val:
                    deps[key] = (sh, val)
        self._wait(e, deps)


def build(T, L):
    NG = T // 512
    NT = T // 128
    nc = bass.Bass("TRN2", target_bir_lowering=False)
    es = ExitStack()

    def din(name, shape):
        return nc.dram_tensor(name, shape, F32, kind="ExternalInput").ap()

    x_d = din("x", [T, D])
    mem_d = din("mem", [256, D])
    lng_d = din("ln_g", [L, D])
    win_d = din("w_in", [L, D, INC])
    cw_d = din("conv_w", [L, 3, BW])
    cb_d = din("conv_b", [L, BW])
    mg_d = din("mem_ln_g", [L, D])
    wkv_d = din("w_mem_kv", [L, D, 2048])
    wbr_d = din("w_branch", [L, 4, BW, D])
    wout_d = din("w_out", [L, D, D])
    fg_d = din("final_g", [1, D])
    ident_d = din("c_ident", [128, 128])
    tri_d = din("c_tri", [128, 128])
    triq_d = din("c_triq", [128, 128])
    eone_d = din("c_eone", [16, 2048])
    pw2_d = din("c_pw2", [128, NIT + 2])
    sel_d = din("c_sel", [128, 16])
    y_d = nc.dram_tensor("y", [T, D], F32, kind="ExternalOutput").ap()
    xmid_d = nc.dram_tensor("xmid", [T, D], F32).ap()
    kT_d = {t: nc.dram_tensor("kT" + t, [8, 128, T], BF16).ap() for t in "as"}
    v_d = {t: nc.dram_tensor("vv" + t, [T, 1024], BF16).ap() for t in "as"}

    with es:
        S = Sched(nc, es)

        def sb(name, shape, dt=F32):
            return es.enter_context(nc.sbuf_tensor(name, shape, dt)), Tk(name)

        def psb(name, shape, dt=F32):
            return es.enter_context(nc.psum_tensor(name, shape, dt)), Tk(name)

        NW = 3
        wt = [sb(f"wt{i}", [128, 4096], BF16) for i in range(NW)]
        xnT, t_xnT = sb("xnT", [128, KC, 512], BF16)
        xall, _ = sb("xall", [128, 4096])
        t_xq = [Tk(f"xq{i}") for i in range(4)]
        scb, t_scb = sb("scb", [128, max(T, 2048)])
        kst, t_kst = sb("kst", [128, 4096], BF16)
        vst, t_vst = sb("vst", [128, 4, 1024], BF16)
        kiT, t_kiT = sb("kiT", [128, T], BF16)
        qiT, t_qiT = sb("qiT", [128, 2, 512], BF16)
        qh = [sb(f"qh{i}", [128, 512], BF16) for i in range(2)]
        sgh = [sb(f"sgh{i}", [128, 512], BF16) for i in range(2)]
        NR = 4
        kring = [sb(f"kr{i}", [128, 1024], BF16) for i in range(NR)]
        vring = [sb(f"vr{i}", [128, 8, 128], BF16) for i in range(NR)]
        pT = [sb(f"pT{i}", [128, 512], BF16) for i in range(3)]
        yT = [sb(f"yT{i}", [128, 8, 512], BF16) for i in range(4)]
        MBW = max(2 * T, 8192)
        MBH = MBW // 2
        mball, _ = sb("mball", [128, MBW], BF16)
        t_mb = [Tk("mb0"), Tk("mb1")]
        Fs = [sb(f"F{i}", [128, 516]) for i in range(6)]
        sgt = [sb(f"sgt{i}", [128, 512], BF16) for i in range(2)]
        mkT, t_mkT = sb("mkT", [128, 8, 256], BF16)
        mv, t_mv = sb("mv", [128, 2, 1024], BF16)
        ident, t_ident = sb("ident", [128, 128], BF16)
        tri, t_tri = sb("tri", [128, 128], BF16)
        triq, t_triq = sb("triq", [128, 128])
        ones, t_ones = sb("ones", [128, 128], BF16)
        eone, t_eone = sb("eone", [16, 2048], BF16)
        pw2, t_pw2 = sb("pw2", [128, NIT + 2])
        sel, t_sel = sb("sel", [128, 16], BF16)
        iwT, t_iwT = sb("iwT", [128, 512], BF16)
        km32, t_km32 = sb("km32", [128, 8, 16])
        kmT, t_kmT = sb("kmT", [128, 8, 16], BF16)
        bsm, t_bsm = sb("bsm", [128, 4, 16])
        m8, t_m8 = sb("m8", [128, 4, 8])
        mbf, t_mbf = sb("mbf", [128, 4, 16], BF16)
        mbT, t_mbT = sb("mbT", [16, 512], BF16)
        wI, t_wI = sb("wI", [128, 4, 4])
        st, t_st = sb("stat", [128, 8])
        epsb, t_eps = sb("epsb", [128, 1])
        bis, t_bis = sb("bis", [128, 8])
        halves, t_halves = sb("halves", [128, NIT + 2])
        nm, t_nm = sb("nm", [128, NIT + 1])
        ssum, t_ssum = sb("ssum", [128, NIT])
        dtmp, t_dtmp = sb("dtmp", [128, NIT])
        cw, t_cw = sb("cw", [128, 8, 3])
        cbias, t_cbias = sb("cbias", [128, 8])
        carry, t_carry = sb("carry", [128, 8, 2])
        sgc, t_sgc = sb("sgc", [128, 512], BF16)

        pj = [psb(f"pj{i}", [128, 512]) for i in range(3)]
        ptr, t_ptr = psb("ptr", [128, 1024], BF16)
        stp = [psb(f"stp{i}", [128, 512]) for i in range(2)]
        ot, t_ot = psb("ot", [128, 512])
        dn, t_dn = psb("dn", [128, 512])
        pjc = [0]

        def next_pj():
            pjc[0] += 1
            return pj[pjc[0] % 3]

        S.dma("pool", ident[:], ident_d, t_ident, writes=[t_ident])
        S.dma("pool", tri[:], tri_d, t_tri, writes=[t_tri])
        S.dma("pool", eone[:], eone_d, t_eone, writes=[t_eone])
        S.dma("sp", triq[:], triq_d, t_triq, writes=[t_triq])
        S.dma("sp", pw2[:], pw2_d, t_pw2, writes=[t_pw2])
        S.dma("pool", sel[:], sel_d, t_sel, writes=[t_sel])
        S.op("dve", lambda e: e.memset(ones[:], 1.0), writes=[t_ones])
        S.op("dve", lambda e: e.memset(epsb[:], EPS), writes=[t_eps])

        wcnt = [0]

        def wload(parts):
            w_sb, w_tk = wt[wcnt[0] % NW]
            wcnt[0] += 1
            for off, nk, ncol, src in parts:
                dst = w_sb[:, off:off + nk * ncol].rearrange("p (k n) -> p k n", k=nk)
                if isinstance(src, list):
                    for lo, sap in src:
                        S.dma("pool", dst[:, :, lo:lo + sap.shape[2]], sap, w_tk, writes=[w_tk])
                else:
                    S.dma("pool", dst, src, w_tk, writes=[w_tk])
            return w_sb, w_tk

        def wview(w_sb, off, nk, ncol):
            return w_sb[:, off:off + nk * ncol].rearrange("p (k n) -> p k n", k=nk)

        def win_cols(l, c0, n, reps=1, stride=0):
            base = win_d[l].rearrange("(k p) n -> p k n", p=128)
            if reps == 1:
                return base[:, :, c0:c0 + n]
            return [(r * n, base[:, :, c0 + r * stride:c0 + r * stride + n]) for r in range(reps)]

        def rmsnorm_rstd(x_ap, x_tks, junk_ap, junk_tks, col):
            S.op("act", lambda e: e.activation(out=junk_ap, in_=x_ap, func=AF.Square, accum_out=st[:, col:col + 1]),
                 reads=x_tks, writes=junk_tks + [t_st])
            S.op("act", lambda e: e.activation(out=st[:, col:col + 1], in_=st[:, col:col + 1], func=AF.Sqrt,
                                               scale=1.0 / D, bias=epsb[:, 0:1]), reads=[t_st, t_eps], writes=[t_st])
            S.op("dve", lambda e: e.reciprocal(st[:, col:col + 1], st[:, col:col + 1]), reads=[t_st], writes=[t_st])

        evc = [0]

        def evac_copy(out_ap, out_tks, in_ap, in_tks):
            evc[0] += 1
            if evc[0] % 2 == 0:
                S.op("act", lambda e: e.activation(out=out_ap, in_=in_ap, func=AF.Copy), reads=in_tks, writes=out_tks)
            else:
                S.op("dve", lambda e: e.tensor_copy(out_ap, in_ap), reads=in_tks, writes=out_tks)

        def norm_T(x_src_ap, g_tk_ready, ncols_tok, tok_off):
            pass

        def fm_chunk(w_view, w_tk, kn, rhs_fn, rhs_tks, ncol_tok):
            ps, tps = next_pj()
            for k in range(kn):
                S.op("pe", lambda e, k=k: e.matmul(ps[:, 0:ncol_tok], w_view[:, k, :], rhs_fn(k),
                                                   start=(k == 0), stop=(k == kn - 1)),
                     reads=[w_tk] + rhs_tks, writes=[tps])
            return ps, tps

        for l in range(L):
            xsrc = x_d if l == 0 else xmid_d
            xreg = [Tk(f"xreg{l}_{g}") for g in range(NG)]
            if l > 0:
                for g in range(NG):
                    xreg[g].w = xreg_prev[g].w
            kreg = {t: [Tk(f"kreg{t}{l}_{g}") for g in range(NG)] for t in "as"}
            vreg = {t: [Tk(f"vreg{t}{l}_{g}") for g in range(NG)] for t in "as"}
            if l > 0:
                for t in "as":
                    for g in range(NG):
                        kreg[t][g].r = list(kreg_prev[t][g].r)
                        kreg[t][g].w = kreg_prev[t][g].w
                        vreg[t][g].r = list(vreg_prev[t][g].r)
                        vreg[t][g].w = vreg_prev[t][g].w

            for k3 in range(3):
                S.dma("sp", cw[:, :, k3], cw_d[l, k3].rearrange("(c p) -> p c", p=128), t_cw, writes=[t_cw],
                      allow_slow_non_contiguous=True)
            S.dma("sp", cbias[:], cb_d[l].rearrange("(c p) -> p c", p=128), t_cbias, writes=[t_cbias],
                  allow_slow_non_contiguous=True)
            S.op("dve", lambda e: e.memset(carry[:], 0.0), writes=[t_carry])
            S.op("dve", lambda e: e.memset(km32[:], 0.0), writes=[t_km32])
            S.op("dve", lambda e: e.memset(kmT[:], 0.0), writes=[t_kmT])

            if DBG == "consts":
                S.drain()
                return nc
            gbc = scb[:, 0:2048]
            S.dma("sp", gbc, mg_d[l:l + 1, :].partition_broadcast(128), t_scb, writes=[t_scb])
            for mt in range(2):
                xt = xall[:, 0:2048]
                S.dma("sp", xt, mem_d[mt * 128:(mt + 1) * 128, :], t_xq[0], writes=[t_xq[0], t_xq[1]])
                xs = kst[:, 0:2048]
                rmsnorm_rstd(xt, [t_xq[0], t_xq[1]], xs, [t_kst], mt)
                S.op("dve", lambda e, mt=mt: e.scalar_tensor_tensor(out=xs, in0=xt, scalar=st[:, mt:mt + 1], in1=gbc,
                                                                    op0=ALU.mult, op1=ALU.mult),
                     reads=[t_xq[0], t_xq[1], t_st, t_scb], writes=[t_kst])
                for k4 in range(4):
                    for kk in range(4):
                        k = k4 * 4 + kk
                        S.op("pe", lambda e, k=k, kk=kk: e.transpose(ptr[:, kk * 128:(kk + 1) * 128],
                                                                     xs[:, k * 128:(k + 1) * 128], ident[:]),
                             reads=[t_kst, t_ident], writes=[t_ptr])
                    evac_copy(xnT[:, k4 * 4:(k4 + 1) * 4, mt * 128:(mt + 1) * 128],
                              [t_xnT], ptr[:, 0:512].rearrange("p (a b) -> p a b", a=4), [t_ptr])
            wkv = wkv_d[l].rearrange("(k p) n -> p k n", p=128)
            for c in range(4):
                w_sb, w_tk = wload([(0, KC, 256, wkv[:, :, c * 256:(c + 1) * 256])])
                wv = wview(w_sb, 0, KC, 256)
                for cc in range(2):
                    ps, tps = fm_chunk(wv[:, :, cc * 128:(cc + 1) * 128], w_tk, KC,
                                       lambda k: xnT[:, k, 0:256], [t_xnT], 256)
                    evac_copy(mkT[:, c * 2 + cc, :], [t_mkT], ps[:, 0:256], [tps])
            for c in range(4):
                w_sb, w_tk = wload([(0, KC, 256, wkv[:, :, 1024 + c * 256:1024 + (c + 1) * 256])])
                wv = wview(w_sb, 0, KC, 256)
                for mt in range(2):
                    ps, tps = next_pj()
                    for k in range(KC):
                        S.op("pe", lambda e, k=k, mt=mt: e.matmul(ps[:, 0:256], xnT[:, k, mt * 128:(mt + 1) * 128],
                                                                  wv[:, k, :], start=(k == 0), stop=(k == KC - 1)),
                             reads=[t_xnT, w_tk], writes=[tps])
                    evac_copy(mv[:, mt, c * 256:(c + 1) * 256], [t_mv], ps[:, 0:256], [tps])

            if DBG == "memkv":
                S.drain()
                return nc
            for g in range(NG):
                tok0 = g * 512
                S.dma("sp", gbc, lng_d[l:l + 1, :].partition_broadcast(128), t_scb, writes=[t_scb])
                for t in range(4):
                    hx = t % 2
                    xt = xall[:, hx * 2048:(hx + 1) * 2048]
                    xtk = [t_xq[2 * hx], t_xq[2 * hx + 1]]
                    S.dma("sp", xt, xsrc[tok0 + t * 128: tok0 + (t + 1) * 128, :], xtk[0], reads=[xreg[g]], writes=xtk)
                    xs = kst[:, 0:2048]
                    rmsnorm_rstd(xt, xtk, xs, [t_kst], t)
                    S.op("dve", lambda e, t=t, xt=xt: e.scalar_tensor_tensor(out=xs, in0=xt, scalar=st[:, t:t + 1],
                                                                             in1=gbc, op0=ALU.mult, op1=ALU.mult),
                         reads=xtk + [t_st, t_scb], writes=[t_kst])
                    for k4 in range(4):
                        for kk in range(4):
                            k = k4 * 4 + kk
                            S.op("pe", lambda e, k=k, kk=kk: e.transpose(ptr[:, kk * 128:(kk + 1) * 128],
                                                                         xs[:, k * 128:(k + 1) * 128], ident[:]),
                                 reads=[t_kst, t_ident], writes=[t_ptr])
                        evac_copy(xnT[:, k4 * 4:(k4 + 1) * 4, t * 128:(t + 1) * 128],
                                  [t_xnT], ptr[:, 0:512].rearrange("p (a b) -> p a b", a=4), [t_ptr])

                if DBG == "stage0":
                    S.drain()
                    return nc
                for typ, ck, cv in (("a", C_AK, C_AV), ("s", C_SK, C_SV)):
                    kst3 = kst[:].rearrange("p (h t) -> p h t", h=8)
                    for c in range(4):
                        w_sb, w_tk = wload([(0, KC, 256, win_cols(l, ck + c * 256, 256))])
                        wv = wview(w_sb, 0, KC, 256)
                        for cc in range(2):
                            h = c * 2 + cc
                            ps, tps = fm_chunk(wv[:, :, cc * 128:(cc + 1) * 128], w_tk, KC,
                                               lambda k: xnT[:, k, :], [t_xnT], 512)
                            S.op("act", lambda e, h=h, ps=ps: e.activation(out=kst3[:, h, :], in_=ps[:], func=AF.Copy),
                                 reads=[tps], writes=[t_kst])
                            if typ == "a":
                                S.op("dve", lambda e, h=h: e.tensor_reduce(
                                    out=km32[:, h, 2 * g:2 * g + 2], in_=kst3[:, h, :].rearrange("p (b t) -> p b t", b=2),
                                    axis=AX.X, op=ALU.add), reads=[t_kst], writes=[t_km32])
                    S.dma("sp", kT_d[typ][:, :, tok0:tok0 + 512].rearrange("h d t -> d h t"), kst3, t_kst,
                          reads=[t_kst], writes=[kreg[typ][g]])
                    if DBG == "kvA":
                        S.drain()
                        return nc
                    if typ == "a":
                        S.op("dve", lambda e: e.tensor_scalar(kmT[:, :, 2 * g:2 * g + 2], km32[:, :, 2 * g:2 * g + 2],
                                                              1.0 / 256.0, None, op0=ALU.mult),
                             reads=[t_km32], writes=[t_kmT])
                    for c in range(4):
                        w_sb, w_tk = wload([(0, KC, 256, win_cols(l, cv + c * 256, 256))])
                        wv = wview(w_sb, 0, KC, 256)
                        for t in range(4):
                            ps, tps = next_pj()
                            for k in range(KC):
                                S.op("pe", lambda e, k=k, t=t, ps=ps: e.matmul(
                                    ps[:, 0:256], xnT[:, k, t * 128:(t + 1) * 128], wv[:, k, :],
                                    start=(k == 0), stop=(k == KC - 1)), reads=[t_xnT, w_tk], writes=[tps])
                            evac_copy(vst[:, t, c * 256:(c + 1) * 256], [t_vst], ps[:, 0:256], [tps])
                    S.dma("sp", v_d[typ][tok0:tok0 + 512, :].rearrange("(t p) c -> p t c", p=128), vst[:], t_vst,
                          reads=[t_vst], writes=[vreg[typ][g]])
                if DBG == "kvC":
                    S.drain()
                    return nc
                w_sb, w_tk = wload([(0, KC, 256, win_cols(l, C_IQ, 256))])
                wv = wview(w_sb, 0, KC, 256)
                for cc in range(2):
                    ps, tps = fm_chunk(wv[:, :, cc * 128:(cc + 1) * 128], w_tk, KC, lambda k: xnT[:, k, :], [t_xnT], 512)
                    evac_copy(qiT[:, cc, :], [t_qiT], ps[:], [tps])
                w_sb, w_tk = wload([(0, KC, 256, win_cols(l, C_IK - 64, 256))])
                wv = wview(w_sb, 0, KC, 256)
                ps, tps = fm_chunk(wv[:, :, 64:192], w_tk, KC, lambda k: xnT[:, k, :], [t_xnT], 512)
                S.op("act", lambda e, ps=ps: e.activation(out=kiT[0:64, tok0:tok0 + 512], in_=ps[0:64, :], func=AF.Copy),
                     reads=[tps], writes=[t_kiT])
                S.op("act", lambda e, ps=ps: e.activation(out=iwT[64:96, :], in_=ps[64:96, :], func=AF.Copy),
                     reads=[tps], writes=[t_iwT])
                ps, tps = fm_chunk(wv[:, :, 0:128], w_tk, KC, lambda k: xnT[:, k, :], [t_xnT], 512)
                S.op("act", lambda e, ps=ps: e.activation(out=kiT[64:128, tok0:tok0 + 512], in_=ps[64:128, :], func=AF.Copy),
                     reads=[tps], writes=[t_kiT])
                ps, tps = next_pj()
                for t in range(4):
                    S.op("pe", lambda e, t=t, ps=ps: e.matmul(ps[:, t * 16:(t + 1) * 16], iwT[64:96, t * 128:(t + 1) * 128],
                                                             sel[64:96, :], start=True, stop=True),
                         reads=[t_iwT, t_sel], writes=[tps])
                S.op("dve", lambda e, ps=ps: e.tensor_scalar(wI[:], ps[:, 0:64].rearrange("p (a b) -> p a b", a=4)[:, :, 0:4],
                                                             (64.0 ** -0.5) * 0.5, None, op0=ALU.mult),
                     reads=[tps], writes=[t_wI])
                if DBG == "kv":
                    S.drain()
                    return nc
                def kv_loader(typ, h, nkt, lookahead=2):
                    nch = (nkt + 7) // 8
                    state = {"issued": 0}

                    def issue(c):
                        slot = (kvc[0] + c) % NR
                        k_sb, k_tk = kring[slot]
                        v_sb, v_tk = vring[slot]
                        k0 = c * 1024
                        n = min(1024, nkt * 128 - k0)
                        gs = sorted(set((k0 + i * 128) // 512 for i in range(n // 128)))
                        S.dma("sp", k_sb[:, 0:n], kT_d[typ][h, :, k0:k0 + n], k_tk,
                              reads=[kreg[typ][gg] for gg in gs], writes=[k_tk])
                        S.dma("sp", v_sb[:, 0:n // 128, :],
                              v_d[typ][k0:k0 + n, h * 128:(h + 1) * 128].rearrange("(a p) d -> p a d", p=128), v_tk,
                              reads=[vreg[typ][gg] for gg in gs], writes=[v_tk])

                    def get(kt):
                        c = kt // 8
                        while state["issued"] < min(nch, c + 1 + lookahead):
                            issue(state["issued"])
                            state["issued"] += 1
                        slot = (kvc[0] + c) % NR
                        k_sb, k_tk = kring[slot]
                        v_sb, v_tk = vring[slot]
                        j = kt % 8
                        return k_sb[:, j * 128:(j + 1) * 128], k_tk, v_sb[:, j, :], v_tk

                    def done():
                        kvc[0] += nch
                    return get, done

                def attention(nkt, qcols, q_ap, q_tk, get_kv, mask_fn, scale):
                    pend = []
                    for kt in range(nkt + 1):
                        if kt < nkt:
                            c0, masks = mask_fn(kt)
                            sp_, tsp = stp[kt % 2]
                            k_ap, k_tk, v_ap, v_tk = get_kv(kt)
                            nm_ = len(masks)
                            S.op("pe", lambda e, sp_=sp_, k_ap=k_ap, c0=c0, nm_=nm_: e.matmul(
                                sp_[:, c0:qcols], k_ap, q_ap[:, c0:qcols], start=True, stop=(nm_ == 0)),
                                reads=[k_tk, q_tk], writes=[tsp])
                            for i, (ml, mr, lo, hi, mtks) in enumerate(masks):
                                S.op("pe", lambda e, sp_=sp_, ml=ml, mr=mr, lo=lo, hi=hi, i=i, nm_=nm_: e.matmul(
                                    sp_[:, lo:hi], ml, mr, start=False, stop=(i == nm_ - 1)),
                                    reads=mtks, writes=[tsp])
                            pend.append((kt, c0, sp_, tsp, v_ap, v_tk))
                        if kt > 0:
                            pk, c0, sp_, tsp, v_ap, v_tk = pend.pop(0)
                            p_sb, p_tk = pT[pk % 3]
                            S.op("act", lambda e, p_sb=p_sb, sp_=sp_, c0=c0: e.activation(
                                out=p_sb[:, c0:qcols], in_=sp_[:, c0:qcols], func=AF.Exp, scale=scale),
                                reads=[tsp], writes=[p_tk])
                            S.op("pe", lambda e, v_ap=v_ap, p_sb=p_sb, c0=c0, pk=pk: e.matmul(
                                ot[:, c0:qcols], v_ap, p_sb[:, c0:qcols], start=(pk == 0), stop=(pk == nkt - 1)),
                                reads=[v_tk, p_tk], writes=[t_ot])
                            S.op("pe", lambda e, p_sb=p_sb, c0=c0, pk=pk: e.matmul(
                                dn[:, c0:qcols], ones[:], p_sb[:, c0:qcols], start=(pk == 0), stop=(pk == nkt - 1)),
                                reads=[t_ones, p_tk], writes=[t_dn])

                def normalize(o_ap, o_tk, qcols, sg_ap, sg_tk, y_ap, y_tk):
                    rden, t_rden = Fs[3]
                    t1, t_t1 = Fs[4]
                    S.op("dve", lambda e: e.reciprocal(rden[:, 0:qcols], dn[:, 0:qcols]), reads=[t_dn], writes=[t_rden])
                    S.op("dve", lambda e: e.tensor_tensor(out=t1[:, 0:qcols], in0=o_ap, in1=rden[:, 0:qcols], op=ALU.mult),
                         reads=[o_tk, t_rden], writes=[t_t1])
                    S.op("pool", lambda e: e.tensor_tensor(out=y_ap, in0=t1[:, 0:qcols], in1=sg_ap, op=ALU.mult),
                         reads=[t_t1, sg_tk], writes=[y_tk])

                for h in range(8):
                    q_sb, q_tk = qh[h % 2]
                    g_sb, g_tk = sgh[h % 2]
                    w_sb, w_tk = wload([(0, KC, 256, win_cols(l, C_AQ + h * 128, 128, reps=2, stride=C_AG - C_AQ))])
                    wv = wview(w_sb, 0, KC, 256)
                    ps, tps = fm_chunk(wv[:, :, 0:128], w_tk, KC, lambda k: xnT[:, k, :], [t_xnT], 512)
                    S.op("act", lambda e, ps=ps, q_sb=q_sb: e.activation(out=q_sb[:], in_=ps[:], func=AF.Copy),
                         reads=[tps], writes=[q_tk])
                    ps, tps = fm_chunk(wv[:, :, 128:256], w_tk, KC, lambda k: xnT[:, k, :], [t_xnT], 512)
                    S.op("act", lambda e, ps=ps, g_sb=g_sb: e.activation(out=g_sb[:], in_=ps[:], func=AF.Silu),
                         reads=[tps], writes=[g_tk])
                    ps, tps = next_pj()
                    for t in range(4):
                        S.op("pe", lambda e, t=t, ps=ps, q_sb=q_sb: e.matmul(
                            ps[:, t * 16:(t + 1) * 16], q_sb[:, t * 128:(t + 1) * 128], kmT[:, h, :], start=True, stop=True),
                            reads=[q_tk, t_kmT], writes=[tps])
                    S.op("dve", lambda e, ps=ps: e.tensor_copy(bsm[:].rearrange("p a b -> p (a b)"), ps[:, 0:64]),
                         reads=[tps], writes=[t_bsm])
                    S.op("dve", lambda e: e.memset(bsm[:, 0:2, 2 * g:16], -BIG), writes=[t_bsm])
                    S.op("dve", lambda e: e.memset(bsm[:, 2:4, 2 * g + 1:16], -BIG), writes=[t_bsm])
                    for t in range(4):
                        S.op("dve", lambda e, t=t: e.max(out=m8[:, t, :], in_=bsm[:, t, :]), reads=[t_bsm], writes=[t_m8])
                    for t in range(4):
                        S.op("dve", lambda e, t=t: e.tensor_scalar(mbf[:, t, :], bsm[:, t, :], m8[:, t, 2:3], NEG,
                                                                  op0=ALU.is_lt, op1=ALU.mult),
                             reads=[t_bsm, t_m8], writes=[t_mbf])
                    for t in range(4):
                        S.op("pe", lambda e, t=t: e.transpose(ptr[0:16, t * 128:(t + 1) * 128], mbf[:, t, :], ident[:]),
                             reads=[t_mbf, t_ident], writes=[t_ptr])
                    S.op("dve", lambda e: e.tensor_copy(mbT[:, :], ptr[0:16, 0:512]), reads=[t_ptr], writes=[t_mbT])

                    def moba_mask(kt):
                        a = kt - 4 * g
                        c0 = 128 * max(a, 0)
                        i = kt // 2
                        masks = []
                        if i < 2 * g:
                            masks.append((eone[:, i * 128:(i + 1) * 128], mbT[:, 0:512], 0, 512, [t_eone, t_mbT]))
                        elif i == 2 * g:
                            masks.append((eone[:, i * 128:(i + 1) * 128], mbT[:, 256:512], 256, 512, [t_eone, t_mbT]))
                            masks.append((ident[:], tri[:], a * 128, (a + 1) * 128, [t_ident, t_tri]))
                        else:
                            masks.append((ident[:], tri[:], a * 128, (a + 1) * 128, [t_ident, t_tri]))
                        return c0, masks

                    get_kv, kv_done = kv_loader("a", h, 4 * g + 4)
                    attention(4 * g + 4, 512, q_sb, q_tk, get_kv, moba_mask, 128.0 ** -0.5)
                    kv_done()
                    normalize(ot[:, 0:512], t_ot, 512, g_sb[:], g_tk, yT[0][0][:, h, :], yT[0][1])

                if DBG == "moba":
                    S.drain()
                    return nc
                for j in range(8):
                    ccs, t_ccs = Fs[0]
                    u, t_u = Fs[1]
                    cacc, t_cacc = Fs[2]
                    w_sb, w_tk = wload([(0, KC, 256, win_cols(l, C_CC + j * 128, 128, reps=2, stride=C_CH - C_CC))])
                    wv = wview(w_sb, 0, KC, 256)
                    ps, tps = fm_chunk(wv[:, :, 0:128], w_tk, KC, lambda k: xnT[:, k, :], [t_xnT], 512)
                    S.op("act", lambda e, ps=ps: e.activation(out=ccs[:, 0:512], in_=ps[:], func=AF.Copy),
                         reads=[tps], writes=[t_ccs])
                    ps, tps = fm_chunk(wv[:, :, 128:256], w_tk, KC, lambda k: xnT[:, k, :], [t_xnT], 512)
                    S.op("dve", lambda e, ps=ps: e.tensor_tensor(out=u[:, 2:514], in0=ps[:], in1=ccs[:, 0:512], op=ALU.mult),
                         reads=[tps, t_ccs], writes=[t_u])
                    S.op("dve", lambda e, j=j: e.tensor_copy(u[:, 0:2], carry[:, j, :]), reads=[t_carry], writes=[t_u])
                    S.op("act", lambda e, j=j: e.activation(out=cacc[:, 0:512], in_=u[:, 2:514], func=AF.Identity,
                                                            scale=cw[:, j, 2:3], bias=cbias[:, j:j + 1]),
                         reads=[t_u, t_cw, t_cbias], writes=[t_cacc])
                    S.op("dve", lambda e, j=j: e.scalar_tensor_tensor(out=cacc[:, 0:512], in0=u[:, 1:513], scalar=cw[:, j, 1:2],
                                                                      in1=cacc[:, 0:512], op0=ALU.mult, op1=ALU.add),
                         reads=[t_u, t_cw, t_cacc], writes=[t_cacc])
                    S.op("dve", lambda e, j=j: e.scalar_tensor_tensor(out=cacc[:, 0:512], in0=u[:, 0:512], scalar=cw[:, j, 0:1],
                                                                      in1=cacc[:, 0:512], op0=ALU.mult, op1=ALU.add),
                         reads=[t_u, t_cw, t_cacc], writes=[t_cacc])
                    S.op("dve", lambda e, j=j: e.tensor_copy(carry[:, j, :], u[:, 512:514]), reads=[t_u], writes=[t_carry])
                    w_sb, w_tk = wload([(0, KC, 256, win_cols(l, C_CB + j * 128, 128, reps=2, stride=C_CG - C_CB))])
                    wv = wview(w_sb, 0, KC, 256)
                    ps, tps = fm_chunk(wv[:, :, 128:256], w_tk, KC, lambda k: xnT[:, k, :], [t_xnT], 512)
                    S.op("act", lambda e, ps=ps: e.activation(out=sgc[:], in_=ps[:], func=AF.Silu), reads=[tps], writes=[t_sgc])
                    ps, tps = fm_chunk(wv[:, :, 0:128], w_tk, KC, lambda k: xnT[:, k, :], [t_xnT], 512)
                    S.op("dve", lambda e, ps=ps: e.tensor_tensor(out=cacc[:, 0:512], in0=ps[:], in1=cacc[:, 0:512], op=ALU.mult),
                         reads=[tps, t_cacc], writes=[t_cacc])
                    S.op("pool", lambda e, j=j: e.tensor_tensor(out=yT[1][0][:, j, :], in0=cacc[:, 0:512], in1=sgc[:], op=ALU.mult),
                         reads=[t_cacc, t_sgc], writes=[yT[1][1]])

                if DBG == "conv":
                    S.drain()
                    return nc
                for hh in range(2):
                    for tt in range(2):
                        t = hh * 2 + tt
                        Q = 4 * g + t
                        nk = (Q + 1) * 128
                        mb_ap = mball[:, tt * MBH: tt * MBH + nk]
                        nchunk = (nk + 511) // 512
                        for c in range(nchunk):
                            wdt = min(512, nk - c * 512)
                            for j in range(4):
                                pb = 64 * (j % 2)
                                ps, tps = next_pj()
                                rl, t_rl = Fs[j % 2]
                                S.op("pe", lambda e, ps=ps, pb=pb, j=j, c=c, wdt=wdt, t=t: e.matmul(
                                    ps[:, 0:wdt], qiT[pb:pb + 64, j // 2, t * 128:(t + 1) * 128],
                                    kiT[pb:pb + 64, c * 512:c * 512 + wdt], start=True, stop=True),
                                    reads=[t_qiT, t_kiT], writes=[tps])
                                S.op("act", lambda e, ps=ps, rl=rl, wdt=wdt: e.activation(out=rl[:, 0:wdt], in_=ps[:, 0:wdt], func=AF.Relu),
                                     reads=[tps], writes=[t_rl])
                                if j == 0:
                                    S.op("dve", lambda e, rl=rl, c=c, wdt=wdt, t=t: e.tensor_scalar(
                                        scb[:, c * 512:c * 512 + wdt], rl[:, 0:wdt], wI[:, t, 0:1], None, op0=ALU.mult),
                                        reads=[t_rl, t_wI], writes=[t_scb])
                                else:
                                    S.op("dve", lambda e, rl=rl, c=c, wdt=wdt, t=t, j=j: e.scalar_tensor_tensor(
                                        out=scb[:, c * 512:c * 512 + wdt], in0=rl[:, 0:wdt], scalar=wI[:, t, j:j + 1],
                                        in1=scb[:, c * 512:c * 512 + wdt], op0=ALU.mult, op1=ALU.add),
                                        reads=[t_rl, t_wI, t_scb], writes=[t_scb])
                        S.op("dve", lambda e, nk=nk: e.tensor_reduce(out=bis[:, 0:1], in_=scb[:, 0:nk], axis=AX.X, op=ALU.min),
                             reads=[t_scb], writes=[t_bis])
                        S.op("dve", lambda e, nk=nk: e.tensor_reduce(out=bis[:, 1:2], in_=scb[:, 0:nk], axis=AX.X, op=ALU.max),
                             reads=[t_scb], writes=[t_bis])
                        S.op("dve", lambda e, Q=Q: e.tensor_tensor(out=scb[:, Q * 128:(Q + 1) * 128], in0=scb[:, Q * 128:(Q + 1) * 128],
                                                                   in1=triq[:], op=ALU.add), reads=[t_scb, t_triq], writes=[t_scb])
                        S.op("dve", lambda e: e.tensor_tensor(out=bis[:, 2:3], in0=bis[:, 1:2], in1=bis[:, 0:1], op=ALU.subtract),
                             reads=[t_bis], writes=[t_bis])
                        S.op("dve", lambda e: e.tensor_scalar(bis[:, 2:3], bis[:, 2:3], 1.0001, 1e-6, op0=ALU.mult, op1=ALU.add),
                             reads=[t_bis], writes=[t_bis])
                        S.op("dve", lambda e: e.tensor_scalar(halves[:], pw2[:], bis[:, 2:3], None, op0=ALU.mult),
                             reads=[t_pw2, t_bis], writes=[t_halves])
                        S.op("dve", lambda e: e.scalar_tensor_tensor(out=nm[:, 0:1], in0=bis[:, 0:1], scalar=-1.0, in1=halves[:, 0:1],
                                                                     op0=ALU.mult, op1=ALU.subtract),
                             reads=[t_bis, t_halves], writes=[t_nm])
                        S.op("dve", lambda e: e.memset(ssum[:], 0.0), writes=[t_ssum])
                        cthr = float(2 * min(TOPK, T // 4) - nk)
                        for it in range(NIT):
                            S.op("act", lambda e, it=it, nk=nk: e.activation(out=kst[:, 0:nk], in_=scb[:, 0:nk], func=AF.Sign,
                                                                             bias=nm[:, it:it + 1], scale=1.0,
                                                                             accum_out=ssum[:, it:it + 1]),
                                 reads=[t_scb, t_nm], writes=[t_kst, t_ssum])
                            S.op("dve", lambda e, it=it: e.tensor_scalar(dtmp[:, it:it + 1], ssum[:, it:it + 1], cthr, -0.5,
                                                                         op0=ALU.is_lt, op1=ALU.add),
                                 reads=[t_ssum], writes=[t_dtmp])
                            S.op("dve", lambda e, it=it: e.scalar_tensor_tensor(out=nm[:, it + 1:it + 2], in0=dtmp[:, it:it + 1],
                                                                                scalar=halves[:, it:it + 1], in1=nm[:, it:it + 1],
                                                                                op0=ALU.mult, op1=ALU.add),
                                 reads=[t_dtmp, t_halves, t_nm], writes=[t_nm])
                        S.op("dve", lambda e: e.scalar_tensor_tensor(out=bis[:, 3:4], in0=nm[:, NIT:NIT + 1], scalar=-1.0,
                                                                     in1=halves[:, NIT + 1:NIT + 2], op0=ALU.mult, op1=ALU.subtract),
                             reads=[t_nm, t_halves], writes=[t_bis])
                        S.op("dve", lambda e, mb_ap=mb_ap, nk=nk: e.tensor_scalar(mb_ap, scb[:, 0:nk], bis[:, 3:4], NEG,
                                                                                 op0=ALU.is_lt, op1=ALU.mult),
                             reads=[t_scb, t_bis], writes=[t_mb[tt]])
                    nkt = 4 * g + 2 * hh + 2
                    for h in range(8):
                        q_sb, q_tk = qh[h % 2]
                        g_sb, g_tk = sgh[h % 2]
                        w_sb, w_tk = wload([(0, KC, 256, win_cols(l, C_SQ + h * 128, 128, reps=2, stride=C_SG - C_SQ))])
                        wv = wview(w_sb, 0, KC, 256)
                        ps, tps = fm_chunk(wv[:, :, 0:128], w_tk, KC, lambda k: xnT[:, k, hh * 256:(hh + 1) * 256], [t_xnT], 256)
                        S.op("act", lambda e, ps=ps, q_sb=q_sb: e.activation(out=q_sb[:, 0:256], in_=ps[:, 0:256], func=AF.Copy),
                             reads=[tps], writes=[q_tk])
                        ps, tps = fm_chunk(wv[:, :, 128:256], w_tk, KC, lambda k: xnT[:, k, hh * 256:(hh + 1) * 256], [t_xnT], 256)
                        S.op("act", lambda e, ps=ps, g_sb=g_sb: e.activation(out=g_sb[:, 0:256], in_=ps[:, 0:256], func=AF.Silu),
                             reads=[tps], writes=[g_tk])

                        def dsa_mask(kt):
                            a = kt - (4 * g + 2 * hh)
                            c0 = 128 * max(a, 0)
                            masks = []
                            for tt in range(2):
                                if kt <= 4 * g + 2 * hh + tt:
                                    masks.append((mball[:, tt * MBH + kt * 128: tt * MBH + (kt + 1) * 128], ident[:],
                                                  tt * 128, (tt + 1) * 128, [t_mb[tt], t_ident]))
                            return c0, masks

                        get_kv, kv_done = kv_loader("s", h, nkt)
                        attention(nkt, 256, q_sb, q_tk, get_kv, dsa_mask, 128.0 ** -0.5)
                        kv_done()
                        normalize(ot[:, 0:256], t_ot, 256, g_sb[:, 0:256], g_tk,
                                  yT[2][0][:, h, hh * 256:(hh + 1) * 256], yT[2][1])

                if DBG == "dsa":
                    S.drain()
                    return nc
                for h in range(4):
                    for dc in range(2):
                        q_sb, q_tk = qh[dc]
                        g_sb, g_tk = sgh[dc]
                        cidx = h * 2 + dc
                        w_sb, w_tk = wload([(0, KC, 256, win_cols(l, C_MQ + cidx * 128, 128, reps=2, stride=C_MG - C_MQ))])
                        wv = wview(w_sb, 0, KC, 256)
                        ps, tps = fm_chunk(wv[:, :, 0:128], w_tk, KC, lambda k: xnT[:, k, :], [t_xnT], 512)
                        S.op("act", lambda e, ps=ps, q_sb=q_sb: e.activation(out=q_sb[:], in_=ps[:], func=AF.Copy),
                             reads=[tps], writes=[q_tk])
                        ps, tps = fm_chunk(wv[:, :, 128:256], w_tk, KC, lambda k: xnT[:, k, :], [t_xnT], 512)
                        S.op("act", lambda e, ps=ps, g_sb=g_sb: e.activation(out=g_sb[:], in_=ps[:], func=AF.Silu),
                             reads=[tps], writes=[g_tk])
                    pts = []
                    for mt in range(2):
                        sp_, tsp = stp[mt]
                        for dc in range(2):
                            S.op("pe", lambda e, sp_=sp_, mt=mt, dc=dc: e.matmul(
                                sp_[:], mkT[:, h * 2 + dc, mt * 128:(mt + 1) * 128], qh[dc][0][:], start=(dc == 0), stop=(dc == 1)),
                                reads=[t_mkT, qh[dc][1]], writes=[tsp])
                        p_sb, p_tk = pT[mt]
                        S.op("act", lambda e, sp_=sp_, p_sb=p_sb: e.activation(out=p_sb[:], in_=sp_[:], func=AF.Exp, scale=256.0 ** -0.5),
                             reads=[tsp], writes=[p_tk])
                        pts.append((p_sb, p_tk))
                    o2, t_o2 = pj[0]
                    for mt in range(2):
                        p_sb, p_tk = pts[mt]
                        S.op("pe", lambda e, mt=mt, p_sb=p_sb: e.matmul(ot[:], mv[:, mt, h * 256:h * 256 + 128], p_sb[:],
                                                                        start=(mt == 0), stop=(mt == 1)),
                             reads=[t_mv, p_tk], writes=[t_ot])
                        S.op("pe", lambda e, mt=mt, p_sb=p_sb: e.matmul(o2[:], mv[:, mt, h * 256 + 128:h * 256 + 256], p_sb[:],
                                                                        start=(mt == 0), stop=(mt == 1)),
                             reads=[t_mv, p_tk], writes=[t_o2])
                        S.op("pe", lambda e, mt=mt, p_sb=p_sb: e.matmul(dn[:], ones[:], p_sb[:], start=(mt == 0), stop=(mt == 1)),
                             reads=[t_ones, p_tk], writes=[t_dn])
                    normalize(ot[:], t_ot, 512, sgh[0][0][:], sgh[0][1], yT[3][0][:, h * 2, :], yT[3][1])
                    normalize(o2[:], t_o2, 512, sgh[1][0][:], sgh[1][1], yT[3][0][:, h * 2 + 1, :], yT[3][1])

                if DBG == "mem":
                    S.drain()
                    return nc
                mrg = mball[:, 0:8192].rearrange("p (c t) -> p c t", c=16)
                t_mrg = t_mb
                for oc in range(16):
                    acc, t_acc = Fs[oc % 2]
                    for br in range(4):
                        tmp, t_tmp = Fs[2 + (br % 2)]
                        sg_sb, sg_tk = sgt[br % 2]
                        w_sb, w_tk = wload([(0, KC, 128, win_cols(l, C_R + 2048 * br + 128 * oc, 128)),
                                            (KC * 128, 8, 128,
                                             wbr_d[l, br].rearrange("(k p) n -> p k n", p=128)[:, :, oc * 128:(oc + 1) * 128])])
                        wr = wview(w_sb, 0, KC, 128)
                        wb = wview(w_sb, KC * 128, 8, 128)
                        ps, tps = fm_chunk(wr, w_tk, KC, lambda k: xnT[:, k, :], [t_xnT], 512)
                        S.op("act", lambda e, ps=ps, sg_sb=sg_sb: e.activation(out=sg_sb[:], in_=ps[:], func=AF.Sigmoid),
                             reads=[tps], writes=[sg_tk])
                        y_sb, y_tk = yT[br]
                        ps, tps = fm_chunk(wb, w_tk, 8, lambda k, y_sb=y_sb: y_sb[:, k, :], [y_tk], 512)
                        if br == 0:
                            S.op("dve", lambda e, ps=ps, sg_sb=sg_sb, acc=acc: e.tensor_tensor(
                                out=acc[:, 0:512], in0=ps[:], in1=sg_sb[:], op=ALU.mult), reads=[tps, sg_tk], writes=[t_acc])
                        else:
                            S.op("dve", lambda e, ps=ps, sg_sb=sg_sb, tmp=tmp: e.tensor_tensor(
                                out=tmp[:, 0:512], in0=ps[:], in1=sg_sb[:], op=ALU.mult), reads=[tps, sg_tk], writes=[t_tmp])
                            if br < 3:
                                S.op("pool", lambda e, tmp=tmp, acc=acc: e.tensor_tensor(
                                    out=acc[:, 0:512], in0=acc[:, 0:512], in1=tmp[:, 0:512], op=ALU.add),
                                    reads=[t_acc, t_tmp], writes=[t_acc])
                            else:
                                S.op("pool", lambda e, tmp=tmp, acc=acc, oc=oc: e.tensor_tensor(
                                    out=mrg[:, oc, :], in0=acc[:, 0:512], in1=tmp[:, 0:512], op=ALU.add),
                                    reads=[t_acc, t_tmp], writes=[t_mb[0], t_mb[1]])

                if DBG == "merge":
                    S.drain()
                    return nc
                wo = wout_d[l].rearrange("(k p) n -> p k n", p=128)
                for cb in range(8):
                    w_sb, w_tk = wload([(0, KC, 256, wo[:, :, cb * 256:(cb + 1) * 256])])
                    wv = wview(w_sb, 0, KC, 256)
                    xr = xall[:, (cb % 2) * 1024:(cb % 2 + 1) * 1024].rearrange("p (t c) -> p t c", t=4)
                    t_xr = t_xq[cb % 2]
                    xo = xall[:, 2048 + (cb % 2) * 1024: 2048 + (cb % 2 + 1) * 1024].rearrange("p (t c) -> p t c", t=4)
                    t_xo = t_xq[2 + cb % 2]
                    S.dma("sp", xr, xsrc[tok0:tok0 + 512, cb * 256:(cb + 1) * 256].rearrange("(t p) c -> p t c", p=128),
                          t_xr, reads=[xreg[g]], writes=[t_xr])
                    for t in range(4):
                        ps, tps = next_pj()
                        for k in range(KC):
                            S.op("pe", lambda e, k=k, t=t, ps=ps: e.matmul(
                                ps[:, 0:256], mrg[:, k, t * 128:(t + 1) * 128], wv[:, k, :], start=(k == 0), stop=(k == KC - 1)),
                                reads=[t_mb[0], t_mb[1], w_tk], writes=[tps])
                        S.op("dve", lambda e, t=t, ps=ps, xo=xo, xr=xr: e.tensor_tensor(
                            out=xo[:, t, :], in0=ps[:, 0:256], in1=xr[:, t, :], op=ALU.add),
                            reads=[tps, t_xr], writes=[t_xo])
                    S.dma("sp", xmid_d[tok0:tok0 + 512, cb * 256:(cb + 1) * 256].rearrange("(t p) c -> p t c", p=128), xo,
                          t_xo, reads=[t_xo], writes=[xreg[g]] if l > 0 or True else [])
            xreg_prev = xreg
            kreg_prev = kreg
            vreg_prev = vreg

        gbc = scb[:, 0:2048]
        S.dma("sp", gbc, fg_d.partition_broadcast(128), t_scb, writes=[t_scb])
        for tt in range(NT):
            hx = tt % 2
            xt = xall[:, hx * 2048:(hx + 1) * 2048]
            xtk = [t_xq[2 * hx], t_xq[2 * hx + 1]]
            S.dma("sp", xt, xmid_d[tt * 128:(tt + 1) * 128, :], xtk[0], reads=[xreg_prev[tt // 4]], writes=xtk)
            rmsnorm_rstd(xt, xtk, kst[:, 0:2048], [t_kst], tt % 8)
            S.op("dve", lambda e, xt=xt, c=tt % 8: e.scalar_tensor_tensor(out=xt, in0=xt, scalar=st[:, c:c + 1], in1=gbc,
                                                                          op0=ALU.mult, op1=ALU.mult),
                 reads=xtk + [t_st, t_scb], writes=xtk)
            S.dma("sp", y_d[tt * 128:(tt + 1) * 128, :], xt, xtk[1], reads=xtk, writes=[])
        S.final_wait("sp", t_xq)
        S.drain()
    return nc


kvc = [0]
DBG = None


def _consts():
    k = np.arange(128)
    tri = np.where(k[None, :] >= k[:, None], 0.0, NEG).astype(np.float32)
    triq = np.where(k[None, :] <= k[:, None], 0.0, -BIG).astype(np.float32)
    eone = np.zeros((16, 2048), np.float32)
    for i in range(16):
        eone[i, i * 128:(i + 1) * 128] = 1.0
    pw2 = np.zeros((128, NIT + 2), np.float32)
    for i in range(NIT + 1):
        pw2[:, i] = 2.0 ** -(i + 1)
    pw2[:, NIT + 1] = 1.25 * 2.0 ** -(NIT + 1)
    selm = np.zeros((128, 16), np.float32)
    for j in range(4):
        selm[64 + j, j] = 1.0
    return {"c_sel": selm, "c_ident": np.eye(128, dtype=np.float32), "c_tri": tri, "c_triq": triq, "c_eone": eone, "c_pw2": pw2}


_CACHE = {}


def run(inputs, T, L, n_cores, batch_of_core):
    key = (T, L)
    if key not in _CACHE:
        kvc[0] = 0
        _CACHE[key] = build(T, L)
    nc = _CACHE[key]
    cst = _consts()
    f = lambda a: np.ascontiguousarray(np.asarray(a, dtype=np.float32))
    shared = {
        "ln_g": f(inputs["ln_g"]), "w_in": f(inputs["w_in"]), "conv_w": f(inputs["conv_w"]),
        "conv_b": f(inputs["conv_b"]), "mem_ln_g": f(inputs["mem_ln_g"]), "w_mem_kv": f(inputs["w_mem_kv"]),
        "w_branch": f(inputs["w_branch"]), "w_out": f(inputs["w_out"]),
        "final_g": f(inputs["final_g"]).reshape(1, D), **cst,
    }
    in_maps = []
    for c in range(n_cores):
        b = batch_of_core[c]
        m = dict(shared)
        m["x"] = f(inputs["x"][b])
        m["mem"] = f(inputs["mem"][b])
        in_maps.append(m)
    res = run_bass_kernel_spmd(nc, in_maps, core_ids=list(range(n_cores)))
    return [r["y"] for r in res.results]


def kernel(x, mem, ln_g, w_in, conv_w, conv_b, mem_ln_g, w_mem_kv, w_branch, w_out, final_g):
    inputs = dict(x=x, mem=mem, ln_g=ln_g, w_in=w_in, conv_w=conv_w, conv_b=conv_b, mem_ln_g=mem_ln_g,
                  w_mem_kv=w_mem_kv, w_branch=w_branch, w_out=w_out, final_g=final_g)
    B, T, _ = np.asarray(x).shape
    L = np.asarray(ln_g).shape[0]
    outs = run(inputs, T, L, 8, [c % B for c in range(8)])
    return np.stack([outs[b] for b in range(B)], axis=0).astype(np.float32)
```

```python
import os
import numpy as np
import concourse.bass as bass
import concourse.mybir as mybir
from concourse.bass_utils import run_bass_kernel_spmd
from contextlib import ExitStack

F32 = mybir.dt.float32
BF16 = mybir.dt.bfloat16
AF = mybir.ActivationFunctionType
ALU = mybir.AluOpType
AX = mybir.AxisListType

D = 2048
KC = 16
BW = 1024
INC = 22852
NEG = -30000.0
BIG = 1.0e30
EPS = 1e-6
C_AQ, C_AK, C_AV, C_AG = 0, 1024, 2048, 3072
C_CB, C_CC, C_CH, C_CG = 4096, 5120, 6144, 7168
C_SQ, C_SK, C_SV, C_SG = 8192, 9216, 10240, 11264
C_MQ, C_MG = 12288, 13312
C_IQ, C_IK, C_IW, C_R = 14336, 14592, 14656, 14660
NIT = 16
TOPK = 256


class Tk:
    __slots__ = ("name", "w", "r", "dsem", "dcnt")

    def __init__(self, name):
        self.name = name
        self.w = None
        self.r = []
        self.dsem = None
        self.dcnt = 0


class Sched:
    def __init__(self, nc, es):
        self.nc = nc
        self.es = es
        self.eng = {"pe": nc.tensor, "act": nc.scalar, "dve": nc.vector, "pool": nc.gpsimd, "sp": nc.sync}
        self.sem = {k: es.enter_context(nc.semaphore("sem_" + k)) for k in self.eng}
        self.cnt = {k: 0 for k in self.eng}
        self.waited = {k: {} for k in self.eng}
        self.ninst = 0
        self.owners = []

    def drain(self):
        for k in self.eng:
            if self.cnt[k] > 0 and k != "sp":
                self.eng["sp"].wait_ge(self.sem[k], self.cnt[k])
        for o in self.owners:
            self.eng["sp"].wait_ge(o.dsem, o.dcnt)

    def _deps(self, e, reads, writes):
        deps = {}

        def add(tk, war):
            if tk is None:
                return
            key, sh, val, te = tk
            if te == e and e == "pe":
                return
            if deps.get(key, (None, 0))[1] < val:
                deps[key] = (sh, val)

        for t in reads:
            add(t.w, False)
        for t in writes:
            add(t.w, False)
            for rt in t.r:
                add(rt, True)
        return deps

    def _wait(self, e, deps):
        for key, (sh, val) in deps.items():
            if self.waited[e].get(key, 0) >= val:
                continue
            self.eng[e].wait_ge(sh, val)
            self.waited[e][key] = val

    def _record(self, tk, reads, writes):
        for t in writes:
            t.w = tk
            t.r = []
        for t in reads:
            if t in writes:
                continue
            t.r = [x for x in t.r if x[0] != tk[0]] + [tk]

    def op(self, e, fn, reads=(), writes=()):
        reads = list(reads)
        writes = list(writes)
        self._wait(e, self._deps(e, reads, writes))
        ins = fn(self.eng[e])
        self.cnt[e] += 1
        ins.then_inc(self.sem[e], 1)
        tk = (e, self.sem[e], self.cnt[e], e)
        self._record(tk, reads, writes)
        self.ninst += 1
        return tk

    def dma(self, q, out_ap, in_ap, owner, reads=(), writes=(), **kw):
        reads = list(reads)
        writes = list(writes)
        if owner.dsem is None:
            owner.dsem = self.es.enter_context(self.nc.semaphore("d_" + owner.name))
            self.owners.append(owner)
        self._wait(q, self._deps(q, reads, writes))
        ins = self.eng[q].dma_start(out=out_ap, in_=in_ap, **kw)
        owner.dcnt += 16
        ins.then_inc(owner.dsem, 16)
        tk = ("d_" + owner.name, owner.dsem, owner.dcnt, None)
        self._record(tk, reads, writes)
        self.ninst += 1
        return tk

    def final_wait(self, e, tiles):
        deps = {}
        for t in tiles:
            for tk in [t.w] + t.r:
                if tk is None:
                    continue
                key, sh, val, te = tk
                if deps.get(key, (None, 0))[1] < val:
                    deps[key] = (sh, val)
        self._wait(e, deps)


def build(T, L):
    NG = T // 512
    NT = T // 128
    nc = bass.Bass("TRN2", target_bir_lowering=False)
    es = ExitStack()

    def din(name, shape):
        return nc.dram_tensor(name, shape, F32, kind="ExternalInput").ap()

    x_d = din("x", [T, D])
    mem_d = din("mem", [256, D])
    lng_d = din("ln_g", [L, D])
    win_d = din("w_in", [L, D, INC])
    cw_d = din("conv_w", [L, 3, BW])
    cb_d = din("conv_b", [L, BW])
    mg_d = din("mem_ln_g", [L, D])
    wkv_d = din("w_mem_kv", [L, D, 2048])
    wbr_d = din("w_branch", [L, 4, BW, D])
    wout_d = din("w_out", [L, D, D])
    fg_d = din("final_g", [1, D])
    ident_d = din("c_ident", [128, 128])
    tri_d = din("c_tri", [128, 128])
    triq_d = din("c_triq", [128, 128])
    eone_d = din("c_eone", [16, 2048])
    pw2_d = din("c_pw2", [128, NIT + 2])
    sel_d = din("c_sel", [128, 16])
    y_d = nc.dram_tensor("y", [T, D], F32, kind="ExternalOutput").ap()
    xmid_d = nc.dram_tensor("xmid", [T, D], F32).ap()
    kT_d = {t: nc.dram_tensor("kT" + t, [8, 128, T], BF16).ap() for t in "as"}
    v_d = {t: nc.dram_tensor("vv" + t, [T, 1024], BF16).ap() for t in "as"}

    with es:
        S = Sched(nc, es)

        def sb(name, shape, dt=F32):
            return es.enter_context(nc.sbuf_tensor(name, shape, dt)), Tk(name)

        def psb(name, shape, dt=F32):
            return es.enter_context(nc.psum_tensor(name, shape, dt)), Tk(name)

        NW = 4
        wt = [sb(f"wt{i}", [128, 4096], BF16) for i in range(NW)]
        xnT, t_xnT = sb("xnT", [128, KC, 512], BF16)
        xall, _ = sb("xall", [128, 4096])
        t_xq = [Tk(f"xq{i}") for i in range(4)]
        scb, t_scb = sb("scb", [128, max(T, 2048)])
        kst, t_kst = sb("kst", [128, 4096], BF16)
        vst, t_vst = sb("vst", [128, 4, 1024], BF16)
        kiT, t_kiT = sb("kiT", [128, T], BF16)
        qiT, t_qiT = sb("qiT", [128, 2, 512], BF16)
        qh = [sb(f"qh{i}", [128, 512], BF16) for i in range(2)]
        sgh = [sb(f"sgh{i}", [128, 512], BF16) for i in range(2)]
        NR = 4
        kring = [sb(f"kr{i}", [128, 1024], BF16) for i in range(NR)]
        vring = [sb(f"vr{i}", [128, 8, 128], BF16) for i in range(NR)]
        pT = [sb(f"pT{i}", [128, 512], BF16) for i in range(3)]
        yT = [sb(f"yT{i}", [128, 8, 512], BF16) for i in range(4)]
        MBW = max(2 * T, 8192)
        MBH = MBW // 2
        mball, _ = sb("mball", [128, MBW], BF16)
        t_mb = [Tk("mb0"), Tk("mb1")]
        Fs = [sb(f"F{i}", [128, 514]) for i in range(5)]
        sgt = [sb(f"sgt{i}", [128, 512], BF16) for i in range(2)]
        mkT, t_mkT = sb("mkT", [128, 8, 256], BF16)
        mv, t_mv = sb("mv", [128, 2, 1024], BF16)
        ident, t_ident = sb("ident", [128, 128], BF16)
        tri, t_tri = sb("tri", [128, 128], BF16)
        triq, t_triq = sb("triq", [128, 128])
        ones, t_ones = sb("ones", [128, 128], BF16)
        eone, t_eone = sb("eone", [16, 2048], BF16)
        pw2, t_pw2 = sb("pw2", [128, NIT + 2])
        sel, t_sel = sb("sel", [128, 16], BF16)
        iwT, t_iwT = sb("iwT", [128, 512], BF16)
        km32, t_km32 = sb("km32", [128, 8, 16])
        kmT, t_kmT = sb("kmT", [128, 8, 16], BF16)
        bsm, t_bsm = sb("bsm", [128, 4, 16])
        m8, t_m8 = sb("m8", [128, 4, 8])
        mbf, t_mbf = sb("mbf", [128, 4, 16], BF16)
        mbT, t_mbT = sb("mbT", [16, 512], BF16)
        wI, t_wI = sb("wI", [128, 4, 4])
        st, t_st = sb("stat", [128, 8])
        epsb, t_eps = sb("epsb", [128, 1])
        bis, t_bis = sb("bis", [128, 8])
        halves, t_halves = sb("halves", [128, NIT + 2])
        nm, t_nm = sb("nm", [128, NIT + 1])
        ssum, t_ssum = sb("ssum", [128, NIT])
        dtmp, t_dtmp = sb("dtmp", [128, NIT])
        cneg, t_cneg = sb("cneg", [128, 1])
        nhh, t_nhh = sb("nhh", [128, NIT + 2])
        cw, t_cw = sb("cw", [128, 8, 3])
        cbias, t_cbias = sb("cbias", [128, 8])
        carry, t_carry = sb("carry", [128, 8, 2])
        sgc, t_sgc = sb("sgc", [128, 512], BF16)

        pj = [psb(f"pj{i}", [128, 512]) for i in range(3)]
        ptr, t_ptr = psb("ptr", [128, 1024], BF16)
        stp = [psb(f"stp{i}", [128, 512]) for i in range(2)]
        ot, t_ot = psb("ot", [128, 512])
        dn, t_dn = psb("dn", [128, 512])
        pjc = [0]

        def next_pj():
            pjc[0] += 1
            return pj[pjc[0] % 3]

        S.dma("pool", ident[:], ident_d, t_ident, writes=[t_ident])
        S.dma("pool", tri[:], tri_d, t_tri, writes=[t_tri])
        S.dma("pool", eone[:], eone_d, t_eone, writes=[t_eone])
        S.dma("sp", triq[:], triq_d, t_triq, writes=[t_triq])
        S.dma("sp", pw2[:], pw2_d, t_pw2, writes=[t_pw2])
        S.dma("pool", sel[:], sel_d, t_sel, writes=[t_sel])
        S.op("dve", lambda e: e.memset(ones[:], 1.0), writes=[t_ones])
        S.op("dve", lambda e: e.memset(epsb[:], EPS), writes=[t_eps])

        wcnt = [0]

        def wload(parts):
            w_sb, w_tk = wt[wcnt[0] % NW]
            wcnt[0] += 1
            for off, nk, ncol, src in parts:
                dst = w_sb[:, off:off + nk * ncol].rearrange("p (k n) -> p k n", k=nk)
                if isinstance(src, list):
                    for lo, sap in src:
                        S.dma("pool", dst[:, :, lo:lo + sap.shape[2]], sap, w_tk, writes=[w_tk])
                else:
                    S.dma("pool", dst, src, w_tk, writes=[w_tk])
            return w_sb, w_tk

        def wview(w_sb, off, nk, ncol):
            return w_sb[:, off:off + nk * ncol].rearrange("p (k n) -> p k n", k=nk)

        def win_cols(l, c0, n, reps=1, stride=0):
            base = win_d[l].rearrange("(k p) n -> p k n", p=128)
            if reps == 1:
                return base[:, :, c0:c0 + n]
            return [(r * n, base[:, :, c0 + r * stride:c0 + r * stride + n]) for r in range(reps)]

        def rmsnorm_rstd(x_ap, x_tks, junk_ap, junk_tks, col):
            S.op("act", lambda e: e.activation(out=junk_ap, in_=x_ap, func=AF.Square, accum_out=st[:, col:col + 1]),
                 reads=x_tks, writes=junk_tks + [t_st])
            S.op("act", lambda e: e.activation(out=st[:, col:col + 1], in_=st[:, col:col + 1], func=AF.Sqrt,
                                               scale=1.0 / D, bias=epsb[:, 0:1]), reads=[t_st, t_eps], writes=[t_st])
            S.op("dve", lambda e: e.reciprocal(st[:, col:col + 1], st[:, col:col + 1]), reads=[t_st], writes=[t_st])

        evc = [0]

        def evac_copy(out_ap, out_tks, in_ap, in_tks):
            evc[0] += 1
            if evc[0] % 2 == 0:
                S.op("act", lambda e: e.activation(out=out_ap, in_=in_ap, func=AF.Copy), reads=in_tks, writes=out_tks)
            else:
                S.op("dve", lambda e: e.tensor_copy(out_ap, in_ap), reads=in_tks, writes=out_tks)

        def norm_T(x_src_ap, g_tk_ready, ncols_tok, tok_off):
            pass

        def fm_chunk(w_view, w_tk, kn, rhs_fn, rhs_tks, ncol_tok):
            ps, tps = next_pj()
            for k in range(kn):
                S.op("pe", lambda e, k=k: e.matmul(ps[:, 0:ncol_tok], w_view[:, k, :], rhs_fn(k),
                                                   start=(k == 0), stop=(k == kn - 1)),
                     reads=[w_tk] + rhs_tks, writes=[tps])
            return ps, tps

        for l in range(L):
            xsrc = x_d if l == 0 else xmid_d
            xreg = [Tk(f"xreg{l}_{g}") for g in range(NG)]
            if l > 0:
                for g in range(NG):
                    xreg[g].w = xreg_prev[g].w
            kreg = {t: [Tk(f"kreg{t}{l}_{g}") for g in range(NG)] for t in "as"}
            vreg = {t: [Tk(f"vreg{t}{l}_{g}") for g in range(NG)] for t in "as"}
            if l > 0:
                for t in "as":
                    for g in range(NG):
                        kreg[t][g].r = list(kreg_prev[t][g].r)
                        kreg[t][g].w = kreg_prev[t][g].w
                        vreg[t][g].r = list(vreg_prev[t][g].r)
                        vreg[t][g].w = vreg_prev[t][g].w

            for k3 in range(3):
                S.dma("sp", cw[:, :, k3], cw_d[l, k3].rearrange("(c p) -> p c", p=128), t_cw, writes=[t_cw],
                      allow_slow_non_contiguous=True)
            S.dma("sp", cbias[:], cb_d[l].rearrange("(c p) -> p c", p=128), t_cbias, writes=[t_cbias],
                  allow_slow_non_contiguous=True)
            S.op("dve", lambda e: e.memset(carry[:], 0.0), writes=[t_carry])
            S.op("dve", lambda e: e.memset(km32[:], 0.0), writes=[t_km32])
            S.op("dve", lambda e: e.memset(kmT[:], 0.0), writes=[t_kmT])

            if DBG == "consts":
                S.drain()
                return nc
            gbc = scb[:, 0:2048]
            S.dma("sp", gbc, mg_d[l:l + 1, :].partition_broadcast(128), t_scb, writes=[t_scb])
            for mt in range(2):
                xt = xall[:, 0:2048]
                S.dma("sp", xt, mem_d[mt * 128:(mt + 1) * 128, :], t_xq[0], writes=[t_xq[0], t_xq[1]])
                xs = kst[:, 0:2048]
                rmsnorm_rstd(xt, [t_xq[0], t_xq[1]], xs, [t_kst], mt)
                S.op("dve", lambda e, mt=mt: e.scalar_tensor_tensor(out=xs, in0=xt, scalar=st[:, mt:mt + 1], in1=gbc,
                                                                    op0=ALU.mult, op1=ALU.mult),
                     reads=[t_xq[0], t_xq[1], t_st, t_scb], writes=[t_kst])
                for k4 in range(4):
                    for kk in range(4):
                        k = k4 * 4 + kk
                        S.op("pe", lambda e, k=k, kk=kk: e.transpose(ptr[:, kk * 128:(kk + 1) * 128],
                                                                     xs[:, k * 128:(k + 1) * 128], ident[:]),
                             reads=[t_kst, t_ident], writes=[t_ptr])
                    evac_copy(xnT[:, k4 * 4:(k4 + 1) * 4, mt * 128:(mt + 1) * 128],
                              [t_xnT], ptr[:, 0:512].rearrange("p (a b) -> p a b", a=4), [t_ptr])
            wkv = wkv_d[l].rearrange("(k p) n -> p k n", p=128)
            for c in range(4):
                w_sb, w_tk = wload([(0, KC, 256, wkv[:, :, c * 256:(c + 1) * 256])])
                wv = wview(w_sb, 0, KC, 256)
                for cc in range(2):
                    ps, tps = fm_chunk(wv[:, :, cc * 128:(cc + 1) * 128], w_tk, KC,
                                       lambda k: xnT[:, k, 0:256], [t_xnT], 256)
                    evac_copy(mkT[:, c * 2 + cc, :], [t_mkT], ps[:, 0:256], [tps])
            for c in range(4):
                w_sb, w_tk = wload([(0, KC, 256, wkv[:, :, 1024 + c * 256:1024 + (c + 1) * 256])])
                wv = wview(w_sb, 0, KC, 256)
                for mt in range(2):
                    ps, tps = next_pj()
                    for k in range(KC):
                        S.op("pe", lambda e, k=k, mt=mt: e.matmul(ps[:, 0:256], xnT[:, k, mt * 128:(mt + 1) * 128],
                                                                  wv[:, k, :], start=(k == 0), stop=(k == KC - 1)),
                             reads=[t_xnT, w_tk], writes=[tps])
                    evac_copy(mv[:, mt, c * 256:(c + 1) * 256], [t_mv], ps[:, 0:256], [tps])

            if DBG == "memkv":
                S.drain()
                return nc
            for g in range(NG):
                tok0 = g * 512
                S.dma("sp", gbc, lng_d[l:l + 1, :].partition_broadcast(128), t_scb, writes=[t_scb])
                for t in range(4):
                    hx = t % 2
                    xt = xall[:, hx * 2048:(hx + 1) * 2048]
                    xtk = [t_xq[2 * hx], t_xq[2 * hx + 1]]
                    S.dma("sp", xt, xsrc[tok0 + t * 128: tok0 + (t + 1) * 128, :], xtk[0], reads=[xreg[g]], writes=xtk)
                    xs = kst[:, 0:2048]
                    rmsnorm_rstd(xt, xtk, xs, [t_kst], t)
                    S.op("dve", lambda e, t=t, xt=xt: e.scalar_tensor_tensor(out=xs, in0=xt, scalar=st[:, t:t + 1],
                                                                             in1=gbc, op0=ALU.mult, op1=ALU.mult),
                         reads=xtk + [t_st, t_scb], writes=[t_kst])
                    for k4 in range(4):
                        for kk in range(4):
                            k = k4 * 4 + kk
                            S.op("pe", lambda e, k=k, kk=kk: e.transpose(ptr[:, kk * 128:(kk + 1) * 128],
                                                                         xs[:, k * 128:(k + 1) * 128], ident[:]),
                                 reads=[t_kst, t_ident], writes=[t_ptr])
                        evac_copy(xnT[:, k4 * 4:(k4 + 1) * 4, t * 128:(t + 1) * 128],
                                  [t_xnT], ptr[:, 0:512].rearrange("p (a b) -> p a b", a=4), [t_ptr])

                if DBG == "stage0":
                    S.drain()
                    return nc
                for typ, ck, cv in (("a", C_AK, C_AV), ("s", C_SK, C_SV)):
                    kst3 = kst[:].rearrange("p (h t) -> p h t", h=8)
                    for c in range(4):
                        w_sb, w_tk = wload([(0, KC, 256, win_cols(l, ck + c * 256, 256))])
                        wv = wview(w_sb, 0, KC, 256)
                        for cc in range(2):
                            h = c * 2 + cc
                            ps, tps = fm_chunk(wv[:, :, cc * 128:(cc + 1) * 128], w_tk, KC,
                                               lambda k: xnT[:, k, :], [t_xnT], 512)
                            S.op("act", lambda e, h=h, ps=ps: e.activation(out=kst3[:, h, :], in_=ps[:], func=AF.Copy),
                                 reads=[tps], writes=[t_kst])
                            if typ == "a":
                                S.op("dve", lambda e, h=h: e.tensor_reduce(
                                    out=km32[:, h, 2 * g:2 * g + 2], in_=kst3[:, h, :].rearrange("p (b t) -> p b t", b=2),
                                    axis=AX.X, op=ALU.add), reads=[t_kst], writes=[t_km32])
                    S.dma("sp", kT_d[typ][:, :, tok0:tok0 + 512].rearrange("h d t -> d h t"), kst3, t_kst,
                          reads=[t_kst], writes=[kreg[typ][g]])
                    if DBG == "kvA":
                        S.drain()
                        return nc
                    if typ == "a":
                        S.op("dve", lambda e: e.tensor_scalar(kmT[:, :, 2 * g:2 * g + 2], km32[:, :, 2 * g:2 * g + 2],
                                                              1.0 / 256.0, None, op0=ALU.mult),
                             reads=[t_km32], writes=[t_kmT])
                    for c in range(4):
                        w_sb, w_tk = wload([(0, KC, 256, win_cols(l, cv + c * 256, 256))])
                        wv = wview(w_sb, 0, KC, 256)
                        for t in range(4):
                            ps, tps = next_pj()
                            for k in range(KC):
                                S.op("pe", lambda e, k=k, t=t, ps=ps: e.matmul(
                                    ps[:, 0:256], xnT[:, k, t * 128:(t + 1) * 128], wv[:, k, :],
                                    start=(k == 0), stop=(k == KC - 1)), reads=[t_xnT, w_tk], writes=[tps])
                            evac_copy(vst[:, t, c * 256:(c + 1) * 256], [t_vst], ps[:, 0:256], [tps])
                    S.dma("sp", v_d[typ][tok0:tok0 + 512, :].rearrange("(t p) c -> p t c", p=128), vst[:], t_vst,
                          reads=[t_vst], writes=[vreg[typ][g]])
                if DBG == "kvC":
                    S.drain()
                    return nc
                w_sb, w_tk = wload([(0, KC, 256, win_cols(l, C_IQ, 256))])
                wv = wview(w_sb, 0, KC, 256)
                for cc in range(2):
                    ps, tps = fm_chunk(wv[:, :, cc * 128:(cc + 1) * 128], w_tk, KC, lambda k: xnT[:, k, :], [t_xnT], 512)
                    evac_copy(qiT[:, cc, :], [t_qiT], ps[:], [tps])
                w_sb, w_tk = wload([(0, KC, 256, win_cols(l, C_IK - 64, 256))])
                wv = wview(w_sb, 0, KC, 256)
                ps, tps = fm_chunk(wv[:, :, 64:192], w_tk, KC, lambda k: xnT[:, k, :], [t_xnT], 512)
                S.op("act", lambda e, ps=ps: e.activation(out=kiT[0:64, tok0:tok0 + 512], in_=ps[0:64, :], func=AF.Copy),
                     reads=[tps], writes=[t_kiT])
                S.op("act", lambda e, ps=ps: e.activation(out=iwT[64:96, :], in_=ps[64:96, :], func=AF.Copy),
                     reads=[tps], writes=[t_iwT])
                ps, tps = fm_chunk(wv[:, :, 0:128], w_tk, KC, lambda k: xnT[:, k, :], [t_xnT], 512)
                S.op("act", lambda e, ps=ps: e.activation(out=kiT[64:128, tok0:tok0 + 512], in_=ps[64:128, :], func=AF.Copy),
                     reads=[tps], writes=[t_kiT])
                ps, tps = next_pj()
                for t in range(4):
                    S.op("pe", lambda e, t=t, ps=ps: e.matmul(ps[:, t * 16:(t + 1) * 16], iwT[64:96, t * 128:(t + 1) * 128],
                                                             sel[64:96, :], start=True, stop=True),
                         reads=[t_iwT, t_sel], writes=[tps])
                S.op("dve", lambda e, ps=ps: e.tensor_scalar(wI[:], ps[:, 0:64].rearrange("p (a b) -> p a b", a=4)[:, :, 0:4],
                                                             (64.0 ** -0.5) * 0.5, None, op0=ALU.mult),
                     reads=[tps], writes=[t_wI])
                if DBG == "kv":
                    S.drain()
                    return nc
                def kv_loader(typ, h, nkt, lookahead=2):
                    nch = (nkt + 7) // 8
                    state = {"issued": 0}

                    def issue(c):
                        slot = (kvc[0] + c) % NR
                        k_sb, k_tk = kring[slot]
                        v_sb, v_tk = vring[slot]
                        k0 = c * 1024
                        n = min(1024, nkt * 128 - k0)
                        gs = sorted(set((k0 + i * 128) // 512 for i in range(n // 128)))
                        S.dma("sp", k_sb[:, 0:n], kT_d[typ][h, :, k0:k0 + n], k_tk,
                              reads=[kreg[typ][gg] for gg in gs], writes=[k_tk])
                        S.dma("sp", v_sb[:, 0:n // 128, :],
                              v_d[typ][k0:k0 + n, h * 128:(h + 1) * 128].rearrange("(a p) d -> p a d", p=128), v_tk,
                              reads=[vreg[typ][gg] for gg in gs], writes=[v_tk])

                    def get(kt):
                        c = kt // 8
                        while state["issued"] < min(nch, c + 1 + lookahead):
                            issue(state["issued"])
                            state["issued"] += 1
                        slot = (kvc[0] + c) % NR
                        k_sb, k_tk = kring[slot]
                        v_sb, v_tk = vring[slot]
                        j = kt % 8
                        return k_sb[:, j * 128:(j + 1) * 128], k_tk, v_sb[:, j, :], v_tk

                    def done():
                        kvc[0] += nch
                    return get, done

                def attention(nkt, qcols, q_ap, q_tk, get_kv, mask_fn, scale):
                    pend = []
                    for kt in range(nkt + 1):
                        if kt < nkt:
                            c0, masks = mask_fn(kt)
                            sp_, tsp = stp[kt % 2]
                            k_ap, k_tk, v_ap, v_tk = get_kv(kt)
                            nm_ = len(masks)
                            S.op("pe", lambda e, sp_=sp_, k_ap=k_ap, c0=c0, nm_=nm_: e.matmul(
                                sp_[:, c0:qcols], k_ap, q_ap[:, c0:qcols], start=True, stop=(nm_ == 0)),
                                reads=[k_tk, q_tk], writes=[tsp])
                            for i, (ml, mr, lo, hi, mtks) in enumerate(masks):
                                S.op("pe", lambda e, sp_=sp_, ml=ml, mr=mr, lo=lo, hi=hi, i=i, nm_=nm_: e.matmul(
                                    sp_[:, lo:hi], ml, mr, start=False, stop=(i == nm_ - 1)),
                                    reads=mtks, writes=[tsp])
                            pend.append((kt, c0, sp_, tsp, v_ap, v_tk))
                        if kt > 0:
                            pk, c0, sp_, tsp, v_ap, v_tk = pend.pop(0)
                            p_sb, p_tk = pT[pk % 3]
                            S.op("act", lambda e, p_sb=p_sb, sp_=sp_, c0=c0: e.activation(
                                out=p_sb[:, c0:qcols], in_=sp_[:, c0:qcols], func=AF.Exp, scale=scale),
                                reads=[tsp], writes=[p_tk])
                            S.op("pe", lambda e, v_ap=v_ap, p_sb=p_sb, c0=c0, pk=pk: e.matmul(
                                ot[:, c0:qcols], v_ap, p_sb[:, c0:qcols], start=(pk == 0), stop=(pk == nkt - 1)),
                                reads=[v_tk, p_tk], writes=[t_ot])
                            S.op("pe", lambda e, p_sb=p_sb, c0=c0, pk=pk: e.matmul(
                                dn[:, c0:qcols], ones[:], p_sb[:, c0:qcols], start=(pk == 0), stop=(pk == nkt - 1)),
                                reads=[t_ones, p_tk], writes=[t_dn])

                def normalize(o_ap, o_tk, qcols, sg_ap, sg_tk, y_ap, y_tk):
                    rden, t_rden = Fs[3]
                    t1, t_t1 = Fs[4]
                    S.op("dve", lambda e: e.reciprocal(rden[:, 0:qcols], dn[:, 0:qcols]), reads=[t_dn], writes=[t_rden])
                    S.op("dve", lambda e: e.tensor_tensor(out=t1[:, 0:qcols], in0=o_ap, in1=rden[:, 0:qcols], op=ALU.mult),
                         reads=[o_tk, t_rden], writes=[t_t1])
                    S.op("dve", lambda e: e.tensor_tensor(out=y_ap, in0=t1[:, 0:qcols], in1=sg_ap, op=ALU.mult),
                         reads=[t_t1, sg_tk], writes=[y_tk])

                for h in range(8):
                    q_sb, q_tk = qh[h % 2]
                    g_sb, g_tk = sgh[h % 2]
                    w_sb, w_tk = wload([(0, KC, 256, win_cols(l, C_AQ + h * 128, 128, reps=2, stride=C_AG - C_AQ))])
                    wv = wview(w_sb, 0, KC, 256)
                    ps, tps = fm_chunk(wv[:, :, 0:128], w_tk, KC, lambda k: xnT[:, k, :], [t_xnT], 512)
                    S.op("act", lambda e, ps=ps, q_sb=q_sb: e.activation(out=q_sb[:], in_=ps[:], func=AF.Copy),
                         reads=[tps], writes=[q_tk])
                    ps, tps = fm_chunk(wv[:, :, 128:256], w_tk, KC, lambda k: xnT[:, k, :], [t_xnT], 512)
                    S.op("act", lambda e, ps=ps, g_sb=g_sb: e.activation(out=g_sb[:], in_=ps[:], func=AF.Silu),
                         reads=[tps], writes=[g_tk])
                    ps, tps = next_pj()
                    for t in range(4):
                        S.op("pe", lambda e, t=t, ps=ps, q_sb=q_sb: e.matmul(
                            ps[:, t * 16:(t + 1) * 16], q_sb[:, t * 128:(t + 1) * 128], kmT[:, h, :], start=True, stop=True),
                            reads=[q_tk, t_kmT], writes=[tps])
                    S.op("dve", lambda e, ps=ps: e.tensor_copy(bsm[:].rearrange("p a b -> p (a b)"), ps[:, 0:64]),
                         reads=[tps], writes=[t_bsm])
                    S.op("dve", lambda e: e.memset(bsm[:, 0:2, 2 * g:16], -BIG), writes=[t_bsm])
                    S.op("dve", lambda e: e.memset(bsm[:, 2:4, 2 * g + 1:16], -BIG), writes=[t_bsm])
                    for t in range(4):
                        S.op("dve", lambda e, t=t: e.max(out=m8[:, t, :], in_=bsm[:, t, :]), reads=[t_bsm], writes=[t_m8])
                    for t in range(4):
                        S.op("dve", lambda e, t=t: e.tensor_scalar(mbf[:, t, :], bsm[:, t, :], m8[:, t, 2:3], NEG,
                                                                  op0=ALU.is_lt, op1=ALU.mult),
                             reads=[t_bsm, t_m8], writes=[t_mbf])
                    for t in range(4):
                        S.op("pe", lambda e, t=t: e.transpose(ptr[0:16, t * 128:(t + 1) * 128], mbf[:, t, :], ident[:]),
                             reads=[t_mbf, t_ident], writes=[t_ptr])
                    S.op("dve", lambda e: e.tensor_copy(mbT[:, :], ptr[0:16, 0:512]), reads=[t_ptr], writes=[t_mbT])

                    def moba_mask(kt):
                        a = kt - 4 * g
                        c0 = 128 * max(a, 0)
                        i = kt // 2
                        masks = []
                        if i < 2 * g:
                            masks.append((eone[:, i * 128:(i + 1) * 128], mbT[:, 0:512], 0, 512, [t_eone, t_mbT]))
                        elif i == 2 * g:
                            masks.append((eone[:, i * 128:(i + 1) * 128], mbT[:, 256:512], 256, 512, [t_eone, t_mbT]))
                            masks.append((ident[:], tri[:], a * 128, (a + 1) * 128, [t_ident, t_tri]))
                        else:
                            masks.append((ident[:], tri[:], a * 128, (a + 1) * 128, [t_ident, t_tri]))
                        return c0, masks

                    get_kv, kv_done = kv_loader("a", h, 4 * g + 4)
                    attention(4 * g + 4, 512, q_sb, q_tk, get_kv, moba_mask, 128.0 ** -0.5)
                    kv_done()
                    normalize(ot[:, 0:512], t_ot, 512, g_sb[:], g_tk, yT[0][0][:, h, :], yT[0][1])

                if DBG == "moba":
                    S.drain()
                    return nc
                for j in range(8):
                    ccs, t_ccs = Fs[0]
                    u, t_u = Fs[1]
                    cacc, t_cacc = Fs[2]
                    w_sb, w_tk = wload([(0, KC, 256, win_cols(l, C_CC + j * 128, 128, reps=2, stride=C_CH - C_CC))])
                    wv = wview(w_sb, 0, KC, 256)
                    ps, tps = fm_chunk(wv[:, :, 0:128], w_tk, KC, lambda k: xnT[:, k, :], [t_xnT], 512)
                    S.op("act", lambda e, ps=ps: e.activation(out=ccs[:, 0:512], in_=ps[:], func=AF.Copy),
                         reads=[tps], writes=[t_ccs])
                    ps, tps = fm_chunk(wv[:, :, 128:256], w_tk, KC, lambda k: xnT[:, k, :], [t_xnT], 512)
                    S.op("dve", lambda e, ps=ps: e.tensor_tensor(out=u[:, 2:514], in0=ps[:], in1=ccs[:, 0:512], op=ALU.mult),
                         reads=[tps, t_ccs], writes=[t_u])
                    S.op("dve", lambda e, j=j: e.tensor_copy(u[:, 0:2], carry[:, j, :]), reads=[t_carry], writes=[t_u])
                    S.op("act", lambda e, j=j: e.activation(out=cacc[:, 0:512], in_=u[:, 2:514], func=AF.Identity,
                                                            scale=cw[:, j, 2:3], bias=cbias[:, j:j + 1]),
                         reads=[t_u, t_cw, t_cbias], writes=[t_cacc])
                    S.op("dve", lambda e, j=j: e.scalar_tensor_tensor(out=cacc[:, 0:512], in0=u[:, 1:513], scalar=cw[:, j, 1:2],
                                                                      in1=cacc[:, 0:512], op0=ALU.mult, op1=ALU.add),
                         reads=[t_u, t_cw, t_cacc], writes=[t_cacc])
                    S.op("dve", lambda e, j=j: e.scalar_tensor_tensor(out=cacc[:, 0:512], in0=u[:, 0:512], scalar=cw[:, j, 0:1],
                                                                      in1=cacc[:, 0:512], op0=ALU.mult, op1=ALU.add),
                         reads=[t_u, t_cw, t_cacc], writes=[t_cacc])
                    S.op("dve", lambda e, j=j: e.tensor_copy(carry[:, j, :], u[:, 512:514]), reads=[t_u], writes=[t_carry])
                    w_sb, w_tk = wload([(0, KC, 256, win_cols(l, C_CB + j * 128, 128, reps=2, stride=C_CG - C_CB))])
                    wv = wview(w_sb, 0, KC, 256)
                    ps, tps = fm_chunk(wv[:, :, 128:256], w_tk, KC, lambda k: xnT[:, k, :], [t_xnT], 512)
                    S.op("act", lambda e, ps=ps: e.activation(out=sgc[:], in_=ps[:], func=AF.Silu), reads=[tps], writes=[t_sgc])
                    ps, tps = fm_chunk(wv[:, :, 0:128], w_tk, KC, lambda k: xnT[:, k, :], [t_xnT], 512)
                    S.op("dve", lambda e, ps=ps: e.tensor_tensor(out=cacc[:, 0:512], in0=ps[:], in1=cacc[:, 0:512], op=ALU.mult),
                         reads=[tps, t_cacc], writes=[t_cacc])
                    S.op("dve", lambda e, j=j: e.tensor_tensor(out=yT[1][0][:, j, :], in0=cacc[:, 0:512], in1=sgc[:], op=ALU.mult),
                         reads=[t_cacc, t_sgc], writes=[yT[1][1]])

                if DBG == "conv":
                    S.drain()
                    return nc
                for hh in range(2):
                    for tt in range(2):
                        t = hh * 2 + tt
                        Q = 4 * g + t
                        nk = (Q + 1) * 128
                        mb_ap = mball[:, tt * MBH: tt * MBH + nk]
                        nchunk = (nk + 511) // 512
                        for c in range(nchunk):
                            wdt = min(512, nk - c * 512)
                            for j in range(4):
                                pb = 64 * (j % 2)
                                ps, tps = next_pj()
                                rl, t_rl = Fs[j % 2]
                                S.op("pe", lambda e, ps=ps, pb=pb, j=j, c=c, wdt=wdt, t=t: e.matmul(
                                    ps[:, 0:wdt], qiT[pb:pb + 64, j // 2, t * 128:(t + 1) * 128],
                                    kiT[pb:pb + 64, c * 512:c * 512 + wdt], start=True, stop=True),
                                    reads=[t_qiT, t_kiT], writes=[tps])
                                S.op("act", lambda e, ps=ps, rl=rl, wdt=wdt: e.activation(out=rl[:, 0:wdt], in_=ps[:, 0:wdt], func=AF.Relu),
                                     reads=[tps], writes=[t_rl])
                                if j == 0:
                                    S.op("dve", lambda e, rl=rl, c=c, wdt=wdt, t=t: e.tensor_scalar(
                                        scb[:, c * 512:c * 512 + wdt], rl[:, 0:wdt], wI[:, t, 0:1], None, op0=ALU.mult),
                                        reads=[t_rl, t_wI], writes=[t_scb])
                                else:
                                    S.op("dve", lambda e, rl=rl, c=c, wdt=wdt, t=t, j=j: e.scalar_tensor_tensor(
                                        out=scb[:, c * 512:c * 512 + wdt], in0=rl[:, 0:wdt], scalar=wI[:, t, j:j + 1],
                                        in1=scb[:, c * 512:c * 512 + wdt], op0=ALU.mult, op1=ALU.add),
                                        reads=[t_rl, t_wI, t_scb], writes=[t_scb])
                        S.op("dve", lambda e, nk=nk: e.tensor_reduce(out=bis[:, 0:1], in_=scb[:, 0:nk], axis=AX.X, op=ALU.min),
                             reads=[t_scb], writes=[t_bis])
                        S.op("dve", lambda e, nk=nk: e.tensor_reduce(out=bis[:, 1:2], in_=scb[:, 0:nk], axis=AX.X, op=ALU.max),
                             reads=[t_scb], writes=[t_bis])
                        S.op("dve", lambda e, Q=Q: e.tensor_tensor(out=scb[:, Q * 128:(Q + 1) * 128], in0=scb[:, Q * 128:(Q + 1) * 128],
                                                                   in1=triq[:], op=ALU.add), reads=[t_scb, t_triq], writes=[t_scb])
                        S.op("dve", lambda e: e.tensor_tensor(out=bis[:, 2:3], in0=bis[:, 1:2], in1=bis[:, 0:1], op=ALU.subtract),
                             reads=[t_bis], writes=[t_bis])
                        S.op("dve", lambda e: e.tensor_scalar(bis[:, 2:3], bis[:, 2:3], 1.0001, 1e-6, op0=ALU.mult, op1=ALU.add),
                             reads=[t_bis], writes=[t_bis])
                        S.op("dve", lambda e: e.tensor_scalar(halves[:], pw2[:], bis[:, 2:3], None, op0=ALU.mult),
                             reads=[t_pw2, t_bis], writes=[t_halves])
                        S.op("dve", lambda e: e.scalar_tensor_tensor(out=nm[:, 0:1], in0=bis[:, 0:1], scalar=-1.0, in1=halves[:, 0:1],
                                                                     op0=ALU.mult, op1=ALU.subtract),
                             reads=[t_bis, t_halves], writes=[t_nm])
                        S.op("dve", lambda e: e.memset(ssum[:], 0.0), writes=[t_ssum])
                        cthr = float(2 * min(TOPK, T // 4) - nk)
                        S.op("dve", lambda e: e.memset(cneg[:], 0.5 - cthr), writes=[t_cneg])
                        S.op("dve", lambda e: e.tensor_scalar(nhh[:], halves[:], -0.5, None, op0=ALU.mult),
                             reads=[t_halves], writes=[t_nhh])
                        for it in range(NIT):
                            S.op("act", lambda e, it=it, nk=nk: e.activation(out=kst[:, 0:nk], in_=scb[:, 0:nk], func=AF.Sign,
                                                                             bias=nm[:, it:it + 1], scale=1.0,
                                                                             accum_out=ssum[:, it:it + 1]),
                                 reads=[t_scb, t_nm], writes=[t_kst, t_ssum])
                            S.op("act", lambda e, it=it: e.activation(out=dtmp[:, it:it + 1], in_=ssum[:, it:it + 1], func=AF.Sign,
                                                                      bias=cneg[:, 0:1], scale=1.0),
                                 reads=[t_ssum, t_cneg], writes=[t_dtmp])
                            S.op("act", lambda e, it=it: e.activation(out=nm[:, it + 1:it + 2], in_=dtmp[:, it:it + 1], func=AF.Identity,
                                                                      scale=nhh[:, it:it + 1], bias=nm[:, it:it + 1]),
                                 reads=[t_dtmp, t_nhh, t_nm], writes=[t_nm])
                        S.op("dve", lambda e: e.scalar_tensor_tensor(out=bis[:, 3:4], in0=nm[:, NIT:NIT + 1], scalar=-1.0,
                                                                     in1=halves[:, NIT + 1:NIT + 2], op0=ALU.mult, op1=ALU.subtract),
                             reads=[t_nm, t_halves], writes=[t_bis])
                        S.op("dve", lambda e, mb_ap=mb_ap, nk=nk: e.tensor_scalar(mb_ap, scb[:, 0:nk], bis[:, 3:4], NEG,
                                                                                 op0=ALU.is_lt, op1=ALU.mult),
                             reads=[t_scb, t_bis], writes=[t_mb[tt]])
                    nkt = 4 * g + 2 * hh + 2
                    for h in range(8):
                        q_sb, q_tk = qh[h % 2]
                        g_sb, g_tk = sgh[h % 2]
                        w_sb, w_tk = wload([(0, KC, 256, win_cols(l, C_SQ + h * 128, 128, reps=2, stride=C_SG - C_SQ))])
                        wv = wview(w_sb, 0, KC, 256)
                        ps, tps = fm_chunk(wv[:, :, 0:128], w_tk, KC, lambda k: xnT[:, k, hh * 256:(hh + 1) * 256], [t_xnT], 256)
                        S.op("act", lambda e, ps=ps, q_sb=q_sb: e.activation(out=q_sb[:, 0:256], in_=ps[:, 0:256], func=AF.Copy),
                             reads=[tps], writes=[q_tk])
                        ps, tps = fm_chunk(wv[:, :, 128:256], w_tk, KC, lambda k: xnT[:, k, hh * 256:(hh + 1) * 256], [t_xnT], 256)
                        S.op("act", lambda e, ps=ps, g_sb=g_sb: e.activation(out=g_sb[:, 0:256], in_=ps[:, 0:256], func=AF.Silu),
                             reads=[tps], writes=[g_tk])

                        def dsa_mask(kt):
                            a = kt - (4 * g + 2 * hh)
                            c0 = 128 * max(a, 0)
                            masks = []
                            for tt in range(2):
                                if kt <= 4 * g + 2 * hh + tt:
                                    masks.append((mball[:, tt * MBH + kt * 128: tt * MBH + (kt + 1) * 128], ident[:],
                                                  tt * 128, (tt + 1) * 128, [t_mb[tt], t_ident]))
                            return c0, masks

                        get_kv, kv_done = kv_loader("s", h, nkt)
                        attention(nkt, 256, q_sb, q_tk, get_kv, dsa_mask, 128.0 ** -0.5)
                        kv_done()
                        normalize(ot[:, 0:256], t_ot, 256, g_sb[:, 0:256], g_tk,
                                  yT[2][0][:, h, hh * 256:(hh + 1) * 256], yT[2][1])

                if DBG == "dsa":
                    S.drain()
                    return nc
                for h in range(4):
                    for dc in range(2):
                        q_sb, q_tk = qh[dc]
                        g_sb, g_tk = sgh[dc]
                        cidx = h * 2 + dc
                        w_sb, w_tk = wload([(0, KC, 256, win_cols(l, C_MQ + cidx * 128, 128, reps=2, stride=C_MG - C_MQ))])
                        wv = wview(w_sb, 0, KC, 256)
                        ps, tps = fm_chunk(wv[:, :, 0:128], w_tk, KC, lambda k: xnT[:, k, :], [t_xnT], 512)
                        S.op("act", lambda e, ps=ps, q_sb=q_sb: e.activation(out=q_sb[:], in_=ps[:], func=AF.Copy),
                             reads=[tps], writes=[q_tk])
                        ps, tps = fm_chunk(wv[:, :, 128:256], w_tk, KC, lambda k: xnT[:, k, :], [t_xnT], 512)
                        S.op("act", lambda e, ps=ps, g_sb=g_sb: e.activation(out=g_sb[:], in_=ps[:], func=AF.Silu),
                             reads=[tps], writes=[g_tk])
                    pts = []
                    for mt in range(2):
                        sp_, tsp = stp[mt]
                        for dc in range(2):
                            S.op("pe", lambda e, sp_=sp_, mt=mt, dc=dc: e.matmul(
                                sp_[:], mkT[:, h * 2 + dc, mt * 128:(mt + 1) * 128], qh[dc][0][:], start=(dc == 0), stop=(dc == 1)),
                                reads=[t_mkT, qh[dc][1]], writes=[tsp])
                        p_sb, p_tk = pT[mt]
                        S.op("act", lambda e, sp_=sp_, p_sb=p_sb: e.activation(out=p_sb[:], in_=sp_[:], func=AF.Exp, scale=256.0 ** -0.5),
                             reads=[tsp], writes=[p_tk])
                        pts.append((p_sb, p_tk))
                    o2, t_o2 = pj[0]
                    for mt in range(2):
                        p_sb, p_tk = pts[mt]
                        S.op("pe", lambda e, mt=mt, p_sb=p_sb: e.matmul(ot[:], mv[:, mt, h * 256:h * 256 + 128], p_sb[:],
                                                                        start=(mt == 0), stop=(mt == 1)),
                             reads=[t_mv, p_tk], writes=[t_ot])
                        S.op("pe", lambda e, mt=mt, p_sb=p_sb: e.matmul(o2[:], mv[:, mt, h * 256 + 128:h * 256 + 256], p_sb[:],
                                                                        start=(mt == 0), stop=(mt == 1)),
                             reads=[t_mv, p_tk], writes=[t_o2])
                        S.op("pe", lambda e, mt=mt, p_sb=p_sb: e.matmul(dn[:], ones[:], p_sb[:], start=(mt == 0), stop=(mt == 1)),
                             reads=[t_ones, p_tk], writes=[t_dn])
                    normalize(ot[:], t_ot, 512, sgh[0][0][:], sgh[0][1], yT[3][0][:, h * 2, :], yT[3][1])
                    normalize(o2[:], t_o2, 512, sgh[1][0][:], sgh[1][1], yT[3][0][:, h * 2 + 1, :], yT[3][1])

                if DBG == "mem":
                    S.drain()
                    return nc
                mrg = mball[:, 0:8192].rearrange("p (c t) -> p c t", c=16)
                t_mrg = t_mb
                for oc in range(16):
                    acc, t_acc = Fs[oc % 2]
                    for br in range(4):
                        tmp, t_tmp = Fs[2 + (br % 2)]
                        sg_sb, sg_tk = sgt[br % 2]
                        w_sb, w_tk = wload([(0, KC, 128, win_cols(l, C_R + 2048 * br + 128 * oc, 128)),
                                            (KC * 128, 8, 128,
                                             wbr_d[l, br].rearrange("(k p) n -> p k n", p=128)[:, :, oc * 128:(oc + 1) * 128])])
                        wr = wview(w_sb, 0, KC, 128)
                        wb = wview(w_sb, KC * 128, 8, 128)
                        ps, tps = fm_chunk(wr, w_tk, KC, lambda k: xnT[:, k, :], [t_xnT], 512)
                        S.op("act", lambda e, ps=ps, sg_sb=sg_sb: e.activation(out=sg_sb[:], in_=ps[:], func=AF.Sigmoid),
                             reads=[tps], writes=[sg_tk])
                        y_sb, y_tk = yT[br]
                        ps, tps = fm_chunk(wb, w_tk, 8, lambda k, y_sb=y_sb: y_sb[:, k, :], [y_tk], 512)
                        if br == 0:
                            S.op("dve", lambda e, ps=ps, sg_sb=sg_sb, acc=acc: e.tensor_tensor(
                                out=acc[:, 0:512], in0=ps[:], in1=sg_sb[:], op=ALU.mult), reads=[tps, sg_tk], writes=[t_acc])
                        else:
                            S.op("dve", lambda e, ps=ps, sg_sb=sg_sb, tmp=tmp: e.tensor_tensor(
                                out=tmp[:, 0:512], in0=ps[:], in1=sg_sb[:], op=ALU.mult), reads=[tps, sg_tk], writes=[t_tmp])
                            if br < 3:
                                S.op("dve", lambda e, tmp=tmp, acc=acc: e.tensor_tensor(
                                    out=acc[:, 0:512], in0=acc[:, 0:512], in1=tmp[:, 0:512], op=ALU.add),
                                    reads=[t_acc, t_tmp], writes=[t_acc])
                            else:
                                S.op("dve", lambda e, tmp=tmp, acc=acc, oc=oc: e.tensor_tensor(
                                    out=mrg[:, oc, :], in0=acc[:, 0:512], in1=tmp[:, 0:512], op=ALU.add),
                                    reads=[t_acc, t_tmp], writes=[t_mb[0], t_mb[1]])

                if DBG == "merge":
                    S.drain()
                    return nc
                wo = wout_d[l].rearrange("(k p) n -> p k n", p=128)
                for cb in range(8):
                    w_sb, w_tk = wload([(0, KC, 256, wo[:, :, cb * 256:(cb + 1) * 256])])
                    wv = wview(w_sb, 0, KC, 256)
                    xr = xall[:, (cb % 2) * 1024:(cb % 2 + 1) * 1024].rearrange("p (t c) -> p t c", t=4)
                    t_xr = t_xq[cb % 2]
                    xo = xall[:, 2048 + (cb % 2) * 1024: 2048 + (cb % 2 + 1) * 1024].rearrange("p (t c) -> p t c", t=4)
                    t_xo = t_xq[2 + cb % 2]
                    S.dma("sp", xr, xsrc[tok0:tok0 + 512, cb * 256:(cb + 1) * 256].rearrange("(t p) c -> p t c", p=128),
                          t_xr, reads=[xreg[g]], writes=[t_xr])
                    for t in range(4):
                        ps, tps = next_pj()
                        for k in range(KC):
                            S.op("pe", lambda e, k=k, t=t, ps=ps: e.matmul(
                                ps[:, 0:256], mrg[:, k, t * 128:(t + 1) * 128], wv[:, k, :], start=(k == 0), stop=(k == KC - 1)),
                                reads=[t_mb[0], t_mb[1], w_tk], writes=[tps])
                        S.op("dve", lambda e, t=t, ps=ps, xo=xo, xr=xr: e.tensor_tensor(
                            out=xo[:, t, :], in0=ps[:, 0:256], in1=xr[:, t, :], op=ALU.add),
                            reads=[tps, t_xr], writes=[t_xo])
                    S.dma("sp", xmid_d[tok0:tok0 + 512, cb * 256:(cb + 1) * 256].rearrange("(t p) c -> p t c", p=128), xo,
                          t_xo, reads=[t_xo], writes=[xreg[g]] if l > 0 or True else [])
            xreg_prev = xreg
            kreg_prev = kreg
            vreg_prev = vreg

        gbc = scb[:, 0:2048]
        S.dma("sp", gbc, fg_d.partition_broadcast(128), t_scb, writes=[t_scb])
        for tt in range(NT):
            hx = tt % 2
            xt = xall[:, hx * 2048:(hx + 1) * 2048]
            xtk = [t_xq[2 * hx], t_xq[2 * hx + 1]]
            S.dma("sp", xt, xmid_d[tt * 128:(tt + 1) * 128, :], xtk[0], reads=[xreg_prev[tt // 4]], writes=xtk)
            rmsnorm_rstd(xt, xtk, kst[:, 0:2048], [t_kst], tt % 8)
            S.op("dve", lambda e, xt=xt, c=tt % 8: e.scalar_tensor_tensor(out=xt, in0=xt, scalar=st[:, c:c + 1], in1=gbc,
                                                                          op0=ALU.mult, op1=ALU.mult),
                 reads=xtk + [t_st, t_scb], writes=xtk)
            S.dma("sp", y_d[tt * 128:(tt + 1) * 128, :], xt, xtk[1], reads=xtk, writes=[])
        S.final_wait("sp", t_xq)
        S.drain()
    return nc


kvc = [0]
DBG = None


def _consts():
    k = np.arange(128)
    tri = np.where(k[None, :] >= k[:, None], 0.0, NEG).astype(np.float32)
    triq = np.where(k[None, :] <= k[:, None], 0.0, -BIG).astype(np.float32)
    eone = np.zeros((16, 2048), np.float32)
    for i in range(16):
        eone[i, i * 128:(i + 1) * 128] = 1.0
    pw2 = np.zeros((128, NIT + 2), np.float32)
    for i in range(NIT + 1):
        pw2[:, i] = 2.0 ** -(i + 1)
    pw2[:, NIT + 1] = 1.25 * 2.0 ** -(NIT + 1)
    selm = np.zeros((128, 16), np.float32)
    for j in range(4):
        selm[64 + j, j] = 1.0
    return {"c_sel": selm, "c_ident": np.eye(128, dtype=np.float32), "c_tri": tri, "c_triq": triq, "c_eone": eone, "c_pw2": pw2}


_CACHE = {}


def run(inputs, T, L, n_cores, batch_of_core):
    key = (T, L)
    if key not in _CACHE:
        kvc[0] = 0
        _CACHE[key] = build(T, L)
    nc = _CACHE[key]
    cst = _consts()
    f = lambda a: np.ascontiguousarray(np.asarray(a, dtype=np.float32))
    shared = {
        "ln_g": f(inputs["ln_g"]), "w_in": f(inputs["w_in"]), "conv_w": f(inputs["conv_w"]),
        "conv_b": f(inputs["conv_b"]), "mem_ln_g": f(inputs["mem_ln_g"]), "w_mem_kv": f(inputs["w_mem_kv"]),
        "w_branch": f(inputs["w_branch"]), "w_out": f(inputs["w_out"]),
        "final_g": f(inputs["final_g"]).reshape(1, D), **cst,
    }
    in_maps = []
    for c in range(n_cores):
        b = batch_of_core[c]
        m = dict(shared)
        m["x"] = f(inputs["x"][b])
        m["mem"] = f(inputs["mem"][b])
        in_maps.append(m)
    res = run_bass_kernel_spmd(nc, in_maps, core_ids=list(range(n_cores)))
    return [r["y"] for r in res.results]


def kernel(x, mem, ln_g, w_in, conv_w, conv_b, mem_ln_g, w_mem_kv, w_branch, w_out, final_g):
    inputs = dict(x=x, mem=mem, ln_g=ln_g, w_in=w_in, conv_w=conv_w, conv_b=conv_b, mem_ln_g=mem_ln_g,
                  w_mem_kv=w_mem_kv, w_branch=w_branch, w_out=w_out, final_g=final_g)
    B, T, _ = np.asarray(x).shape
    L = np.asarray(ln_g).shape[0]
    outs = run(inputs, T, L, 8, [c % B for c in range(8)])
    return np.stack([outs[b] for b in range(B)], axis=0).astype(np.float32)
```

```python
import os
import numpy as np
import concourse.bass as bass
import concourse.mybir as mybir
from concourse.bass_utils import run_bass_kernel_spmd
from contextlib import ExitStack

F32 = mybir.dt.float32
BF16 = mybir.dt.bfloat16
AF = mybir.ActivationFunctionType
ALU = mybir.AluOpType
AX = mybir.AxisListType

D = 2048
KC = 16
BW = 1024
INC = 22852
NEG = -30000.0
BIG = 1.0e30
EPS = 1e-6
C_AQ, C_AK, C_AV, C_AG = 0, 1024, 2048, 3072
C_CB, C_CC, C_CH, C_CG = 4096, 5120, 6144, 7168
C_SQ, C_SK, C_SV, C_SG = 8192, 9216, 10240, 11264
C_MQ, C_MG = 12288, 13312
C_IQ, C_IK, C_IW, C_R = 14336, 14592, 14656, 14660
NIT = 16
TOPK = 256


class Tk:
    __slots__ = ("name", "w", "r", "dsem", "dcnt")

    def __init__(self, name):
        self.name = name
        self.w = None
        self.r = []
        self.dsem = None
        self.dcnt = 0


class Sched:
    def __init__(self, nc, es):
        self.nc = nc
        self.es = es
        self.eng = {"pe": nc.tensor, "act": nc.scalar, "dve": nc.vector, "pool": nc.gpsimd, "sp": nc.sync}
        self.sem = {k: es.enter_context(nc.semaphore("sem_" + k)) for k in self.eng}
        self.cnt = {k: 0 for k in self.eng}
        self.waited = {k: {} for k in self.eng}
        self.ninst = 0
        self.owners = []

    def drain(self):
        for k in self.eng:
            if self.cnt[k] > 0 and k != "sp":
                self.eng["sp"].wait_ge(self.sem[k], self.cnt[k])
        for o in self.owners:
            self.eng["sp"].wait_ge(o.dsem, o.dcnt)

    def _deps(self, e, reads, writes):
        deps = {}

        def add(tk, war):
            if tk is None:
                return
            key, sh, val, te = tk
            if te == e and e == "pe":
                return
            if deps.get(key, (None, 0))[1] < val:
                deps[key] = (sh, val)

        for t in reads:
            add(t.w, False)
        for t in writes:
            add(t.w, False)
            for rt in t.r:
                add(rt, True)
        return deps

    def _wait(self, e, deps):
        for key, (sh, val) in deps.items():
            if self.waited[e].get(key, 0) >= val:
                continue
            self.eng[e].wait_ge(sh, val)
            self.waited[e][key] = val

    def _record(self, tk, reads, writes):
        for t in writes:
            t.w = tk
            t.r = []
        for t in reads:
            if t in writes:
                continue
            t.r = [x for x in t.r if x[0] != tk[0]] + [tk]

    def op(self, e, fn, reads=(), writes=()):
        reads = list(reads)
        writes = list(writes)
        self._wait(e, self._deps(e, reads, writes))
        ins = fn(self.eng[e])
        self.cnt[e] += 1
        ins.then_inc(self.sem[e], 1)
        tk = (e, self.sem[e], self.cnt[e], e)
        self._record(tk, reads, writes)
        self.ninst += 1
        return tk

    def dma(self, q, out_ap, in_ap, owner, reads=(), writes=(), **kw):
        reads = list(reads)
        writes = list(writes)
        if owner.dsem is None:
            owner.dsem = self.es.enter_context(self.nc.semaphore("d_" + owner.name))
            self.owners.append(owner)
        self._wait(q, self._deps(q, reads, writes))
        ins = self.eng[q].dma_start(out=out_ap, in_=in_ap, **kw)
        owner.dcnt += 16
        ins.then_inc(owner.dsem, 16)
        tk = ("d_" + owner.name, owner.dsem, owner.dcnt, None)
        self._record(tk, reads, writes)
        self.ninst += 1
        return tk

    def final_wait(self, e, tiles):
        deps = {}
        for t in tiles:
            for tk in [t.w] + t.r:
                if tk is None:
                    continue
                key, sh, val, te = tk
                if deps.get(key, (None, 0))[1] < val:
                    deps[key] = (sh, val)
        self._wait(e, deps)


def build(T, L):
    NG = T // 512
    NT = T // 128
    nc = bass.Bass("TRN2", target_bir_lowering=False)
    es = ExitStack()

    def din(name, shape):
        return nc.dram_tensor(name, shape, F32, kind="ExternalInput").ap()

    x_d = din("x", [T, D])
    mem_d = din("mem", [256, D])
    lng_d = din("ln_g", [L, D])
    win_d = din("w_in", [L, D, INC])
    cw_d = din("conv_w", [L, 3, BW])
    cb_d = din("conv_b", [L, BW])
    mg_d = din("mem_ln_g", [L, D])
    wkv_d = din("w_mem_kv", [L, D, 2048])
    wbr_d = din("w_branch", [L, 4, BW, D])
    wout_d = din("w_out", [L, D, D])
    fg_d = din("final_g", [1, D])
    ident_d = din("c_ident", [128, 128])
    tri_d = din("c_tri", [128, 128])
    triq_d = din("c_triq", [128, 128])
    eone_d = din("c_eone", [16, 2048])
    pw2_d = din("c_pw2", [128, NIT + 2])
    sel_d = din("c_sel", [128, 16])
    y_d = nc.dram_tensor("y", [T, D], F32, kind="ExternalOutput").ap()
    xmid_d = nc.dram_tensor("xmid", [T, D], F32).ap()
    kT_d = {t: nc.dram_tensor("kT" + t, [8, 128, T], BF16).ap() for t in "as"}
    v_d = {t: nc.dram_tensor("vv" + t, [T, 1024], BF16).ap() for t in "as"}

    with es:
        S = Sched(nc, es)

        def sb(name, shape, dt=F32):
            return es.enter_context(nc.sbuf_tensor(name, shape, dt)), Tk(name)

        def psb(name, shape, dt=F32):
            return es.enter_context(nc.psum_tensor(name, shape, dt)), Tk(name)

        NW = 4
        wt = [sb(f"wt{i}", [128, 4096], BF16) for i in range(NW)]
        xnT, t_xnT = sb("xnT", [128, KC, 512], BF16)
        xall, _ = sb("xall", [128, 4096])
        t_xq = [Tk(f"xq{i}") for i in range(4)]
        scb, t_scb = sb("scb", [128, max(T, 2048)])
        kst, t_kst = sb("kst", [128, 4096], BF16)
        vst, t_vst = sb("vst", [128, 4, 1024], BF16)
        kiT, t_kiT = sb("kiT", [128, T], BF16)
        qiT, t_qiT = sb("qiT", [128, 2, 512], BF16)
        qh = [sb(f"qh{i}", [128, 512], BF16) for i in range(2)]
        sgh = [sb(f"sgh{i}", [128, 512], BF16) for i in range(2)]
        NR = 4
        kring = [sb(f"kr{i}", [128, 1024], BF16) for i in range(NR)]
        vring = [sb(f"vr{i}", [128, 8, 128], BF16) for i in range(NR)]
        pT = [sb(f"pT{i}", [128, 512], BF16) for i in range(3)]
        yT = [sb(f"yT{i}", [128, 8, 512], BF16) for i in range(4)]
        MBW = max(2 * T, 8192)
        MBH = MBW // 2
        mball, _ = sb("mball", [128, MBW], BF16)
        t_mb = [Tk("mb0"), Tk("mb1")]
        Fs = [sb(f"F{i}", [128, 514]) for i in range(5)]
        sgt = [sb(f"sgt{i}", [128, 512], BF16) for i in range(2)]
        mkT, t_mkT = sb("mkT", [128, 8, 256], BF16)
        mv, t_mv = sb("mv", [128, 2, 1024], BF16)
        ident, t_ident = sb("ident", [128, 128], BF16)
        tri, t_tri = sb("tri", [128, 128], BF16)
        triq, t_triq = sb("triq", [128, 128])
        ones, t_ones = sb("ones", [128, 128], BF16)
        eone, t_eone = sb("eone", [16, 2048], BF16)
        pw2, t_pw2 = sb("pw2", [128, NIT + 2])
        sel, t_sel = sb("sel", [128, 16], BF16)
        iwT, t_iwT = sb("iwT", [128, 512], BF16)
        km32, t_km32 = sb("km32", [128, 8, 16])
        kmT, t_kmT = sb("kmT", [128, 8, 16], BF16)
        bsm, t_bsm = sb("bsm", [128, 4, 16])
        m8, t_m8 = sb("m8", [128, 4, 8])
        mbf, t_mbf = sb("mbf", [128, 4, 16], BF16)
        mbT, t_mbT = sb("mbT", [16, 512], BF16)
        wI, t_wI = sb("wI", [128, 4, 4])
        st, t_st = sb("stat", [128, 8])
        epsb, t_eps = sb("epsb", [128, 1])
        bis, t_bis = sb("bis", [128, 8])
        halves, t_halves = sb("halves", [128, NIT + 2])
        nm, t_nm = sb("nm", [128, NIT + 1])
        ssum, t_ssum = sb("ssum", [128, NIT])
        dtmp, t_dtmp = sb("dtmp", [128, NIT])
        cneg, t_cneg = sb("cneg", [128, 1])
        nhh, t_nhh = sb("nhh", [128, NIT + 2])
        cw, t_cw = sb("cw", [128, 8, 3])
        cbias, t_cbias = sb("cbias", [128, 8])
        carry, t_carry = sb("carry", [128, 8, 2])
        sgc, t_sgc = sb("sgc", [128, 512], BF16)

        pj = [psb(f"pj{i}", [128, 512]) for i in range(3)]
        ptr, t_ptr = psb("ptr", [128, 1024], BF16)
        stp = [psb(f"stp{i}", [128, 512]) for i in range(2)]
        ot, t_ot = psb("ot", [128, 512])
        dn, t_dn = psb("dn", [128, 512])
        pjc = [0]

        def next_pj():
            pjc[0] += 1
            return pj[pjc[0] % 3]

        S.dma("pool", ident[:], ident_d, t_ident, writes=[t_ident])
        S.dma("pool", tri[:], tri_d, t_tri, writes=[t_tri])
        S.dma("pool", eone[:], eone_d, t_eone, writes=[t_eone])
        S.dma("sp", triq[:], triq_d, t_triq, writes=[t_triq])
        S.dma("sp", pw2[:], pw2_d, t_pw2, writes=[t_pw2])
        S.dma("pool", sel[:], sel_d, t_sel, writes=[t_sel])
        S.op("dve", lambda e: e.memset(ones[:], 1.0), writes=[t_ones])
        S.op("dve", lambda e: e.memset(epsb[:], EPS), writes=[t_eps])

        wcnt = [0]

        def wload(parts):
            w_sb, w_tk = wt[wcnt[0] % NW]
            wcnt[0] += 1
            for off, nk, ncol, src in parts:
                dst = w_sb[:, off:off + nk * ncol].rearrange("p (k n) -> p k n", k=nk)
                if isinstance(src, list):
                    for lo, sap in src:
                        S.dma("pool", dst[:, :, lo:lo + sap.shape[2]], sap, w_tk, writes=[w_tk])
                else:
                    S.dma("pool", dst, src, w_tk, writes=[w_tk])
            return w_sb, w_tk

        def wview(w_sb, off, nk, ncol):
            return w_sb[:, off:off + nk * ncol].rearrange("p (k n) -> p k n", k=nk)

        def win_cols(l, c0, n, reps=1, stride=0):
            base = win_d[l].rearrange("(k p) n -> p k n", p=128)
            if reps == 1:
                return base[:, :, c0:c0 + n]
            return [(r * n, base[:, :, c0 + r * stride:c0 + r * stride + n]) for r in range(reps)]

        def rmsnorm_rstd(x_ap, x_tks, junk_ap, junk_tks, col):
            S.op("act", lambda e: e.activation(out=junk_ap, in_=x_ap, func=AF.Square, accum_out=st[:, col:col + 1]),
                 reads=x_tks, writes=junk_tks + [t_st])
            S.op("act", lambda e: e.activation(out=st[:, col:col + 1], in_=st[:, col:col + 1], func=AF.Sqrt,
                                               scale=1.0 / D, bias=epsb[:, 0:1]), reads=[t_st, t_eps], writes=[t_st])
            S.op("dve", lambda e: e.reciprocal(st[:, col:col + 1], st[:, col:col + 1]), reads=[t_st], writes=[t_st])

        evc = [0]

        def evac_copy(out_ap, out_tks, in_ap, in_tks):
            evc[0] += 1
            if evc[0] % 2 == 0:
                S.op("act", lambda e: e.activation(out=out_ap, in_=in_ap, func=AF.Copy), reads=in_tks, writes=out_tks)
            else:
                S.op("dve", lambda e: e.tensor_copy(out_ap, in_ap), reads=in_tks, writes=out_tks)

        def norm_T(x_src_ap, g_tk_ready, ncols_tok, tok_off):
            pass

        def fm_chunk(w_view, w_tk, kn, rhs_fn, rhs_tks, ncol_tok):
            ps, tps = next_pj()
            for k in range(kn):
                S.op("pe", lambda e, k=k: e.matmul(ps[:, 0:ncol_tok], w_view[:, k, :], rhs_fn(k),
                                                   start=(k == 0), stop=(k == kn - 1)),
                     reads=[w_tk] + rhs_tks, writes=[tps])
            return ps, tps

        for l in range(L):
            xsrc = x_d if l == 0 else xmid_d
            xreg = [Tk(f"xreg{l}_{g}") for g in range(NG)]
            if l > 0:
                for g in range(NG):
                    xreg[g].w = xreg_prev[g].w
            kreg = {t: [Tk(f"kreg{t}{l}_{g}") for g in range(NG)] for t in "as"}
            vreg = {t: [Tk(f"vreg{t}{l}_{g}") for g in range(NG)] for t in "as"}
            if l > 0:
                for t in "as":
                    for g in range(NG):
                        kreg[t][g].r = list(kreg_prev[t][g].r)
                        kreg[t][g].w = kreg_prev[t][g].w
                        vreg[t][g].r = list(vreg_prev[t][g].r)
                        vreg[t][g].w = vreg_prev[t][g].w

            for k3 in range(3):
                S.dma("sp", cw[:, :, k3], cw_d[l, k3].rearrange("(c p) -> p c", p=128), t_cw, writes=[t_cw],
                      allow_slow_non_contiguous=True)
            S.dma("sp", cbias[:], cb_d[l].rearrange("(c p) -> p c", p=128), t_cbias, writes=[t_cbias],
                  allow_slow_non_contiguous=True)
            S.op("dve", lambda e: e.memset(carry[:], 0.0), writes=[t_carry])
            S.op("dve", lambda e: e.memset(km32[:], 0.0), writes=[t_km32])
            S.op("dve", lambda e: e.memset(kmT[:], 0.0), writes=[t_kmT])

            if DBG == "consts":
                S.drain()
                return nc
            gbc = scb[:, 0:2048]
            S.dma("sp", gbc, mg_d[l:l + 1, :].partition_broadcast(128), t_scb, writes=[t_scb])
            for mt in range(2):
                xt = xall[:, 0:2048]
                S.dma("sp", xt, mem_d[mt * 128:(mt + 1) * 128, :], t_xq[0], writes=[t_xq[0], t_xq[1]])
                xs = kst[:, 0:2048]
                rmsnorm_rstd(xt, [t_xq[0], t_xq[1]], xs, [t_kst], mt)
                S.op("dve", lambda e, mt=mt: e.scalar_tensor_tensor(out=xs, in0=xt, scalar=st[:, mt:mt + 1], in1=gbc,
                                                                    op0=ALU.mult, op1=ALU.mult),
                     reads=[t_xq[0], t_xq[1], t_st, t_scb], writes=[t_kst])
                for k4 in range(4):
                    for kk in range(4):
                        k = k4 * 4 + kk
                        S.op("pe", lambda e, k=k, kk=kk: e.transpose(ptr[:, kk * 128:(kk + 1) * 128],
                                                                     xs[:, k * 128:(k + 1) * 128], ident[:]),
                             reads=[t_kst, t_ident], writes=[t_ptr])
                    evac_copy(xnT[:, k4 * 4:(k4 + 1) * 4, mt * 128:(mt + 1) * 128],
                              [t_xnT], ptr[:, 0:512].rearrange("p (a b) -> p a b", a=4), [t_ptr])
            wkv = wkv_d[l].rearrange("(k p) n -> p k n", p=128)
            for c in range(4):
                w_sb, w_tk = wload([(0, KC, 256, wkv[:, :, c * 256:(c + 1) * 256])])
                wv = wview(w_sb, 0, KC, 256)
                for cc in range(2):
                    ps, tps = fm_chunk(wv[:, :, cc * 128:(cc + 1) * 128], w_tk, KC,
                                       lambda k: xnT[:, k, 0:256], [t_xnT], 256)
                    evac_copy(mkT[:, c * 2 + cc, :], [t_mkT], ps[:, 0:256], [tps])
            for c in range(4):
                w_sb, w_tk = wload([(0, KC, 256, wkv[:, :, 1024 + c * 256:1024 + (c + 1) * 256])])
                wv = wview(w_sb, 0, KC, 256)
                for mt in range(2):
                    ps, tps = next_pj()
                    for k in range(KC):
                        S.op("pe", lambda e, k=k, mt=mt: e.matmul(ps[:, 0:256], xnT[:, k, mt * 128:(mt + 1) * 128],
                                                                  wv[:, k, :], start=(k == 0), stop=(k == KC - 1)),
                             reads=[t_xnT, w_tk], writes=[tps])
                    evac_copy(mv[:, mt, c * 256:(c + 1) * 256], [t_mv], ps[:, 0:256], [tps])

            if DBG == "memkv":
                S.drain()
                return nc
            for g in range(NG):
                tok0 = g * 512
                S.dma("sp", gbc, lng_d[l:l + 1, :].partition_broadcast(128), t_scb, writes=[t_scb])
                for t in range(4):
                    hx = t % 2
                    xt = xall[:, hx * 2048:(hx + 1) * 2048]
                    xtk = [t_xq[2 * hx], t_xq[2 * hx + 1]]
                    S.dma("sp", xt, xsrc[tok0 + t * 128: tok0 + (t + 1) * 128, :], xtk[0], reads=[xreg[g]], writes=xtk)
                    xs = kst[:, 0:2048]
                    rmsnorm_rstd(xt, xtk, xs, [t_kst], t)
                    S.op("dve", lambda e, t=t, xt=xt: e.scalar_tensor_tensor(out=xs, in0=xt, scalar=st[:, t:t + 1],
                                                                             in1=gbc, op0=ALU.mult, op1=ALU.mult),
                         reads=xtk + [t_st, t_scb], writes=[t_kst])
                    for k4 in range(4):
                        for kk in range(4):
                            k = k4 * 4 + kk
                            S.op("pe", lambda e, k=k, kk=kk: e.transpose(ptr[:, kk * 128:(kk + 1) * 128],
                                                                         xs[:, k * 128:(k + 1) * 128], ident[:]),
                                 reads=[t_kst, t_ident], writes=[t_ptr])
                        evac_copy(xnT[:, k4 * 4:(k4 + 1) * 4, t * 128:(t + 1) * 128],
                                  [t_xnT], ptr[:, 0:512].rearrange("p (a b) -> p a b", a=4), [t_ptr])

                if DBG == "stage0":
                    S.drain()
                    return nc
                for typ, ck, cv in (("a", C_AK, C_AV), ("s", C_SK, C_SV)):
                    kst3 = kst[:].rearrange("p (h t) -> p h t", h=8)
                    for c in range(4):
                        w_sb, w_tk = wload([(0, KC, 256, win_cols(l, ck + c * 256, 256))])
                        wv = wview(w_sb, 0, KC, 256)
                        for cc in range(2):
                            h = c * 2 + cc
                            ps, tps = fm_chunk(wv[:, :, cc * 128:(cc + 1) * 128], w_tk, KC,
                                               lambda k: xnT[:, k, :], [t_xnT], 512)
                            S.op("act", lambda e, h=h, ps=ps: e.activation(out=kst3[:, h, :], in_=ps[:], func=AF.Copy),
                                 reads=[tps], writes=[t_kst])
                            if typ == "a":
                                S.op("dve", lambda e, h=h: e.tensor_reduce(
                                    out=km32[:, h, 2 * g:2 * g + 2], in_=kst3[:, h, :].rearrange("p (b t) -> p b t", b=2),
                                    axis=AX.X, op=ALU.add), reads=[t_kst], writes=[t_km32])
                    S.dma("sp", kT_d[typ][:, :, tok0:tok0 + 512].rearrange("h d t -> d h t"), kst3, t_kst,
                          reads=[t_kst], writes=[kreg[typ][g]])
                    if DBG == "kvA":
                        S.drain()
                        return nc
                    if typ == "a":
                        S.op("dve", lambda e: e.tensor_scalar(kmT[:, :, 2 * g:2 * g + 2], km32[:, :, 2 * g:2 * g + 2],
                                                              1.0 / 256.0, None, op0=ALU.mult),
                             reads=[t_km32], writes=[t_kmT])
                    for c in range(4):
                        w_sb, w_tk = wload([(0, KC, 256, win_cols(l, cv + c * 256, 256))])
                        wv = wview(w_sb, 0, KC, 256)
                        for t in range(4):
                            ps, tps = next_pj()
                            for k in range(KC):
                                S.op("pe", lambda e, k=k, t=t, ps=ps: e.matmul(
                                    ps[:, 0:256], xnT[:, k, t * 128:(t + 1) * 128], wv[:, k, :],
                                    start=(k == 0), stop=(k == KC - 1)), reads=[t_xnT, w_tk], writes=[tps])
                            evac_copy(vst[:, t, c * 256:(c + 1) * 256], [t_vst], ps[:, 0:256], [tps])
                    S.dma("sp", v_d[typ][tok0:tok0 + 512, :].rearrange("(t p) c -> p t c", p=128), vst[:], t_vst,
                          reads=[t_vst], writes=[vreg[typ][g]])
                if DBG == "kvC":
                    S.drain()
                    return nc
                w_sb, w_tk = wload([(0, KC, 256, win_cols(l, C_IQ, 256))])
                wv = wview(w_sb, 0, KC, 256)
                for cc in range(2):
                    ps, tps = fm_chunk(wv[:, :, cc * 128:(cc + 1) * 128], w_tk, KC, lambda k: xnT[:, k, :], [t_xnT], 512)
                    evac_copy(qiT[:, cc, :], [t_qiT], ps[:], [tps])
                w_sb, w_tk = wload([(0, KC, 256, win_cols(l, C_IK - 64, 256))])
                wv = wview(w_sb, 0, KC, 256)
                ps, tps = fm_chunk(wv[:, :, 64:192], w_tk, KC, lambda k: xnT[:, k, :], [t_xnT], 512)
                S.op("act", lambda e, ps=ps: e.activation(out=kiT[0:64, tok0:tok0 + 512], in_=ps[0:64, :], func=AF.Copy),
                     reads=[tps], writes=[t_kiT])
                S.op("act", lambda e, ps=ps: e.activation(out=iwT[64:96, :], in_=ps[64:96, :], func=AF.Copy),
                     reads=[tps], writes=[t_iwT])
                ps, tps = fm_chunk(wv[:, :, 0:128], w_tk, KC, lambda k: xnT[:, k, :], [t_xnT], 512)
                S.op("act", lambda e, ps=ps: e.activation(out=kiT[64:128, tok0:tok0 + 512], in_=ps[64:128, :], func=AF.Copy),
                     reads=[tps], writes=[t_kiT])
                ps, tps = next_pj()
                for t in range(4):
                    S.op("pe", lambda e, t=t, ps=ps: e.matmul(ps[:, t * 16:(t + 1) * 16], iwT[64:96, t * 128:(t + 1) * 128],
                                                             sel[64:96, :], start=True, stop=True),
                         reads=[t_iwT, t_sel], writes=[tps])
                S.op("dve", lambda e, ps=ps: e.tensor_scalar(wI[:], ps[:, 0:64].rearrange("p (a b) -> p a b", a=4)[:, :, 0:4],
                                                             (64.0 ** -0.5) * 0.5, None, op0=ALU.mult),
                     reads=[tps], writes=[t_wI])
                if DBG == "kv":
                    S.drain()
                    return nc
                def kv_loader(typ, h, nkt, lookahead=2):
                    nch = (nkt + 7) // 8
                    state = {"issued": 0}

                    def issue(c):
                        slot = (kvc[0] + c) % NR
                        k_sb, k_tk = kring[slot]
                        v_sb, v_tk = vring[slot]
                        k0 = c * 1024
                        n = min(1024, nkt * 128 - k0)
                        gs = sorted(set((k0 + i * 128) // 512 for i in range(n // 128)))
                        S.dma("sp", k_sb[:, 0:n], kT_d[typ][h, :, k0:k0 + n], k_tk,
                              reads=[kreg[typ][gg] for gg in gs], writes=[k_tk])
                        S.dma("sp", v_sb[:, 0:n // 128, :],
                              v_d[typ][k0:k0 + n, h * 128:(h + 1) * 128].rearrange("(a p) d -> p a d", p=128), v_tk,
                              reads=[vreg[typ][gg] for gg in gs], writes=[v_tk])

                    def get(kt):
                        c = kt // 8
                        while state["issued"] < min(nch, c + 1 + lookahead):
                            issue(state["issued"])
                            state["issued"] += 1
                        slot = (kvc[0] + c) % NR
                        k_sb, k_tk = kring[slot]
                        v_sb, v_tk = vring[slot]
                        j = kt % 8
                        return k_sb[:, j * 128:(j + 1) * 128], k_tk, v_sb[:, j, :], v_tk

                    def done():
                        kvc[0] += nch
                    return get, done

                def attention(nkt, qcols, q_ap, q_tk, get_kv, mask_fn, scale):
                    pend = []
                    for kt in range(nkt + 1):
                        if kt < nkt:
                            c0, masks = mask_fn(kt)
                            sp_, tsp = stp[kt % 2]
                            k_ap, k_tk, v_ap, v_tk = get_kv(kt)
                            nm_ = len(masks)
                            S.op("pe", lambda e, sp_=sp_, k_ap=k_ap, c0=c0, nm_=nm_: e.matmul(
                                sp_[:, c0:qcols], k_ap, q_ap[:, c0:qcols], start=True, stop=(nm_ == 0)),
                                reads=[k_tk, q_tk], writes=[tsp])
                            for i, (ml, mr, lo, hi, mtks) in enumerate(masks):
                                S.op("pe", lambda e, sp_=sp_, ml=ml, mr=mr, lo=lo, hi=hi, i=i, nm_=nm_: e.matmul(
                                    sp_[:, lo:hi], ml, mr, start=False, stop=(i == nm_ - 1)),
                                    reads=mtks, writes=[tsp])
                            pend.append((kt, c0, sp_, tsp, v_ap, v_tk))
                        if kt > 0:
                            pk, c0, sp_, tsp, v_ap, v_tk = pend.pop(0)
                            p_sb, p_tk = pT[pk % 3]
                            S.op("act", lambda e, p_sb=p_sb, sp_=sp_, c0=c0: e.activation(
                                out=p_sb[:, c0:qcols], in_=sp_[:, c0:qcols], func=AF.Exp, scale=scale),
                                reads=[tsp], writes=[p_tk])
                            S.op("pe", lambda e, v_ap=v_ap, p_sb=p_sb, c0=c0, pk=pk: e.matmul(
                                ot[:, c0:qcols], v_ap, p_sb[:, c0:qcols], start=(pk == 0), stop=(pk == nkt - 1)),
                                reads=[v_tk, p_tk], writes=[t_ot])
                            S.op("pe", lambda e, p_sb=p_sb, c0=c0, pk=pk: e.matmul(
                                dn[:, c0:qcols], ones[:], p_sb[:, c0:qcols], start=(pk == 0), stop=(pk == nkt - 1)),
                                reads=[t_ones, p_tk], writes=[t_dn])

                def normalize(o_ap, o_tk, qcols, sg_ap, sg_tk, y_ap, y_tk):
                    rden, t_rden = Fs[3]
                    t1, t_t1 = Fs[4]
                    S.op("dve", lambda e: e.reciprocal(rden[:, 0:qcols], dn[:, 0:qcols]), reads=[t_dn], writes=[t_rden])
                    S.op("dve", lambda e: e.tensor_tensor(out=t1[:, 0:qcols], in0=o_ap, in1=rden[:, 0:qcols], op=ALU.mult),
                         reads=[o_tk, t_rden], writes=[t_t1])
                    S.op("dve", lambda e: e.tensor_tensor(out=y_ap, in0=t1[:, 0:qcols], in1=sg_ap, op=ALU.mult),
                         reads=[t_t1, sg_tk], writes=[y_tk])

                for h in range(8):
                    q_sb, q_tk = qh[h % 2]
                    g_sb, g_tk = sgh[h % 2]
                    w_sb, w_tk = wload([(0, KC, 256, win_cols(l, C_AQ + h * 128, 128, reps=2, stride=C_AG - C_AQ))])
                    wv = wview(w_sb, 0, KC, 256)
                    ps, tps = fm_chunk(wv[:, :, 0:128], w_tk, KC, lambda k: xnT[:, k, :], [t_xnT], 512)
                    S.op("act", lambda e, ps=ps, q_sb=q_sb: e.activation(out=q_sb[:], in_=ps[:], func=AF.Copy),
                         reads=[tps], writes=[q_tk])
                    ps, tps = fm_chunk(wv[:, :, 128:256], w_tk, KC, lambda k: xnT[:, k, :], [t_xnT], 512)
                    S.op("act", lambda e, ps=ps, g_sb=g_sb: e.activation(out=g_sb[:], in_=ps[:], func=AF.Silu),
                         reads=[tps], writes=[g_tk])
                    ps, tps = next_pj()
                    for t in range(4):
                        S.op("pe", lambda e, t=t, ps=ps, q_sb=q_sb: e.matmul(
                            ps[:, t * 16:(t + 1) * 16], q_sb[:, t * 128:(t + 1) * 128], kmT[:, h, :], start=True, stop=True),
                            reads=[q_tk, t_kmT], writes=[tps])
                    S.op("dve", lambda e, ps=ps: e.tensor_copy(bsm[:].rearrange("p a b -> p (a b)"), ps[:, 0:64]),
                         reads=[tps], writes=[t_bsm])
                    S.op("dve", lambda e: e.memset(bsm[:, 0:2, 2 * g:16], -BIG), writes=[t_bsm])
                    S.op("dve", lambda e: e.memset(bsm[:, 2:4, 2 * g + 1:16], -BIG), writes=[t_bsm])
                    for t in range(4):
                        S.op("dve", lambda e, t=t: e.max(out=m8[:, t, :], in_=bsm[:, t, :]), reads=[t_bsm], writes=[t_m8])
                    for t in range(4):
                        S.op("dve", lambda e, t=t: e.tensor_scalar(mbf[:, t, :], bsm[:, t, :], m8[:, t, 2:3], NEG,
                                                                  op0=ALU.is_lt, op1=ALU.mult),
                             reads=[t_bsm, t_m8], writes=[t_mbf])
                    for t in range(4):
                        S.op("pe", lambda e, t=t: e.transpose(ptr[0:16, t * 128:(t + 1) * 128], mbf[:, t, :], ident[:]),
                             reads=[t_mbf, t_ident], writes=[t_ptr])
                    S.op("dve", lambda e: e.tensor_copy(mbT[:, :], ptr[0:16, 0:512]), reads=[t_ptr], writes=[t_mbT])

                    def moba_mask(kt):
                        a = kt - 4 * g
                        c0 = 128 * max(a, 0)
                        i = kt // 2
                        masks = []
                        if i < 2 * g:
                            masks.append((eone[:, i * 128:(i + 1) * 128], mbT[:, 0:512], 0, 512, [t_eone, t_mbT]))
                        elif i == 2 * g:
                            masks.append((eone[:, i * 128:(i + 1) * 128], mbT[:, 256:512], 256, 512, [t_eone, t_mbT]))
                            masks.append((ident[:], tri[:], a * 128, (a + 1) * 128, [t_ident, t_tri]))
                        else:
                            masks.append((ident[:], tri[:], a * 128, (a + 1) * 128, [t_ident, t_tri]))
                        return c0, masks

                    get_kv, kv_done = kv_loader("a", h, 4 * g + 4)
                    attention(4 * g + 4, 512, q_sb, q_tk, get_kv, moba_mask, 128.0 ** -0.5)
                    kv_done()
                    normalize(ot[:, 0:512], t_ot, 512, g_sb[:], g_tk, yT[0][0][:, h, :], yT[0][1])

                if DBG == "moba":
                    S.drain()
                    return nc
                for j in range(8):
                    ccs, t_ccs = Fs[0]
                    u, t_u = Fs[1]
                    cacc, t_cacc = Fs[2]
                    w_sb, w_tk = wload([(0, KC, 256, win_cols(l, C_CC + j * 128, 128, reps=2, stride=C_CH - C_CC))])
                    wv = wview(w_sb, 0, KC, 256)
                    ps, tps = fm_chunk(wv[:, :, 0:128], w_tk, KC, lambda k: xnT[:, k, :], [t_xnT], 512)
                    S.op("act", lambda e, ps=ps: e.activation(out=ccs[:, 0:512], in_=ps[:], func=AF.Copy),
                         reads=[tps], writes=[t_ccs])
                    ps, tps = fm_chunk(wv[:, :, 128:256], w_tk, KC, lambda k: xnT[:, k, :], [t_xnT], 512)
                    S.op("dve", lambda e, ps=ps: e.tensor_tensor(out=u[:, 2:514], in0=ps[:], in1=ccs[:, 0:512], op=ALU.mult),
                         reads=[tps, t_ccs], writes=[t_u])
                    S.op("dve", lambda e, j=j: e.tensor_copy(u[:, 0:2], carry[:, j, :]), reads=[t_carry], writes=[t_u])
                    S.op("act", lambda e, j=j: e.activation(out=cacc[:, 0:512], in_=u[:, 2:514], func=AF.Identity,
                                                            scale=cw[:, j, 2:3], bias=cbias[:, j:j + 1]),
                         reads=[t_u, t_cw, t_cbias], writes=[t_cacc])
                    S.op("dve", lambda e, j=j: e.scalar_tensor_tensor(out=cacc[:, 0:512], in0=u[:, 1:513], scalar=cw[:, j, 1:2],
                                                                      in1=cacc[:, 0:512], op0=ALU.mult, op1=ALU.add),
                         reads=[t_u, t_cw, t_cacc], writes=[t_cacc])
                    S.op("dve", lambda e, j=j: e.scalar_tensor_tensor(out=cacc[:, 0:512], in0=u[:, 0:512], scalar=cw[:, j, 0:1],
                                                                      in1=cacc[:, 0:512], op0=ALU.mult, op1=ALU.add),
                         reads=[t_u, t_cw, t_cacc], writes=[t_cacc])
                    S.op("dve", lambda e, j=j: e.tensor_copy(carry[:, j, :], u[:, 512:514]), reads=[t_u], writes=[t_carry])
                    w_sb, w_tk = wload([(0, KC, 256, win_cols(l, C_CB + j * 128, 128, reps=2, stride=C_CG - C_CB))])
                    wv = wview(w_sb, 0, KC, 256)
                    ps, tps = fm_chunk(wv[:, :, 128:256], w_tk, KC, lambda k: xnT[:, k, :], [t_xnT], 512)
                    S.op("act", lambda e, ps=ps: e.activation(out=sgc[:], in_=ps[:], func=AF.Silu), reads=[tps], writes=[t_sgc])
                    ps, tps = fm_chunk(wv[:, :, 0:128], w_tk, KC, lambda k: xnT[:, k, :], [t_xnT], 512)
                    S.op("dve", lambda e, ps=ps: e.tensor_tensor(out=cacc[:, 0:512], in0=ps[:], in1=cacc[:, 0:512], op=ALU.mult),
                         reads=[tps, t_cacc], writes=[t_cacc])
                    S.op("dve", lambda e, j=j: e.tensor_tensor(out=yT[1][0][:, j, :], in0=cacc[:, 0:512], in1=sgc[:], op=ALU.mult),
                         reads=[t_cacc, t_sgc], writes=[yT[1][1]])

                if DBG == "conv":
                    S.drain()
                    return nc
                for hh in range(2):
                    for tt in range(2):
                        t = hh * 2 + tt
                        Q = 4 * g + t
                        nk = (Q + 1) * 128
                        mb_ap = mball[:, tt * MBH: tt * MBH + nk]
                        nchunk = (nk + 511) // 512
                        for c in range(nchunk):
                            wdt = min(512, nk - c * 512)
                            for j in range(4):
                                pb = 64 * (j % 2)
                                ps, tps = next_pj()
                                rl, t_rl = Fs[j % 2]
                                S.op("pe", lambda e, ps=ps, pb=pb, j=j, c=c, wdt=wdt, t=t: e.matmul(
                                    ps[:, 0:wdt], qiT[pb:pb + 64, j // 2, t * 128:(t + 1) * 128],
                                    kiT[pb:pb + 64, c * 512:c * 512 + wdt], start=True, stop=True),
                                    reads=[t_qiT, t_kiT], writes=[tps])
                                S.op("act", lambda e, ps=ps, rl=rl, wdt=wdt: e.activation(out=rl[:, 0:wdt], in_=ps[:, 0:wdt], func=AF.Relu),
                                     reads=[tps], writes=[t_rl])
                                if j == 0:
                                    S.op("dve", lambda e, rl=rl, c=c, wdt=wdt, t=t: e.tensor_scalar(
                                        scb[:, c * 512:c * 512 + wdt], rl[:, 0:wdt], wI[:, t, 0:1], None, op0=ALU.mult),
                                        reads=[t_rl, t_wI], writes=[t_scb])
                                else:
                                    S.op("dve", lambda e, rl=rl, c=c, wdt=wdt, t=t, j=j: e.scalar_tensor_tensor(
                                        out=scb[:, c * 512:c * 512 + wdt], in0=rl[:, 0:wdt], scalar=wI[:, t, j:j + 1],
                                        in1=scb[:, c * 512:c * 512 + wdt], op0=ALU.mult, op1=ALU.add),
                                        reads=[t_rl, t_wI, t_scb], writes=[t_scb])
                        S.op("dve", lambda e, nk=nk: e.tensor_reduce(out=bis[:, 0:1], in_=scb[:, 0:nk], axis=AX.X, op=ALU.min),
                             reads=[t_scb], writes=[t_bis])
                        S.op("dve", lambda e, nk=nk: e.tensor_reduce(out=bis[:, 1:2], in_=scb[:, 0:nk], axis=AX.X, op=ALU.max),
                             reads=[t_scb], writes=[t_bis])
                        S.op("dve", lambda e, Q=Q: e.tensor_tensor(out=scb[:, Q * 128:(Q + 1) * 128], in0=scb[:, Q * 128:(Q + 1) * 128],
                                                                   in1=triq[:], op=ALU.add), reads=[t_scb, t_triq], writes=[t_scb])
                        S.op("dve", lambda e: e.tensor_tensor(out=bis[:, 2:3], in0=bis[:, 1:2], in1=bis[:, 0:1], op=ALU.subtract),
                             reads=[t_bis], writes=[t_bis])
                        S.op("dve", lambda e: e.tensor_scalar(bis[:, 2:3], bis[:, 2:3], 1.0001, 1e-6, op0=ALU.mult, op1=ALU.add),
                             reads=[t_bis], writes=[t_bis])
                        S.op("dve", lambda e: e.tensor_scalar(halves[:], pw2[:], bis[:, 2:3], None, op0=ALU.mult),
                             reads=[t_pw2, t_bis], writes=[t_halves])
                        S.op("dve", lambda e: e.scalar_tensor_tensor(out=nm[:, 0:1], in0=bis[:, 0:1], scalar=-1.0, in1=halves[:, 0:1],
                                                                     op0=ALU.mult, op1=ALU.subtract),
                             reads=[t_bis, t_halves], writes=[t_nm])
                        S.op("dve", lambda e: e.memset(ssum[:], 0.0), writes=[t_ssum])
                        cthr = float(2 * min(TOPK, T // 4) - nk)
                        S.op("dve", lambda e: e.memset(cneg[:], 0.5 - cthr), writes=[t_cneg])
                        S.op("dve", lambda e: e.tensor_scalar(nhh[:], halves[:], -0.5, None, op0=ALU.mult),
                             reads=[t_halves], writes=[t_nhh])
                        for it in range(NIT):
                            S.op("act", lambda e, it=it, nk=nk: e.activation(out=kst[:, 0:nk], in_=scb[:, 0:nk], func=AF.Sign,
                                                                             bias=nm[:, it:it + 1], scale=1.0,
                                                                             accum_out=ssum[:, it:it + 1]),
                                 reads=[t_scb, t_nm], writes=[t_kst, t_ssum])
                            S.op("act", lambda e, it=it: e.activation(out=dtmp[:, it:it + 1], in_=ssum[:, it:it + 1], func=AF.Sign,
                                                                      bias=cneg[:, 0:1], scale=1.0),
                                 reads=[t_ssum, t_cneg], writes=[t_dtmp])
                            S.op("act", lambda e, it=it: e.activation(out=nm[:, it + 1:it + 2], in_=dtmp[:, it:it + 1], func=AF.Identity,
                                                                      scale=nhh[:, it:it + 1], bias=nm[:, it:it + 1]),
                                 reads=[t_dtmp, t_nhh, t_nm], writes=[t_nm])
                        S.op("dve", lambda e: e.scalar_tensor_tensor(out=bis[:, 3:4], in0=nm[:, NIT:NIT + 1], scalar=-1.0,
                                                                     in1=halves[:, NIT + 1:NIT + 2], op0=ALU.mult, op1=ALU.subtract),
                             reads=[t_nm, t_halves], writes=[t_bis])
                        S.op("dve", lambda e, mb_ap=mb_ap, nk=nk: e.tensor_scalar(mb_ap, scb[:, 0:nk], bis[:, 3:4], NEG,
                                                                                 op0=ALU.is_lt, op1=ALU.mult),
                             reads=[t_scb, t_bis], writes=[t_mb[tt]])
                    nkt = 4 * g + 2 * hh + 2
                    for h in range(8):
                        q_sb, q_tk = qh[h % 2]
                        g_sb, g_tk = sgh[h % 2]
                        w_sb, w_tk = wload([(0, KC, 256, win_cols(l, C_SQ + h * 128, 128, reps=2, stride=C_SG - C_SQ))])
                        wv = wview(w_sb, 0, KC, 256)
                        ps, tps = fm_chunk(wv[:, :, 0:128], w_tk, KC, lambda k: xnT[:, k, hh * 256:(hh + 1) * 256], [t_xnT], 256)
                        S.op("act", lambda e, ps=ps, q_sb=q_sb: e.activation(out=q_sb[:, 0:256], in_=ps[:, 0:256], func=AF.Copy),
                             reads=[tps], writes=[q_tk])
                        ps, tps = fm_chunk(wv[:, :, 128:256], w_tk, KC, lambda k: xnT[:, k, hh * 256:(hh + 1) * 256], [t_xnT], 256)
                        S.op("act", lambda e, ps=ps, g_sb=g_sb: e.activation(out=g_sb[:, 0:256], in_=ps[:, 0:256], func=AF.Silu),
                             reads=[tps], writes=[g_tk])

                        def dsa_mask(kt):
                            a = kt - (4 * g + 2 * hh)
                            c0 = 128 * max(a, 0)
                            masks = []
                            for tt in range(2):
                                if kt <= 4 * g + 2 * hh + tt:
                                    masks.append((mball[:, tt * MBH + kt * 128: tt * MBH + (kt + 1) * 128], ident[:],
                                                  tt * 128, (tt + 1) * 128, [t_mb[tt], t_ident]))
                            return c0, masks

                        get_kv, kv_done = kv_loader("s", h, nkt)
                        attention(nkt, 256, q_sb, q_tk, get_kv, dsa_mask, 128.0 ** -0.5)
                        kv_done()
                        normalize(ot[:, 0:256], t_ot, 256, g_sb[:, 0:256], g_tk,
                                  yT[2][0][:, h, hh * 256:(hh + 1) * 256], yT[2][1])

                if DBG == "dsa":
                    S.drain()
                    return nc
                for h in range(4):
                    for dc in range(2):
                        q_sb, q_tk = qh[dc]
                        g_sb, g_tk = sgh[dc]
                        cidx = h * 2 + dc
                        w_sb, w_tk = wload([(0, KC, 256, win_cols(l, C_MQ + cidx * 128, 128, reps=2, stride=C_MG - C_MQ))])
                        wv = wview(w_sb, 0, KC, 256)
                        ps, tps = fm_chunk(wv[:, :, 0:128], w_tk, KC, lambda k: xnT[:, k, :], [t_xnT], 512)
                        S.op("act", lambda e, ps=ps, q_sb=q_sb: e.activation(out=q_sb[:], in_=ps[:], func=AF.Copy),
                             reads=[tps], writes=[q_tk])
                        ps, tps = fm_chunk(wv[:, :, 128:256], w_tk, KC, lambda k: xnT[:, k, :], [t_xnT], 512)
                        S.op("act", lambda e, ps=ps, g_sb=g_sb: e.activation(out=g_sb[:], in_=ps[:], func=AF.Silu),
                             reads=[tps], writes=[g_tk])
                    pts = []
                    for mt in range(2):
                        sp_, tsp = stp[mt]
                        for dc in range(2):
                            S.op("pe", lambda e, sp_=sp_, mt=mt, dc=dc: e.matmul(
                                sp_[:], mkT[:, h * 2 + dc, mt * 128:(mt + 1) * 128], qh[dc][0][:], start=(dc == 0), stop=(dc == 1)),
                                reads=[t_mkT, qh[dc][1]], writes=[tsp])
                        p_sb, p_tk = pT[mt]
                        S.op("act", lambda e, sp_=sp_, p_sb=p_sb: e.activation(out=p_sb[:], in_=sp_[:], func=AF.Exp, scale=256.0 ** -0.5),
                             reads=[tsp], writes=[p_tk])
                        pts.append((p_sb, p_tk))
                    o2, t_o2 = pj[0]
                    for mt in range(2):
                        p_sb, p_tk = pts[mt]
                        S.op("pe", lambda e, mt=mt, p_sb=p_sb: e.matmul(ot[:], mv[:, mt, h * 256:h * 256 + 128], p_sb[:],
                                                                        start=(mt == 0), stop=(mt == 1)),
                             reads=[t_mv, p_tk], writes=[t_ot])
                        S.op("pe", lambda e, mt=mt, p_sb=p_sb: e.matmul(o2[:], mv[:, mt, h * 256 + 128:h * 256 + 256], p_sb[:],
                                                                        start=(mt == 0), stop=(mt == 1)),
                             reads=[t_mv, p_tk], writes=[t_o2])
                        S.op("pe", lambda e, mt=mt, p_sb=p_sb: e.matmul(dn[:], ones[:], p_sb[:], start=(mt == 0), stop=(mt == 1)),
                             reads=[t_ones, p_tk], writes=[t_dn])
                    normalize(ot[:], t_ot, 512, sgh[0][0][:], sgh[0][1], yT[3][0][:, h * 2, :], yT[3][1])
                    normalize(o2[:], t_o2, 512, sgh[1][0][:], sgh[1][1], yT[3][0][:, h * 2 + 1, :], yT[3][1])

                if DBG == "mem":
                    S.drain()
                    return nc
                mrg = mball[:, 0:8192].rearrange("p (c t) -> p c t", c=16)
                t_mrg = t_mb
                for oc in range(16):
                    acc, t_acc = Fs[oc % 2]
                    for br in range(4):
                        tmp, t_tmp = Fs[2 + (br % 2)]
                        sg_sb, sg_tk = sgt[br % 2]
                        w_sb, w_tk = wload([(0, KC, 128, win_cols(l, C_R + 2048 * br + 128 * oc, 128)),
                                            (KC * 128, 8, 128,
                                             wbr_d[l, br].rearrange("(k p) n -> p k n", p=128)[:, :, oc * 128:(oc + 1) * 128])])
                        wr = wview(w_sb, 0, KC, 128)
                        wb = wview(w_sb, KC * 128, 8, 128)
                        ps, tps = fm_chunk(wr, w_tk, KC, lambda k: xnT[:, k, :], [t_xnT], 512)
                        S.op("act", lambda e, ps=ps, sg_sb=sg_sb: e.activation(out=sg_sb[:], in_=ps[:], func=AF.Sigmoid),
                             reads=[tps], writes=[sg_tk])
                        y_sb, y_tk = yT[br]
                        ps, tps = fm_chunk(wb, w_tk, 8, lambda k, y_sb=y_sb: y_sb[:, k, :], [y_tk], 512)
                        if br == 0:
                            S.op("dve", lambda e, ps=ps, sg_sb=sg_sb, acc=acc: e.tensor_tensor(
                                out=acc[:, 0:512], in0=ps[:], in1=sg_sb[:], op=ALU.mult), reads=[tps, sg_tk], writes=[t_acc])
                        else:
                            S.op("dve", lambda e, ps=ps, sg_sb=sg_sb, tmp=tmp: e.tensor_tensor(
                                out=tmp[:, 0:512], in0=ps[:], in1=sg_sb[:], op=ALU.mult), reads=[tps, sg_tk], writes=[t_tmp])
                            if br < 3:
                                S.op("dve", lambda e, tmp=tmp, acc=acc: e.tensor_tensor(
                                    out=acc[:, 0:512], in0=acc[:, 0:512], in1=tmp[:, 0:512], op=ALU.add),
                                    reads=[t_acc, t_tmp], writes=[t_acc])
                            else:
                                S.op("dve", lambda e, tmp=tmp, acc=acc, oc=oc: e.tensor_tensor(
                                    out=mrg[:, oc, :], in0=acc[:, 0:512], in1=tmp[:, 0:512], op=ALU.add),
                                    reads=[t_acc, t_tmp], writes=[t_mb[0], t_mb[1]])

                if DBG == "merge":
                    S.drain()
                    return nc
                wo = wout_d[l].rearrange("(k p) n -> p k n", p=128)
                for cb in range(8):
                    w_sb, w_tk = wload([(0, KC, 256, wo[:, :, cb * 256:(cb + 1) * 256])])
                    wv = wview(w_sb, 0, KC, 256)
                    xr = xall[:, (cb % 2) * 1024:(cb % 2 + 1) * 1024].rearrange("p (t c) -> p t c", t=4)
                    t_xr = t_xq[cb % 2]
                    xo = xall[:, 2048 + (cb % 2) * 1024: 2048 + (cb % 2 + 1) * 1024].rearrange("p (t c) -> p t c", t=4)
                    t_xo = t_xq[2 + cb % 2]
                    S.dma("sp", xr, xsrc[tok0:tok0 + 512, cb * 256:(cb + 1) * 256].rearrange("(t p) c -> p t c", p=128),
                          t_xr, reads=[xreg[g]], writes=[t_xr])
                    for t in range(4):
                        ps, tps = next_pj()
                        for k in range(KC):
                            S.op("pe", lambda e, k=k, t=t, ps=ps: e.matmul(
                                ps[:, 0:256], mrg[:, k, t * 128:(t + 1) * 128], wv[:, k, :], start=(k == 0), stop=(k == KC - 1)),
                                reads=[t_mb[0], t_mb[1], w_tk], writes=[tps])
                        S.op("dve", lambda e, t=t, ps=ps, xo=xo, xr=xr: e.tensor_tensor(
                            out=xo[:, t, :], in0=ps[:, 0:256], in1=xr[:, t, :], op=ALU.add),
                            reads=[tps, t_xr], writes=[t_xo])
                    S.dma("sp", xmid_d[tok0:tok0 + 512, cb * 256:(cb + 1) * 256].rearrange("(t p) c -> p t c", p=128), xo,
                          t_xo, reads=[t_xo], writes=[xreg[g]] if l > 0 or True else [])
            xreg_prev = xreg
            kreg_prev = kreg
            vreg_prev = vreg

        gbc = scb[:, 0:2048]
        S.dma("sp", gbc, fg_d.partition_broadcast(128), t_scb, writes=[t_scb])
        for tt in range(NT):
            hx = tt % 2
            xt = xall[:, hx * 2048:(hx + 1) * 2048]
            xtk = [t_xq[2 * hx], t_xq[2 * hx + 1]]
            S.dma("sp", xt, xmid_d[tt * 128:(tt + 1) * 128, :], xtk[0], reads=[xreg_prev[tt // 4]], writes=xtk)
            rmsnorm_rstd(xt, xtk, kst[:, 0:2048], [t_kst], tt % 8)
            S.op("dve", lambda e, xt=xt, c=tt % 8: e.scalar_tensor_tensor(out=xt, in0=xt, scalar=st[:, c:c + 1], in1=gbc,
                                                                          op0=ALU.mult, op1=ALU.mult),
                 reads=xtk + [t_st, t_scb], writes=xtk)
            S.dma("sp", y_d[tt * 128:(tt + 1) * 128, :], xt, xtk[1], reads=xtk, writes=[])
        S.final_wait("sp", t_xq)
        S.drain()
    return nc


kvc = [0]
DBG = None


def _consts():
    k = np.arange(128)
    tri = np.where(k[None, :] >= k[:, None], 0.0, NEG).astype(np.float32)
    triq = np.where(k[None, :] <= k[:, None], 0.0, -BIG).astype(np.float32)
    eone = np.zeros((16, 2048), np.float32)
    for i in range(16):
        eone[i, i * 128:(i + 1) * 128] = 1.0
    pw2 = np.zeros((128, NIT + 2), np.float32)
    for i in range(NIT + 1):
        pw2[:, i] = 2.0 ** -(i + 1)
    pw2[:, NIT + 1] = 1.25 * 2.0 ** -(NIT + 1)
    selm = np.zeros((128, 16), np.float32)
    for j in range(4):
        selm[64 + j, j] = 1.0
    return {"c_sel": selm, "c_ident": np.eye(128, dtype=np.float32), "c_tri": tri, "c_triq": triq, "c_eone": eone, "c_pw2": pw2}


_CACHE = {}


def run(inputs, T, L, n_cores, batch_of_core):
    key = (T, L)
    if key not in _CACHE:
        kvc[0] = 0
        _CACHE[key] = build(T, L)
    nc = _CACHE[key]
    cst = _consts()
    f = lambda a: np.ascontiguousarray(np.asarray(a, dtype=np.float32))
    shared = {
        "ln_g": f(inputs["ln_g"]), "w_in": f(inputs["w_in"]), "conv_w": f(inputs["conv_w"]),
        "conv_b": f(inputs["conv_b"]), "mem_ln_g": f(inputs["mem_ln_g"]), "w_mem_kv": f(inputs["w_mem_kv"]),
        "w_branch": f(inputs["w_branch"]), "w_out": f(inputs["w_out"]),
        "final_g": f(inputs["final_g"]).reshape(1, D), **cst,
    }
    in_maps = []
    zshared = None
    for c in range(n_cores):
        b = batch_of_core[c]
        if b is None:
            if zshared is None:
                zshared = {k: (v if k.startswith("c_") else np.zeros_like(v)) for k, v in shared.items()}
                zshared["x"] = np.zeros((T, D), np.float32)
                zshared["mem"] = np.zeros((256, D), np.float32)
            in_maps.append(zshared)
            continue
        m = dict(shared)
        m["x"] = f(inputs["x"][b])
        m["mem"] = f(inputs["mem"][b])
        in_maps.append(m)
    res = run_bass_kernel_spmd(nc, in_maps, core_ids=list(range(n_cores)))
    return [r["y"] for r in res.results]


def kernel(x, mem, ln_g, w_in, conv_w, conv_b, mem_ln_g, w_mem_kv, w_branch, w_out, final_g):
    inputs = dict(x=x, mem=mem, ln_g=ln_g, w_in=w_in, conv_w=conv_w, conv_b=conv_b, mem_ln_g=mem_ln_g,
                  w_mem_kv=w_mem_kv, w_branch=w_branch, w_out=w_out, final_g=final_g)
    B, T, _ = np.asarray(x).shape
    L = np.asarray(ln_g).shape[0]
    owner = {0: 0, 1: 1, 4: 2, 5: 3}
    outs = run(inputs, T, L, 8, [owner.get(c) if owner.get(c, B) < B else None for c in range(8)])
    core_of = {b: c for c, b in owner.items()}
    return np.stack([outs[core_of[b]] for b in range(B)], axis=0).astype(np.float32)
```

```python
import os
import numpy as np
import concourse.bass as bass
import concourse.mybir as mybir
from concourse.bass_utils import run_bass_kernel_spmd
from contextlib import ExitStack

F32 = mybir.dt.float32
BF16 = mybir.dt.bfloat16
AF = mybir.ActivationFunctionType
ALU = mybir.AluOpType
AX = mybir.AxisListType

D = 2048
KC = 16
BW = 1024
INC = 22852
NEG = -30000.0
BIG = 1.0e30
EPS = 1e-6
C_AQ, C_AK, C_AV, C_AG = 0, 1024, 2048, 3072
C_CB, C_CC, C_CH, C_CG = 4096, 5120, 6144, 7168
C_SQ, C_SK, C_SV, C_SG = 8192, 9216, 10240, 11264
C_MQ, C_MG = 12288, 13312
C_IQ, C_IK, C_IW, C_R = 14336, 14592, 14656, 14660
NIT = 16
TOPK = 256


class Tk:
    __slots__ = ("name", "w", "r", "dsem", "dcnt")

    def __init__(self, name):
        self.name = name
        self.w = None
        self.r = []
        self.dsem = None
        self.dcnt = 0


class Sched:
    def __init__(self, nc, es):
        self.nc = nc
        self.es = es
        self.eng = {"pe": nc.tensor, "act": nc.scalar, "dve": nc.vector, "pool": nc.gpsimd, "sp": nc.sync}
        self.sem = {k: es.enter_context(nc.semaphore("sem_" + k)) for k in self.eng}
        self.cnt = {k: 0 for k in self.eng}
        self.waited = {k: {} for k in self.eng}
        self.ninst = 0
        self.owners = []

    def drain(self):
        for k in self.eng:
            if self.cnt[k] > 0 and k != "sp":
                self.eng["sp"].wait_ge(self.sem[k], self.cnt[k])
        for o in self.owners:
            self.eng["sp"].wait_ge(o.dsem, o.dcnt)

    def _deps(self, e, reads, writes):
        deps = {}

        def add(tk, war):
            if tk is None:
                return
            key, sh, val, te = tk
            if te == e and e == "pe":
                return
            if deps.get(key, (None, 0))[1] < val:
                deps[key] = (sh, val)

        for t in reads:
            add(t.w, False)
        for t in writes:
            add(t.w, False)
            for rt in t.r:
                add(rt, True)
        return deps

    def _wait(self, e, deps):
        for key, (sh, val) in deps.items():
            if self.waited[e].get(key, 0) >= val:
                continue
            self.eng[e].wait_ge(sh, val)
            self.waited[e][key] = val

    def _record(self, tk, reads, writes):
        for t in writes:
            t.w = tk
            t.r = []
        for t in reads:
            if t in writes:
                continue
            t.r = [x for x in t.r if x[0] != tk[0]] + [tk]

    def op(self, e, fn, reads=(), writes=()):
        reads = list(reads)
        writes = list(writes)
        self._wait(e, self._deps(e, reads, writes))
        ins = fn(self.eng[e])
        self.cnt[e] += 1
        ins.then_inc(self.sem[e], 1)
        tk = (e, self.sem[e], self.cnt[e], e)
        self._record(tk, reads, writes)
        self.ninst += 1
        return tk

    def dma(self, q, out_ap, in_ap, owner, reads=(), writes=(), **kw):
        reads = list(reads)
        writes = list(writes)
        if owner.dsem is None:
            owner.dsem = self.es.enter_context(self.nc.semaphore("d_" + owner.name))
            self.owners.append(owner)
        self._wait(q, self._deps(q, reads, writes))
        ins = self.eng[q].dma_start(out=out_ap, in_=in_ap, **kw)
        owner.dcnt += 16
        ins.then_inc(owner.dsem, 16)
        tk = ("d_" + owner.name, owner.dsem, owner.dcnt, None)
        self._record(tk, reads, writes)
        self.ninst += 1
        return tk

    def final_wait(self, e, tiles):
        deps = {}
        for t in tiles:
            for tk in [t.w] + t.r:
                if tk is None:
                    continue
                key, sh, val, te = tk
                if deps.get(key, (None, 0))[1] < val:
                    deps[key] = (sh, val)
        self._wait(e, deps)


def build(T, L):
    NG = T // 512
    NT = T // 128
    nc = bass.Bass("TRN2", target_bir_lowering=False)
    es = ExitStack()

    def din(name, shape):
        return nc.dram_tensor(name, shape, F32, kind="ExternalInput").ap()

    x_d = din("x", [T, D])
    mem_d = din("mem", [256, D])
    lng_d = din("ln_g", [L, D])
    win_d = din("w_in", [L, D, INC])
    cw_d = din("conv_w", [L, 3, BW])
    cb_d = din("conv_b", [L, BW])
    mg_d = din("mem_ln_g", [L, D])
    wkv_d = din("w_mem_kv", [L, D, 2048])
    wbr_d = din("w_branch", [L, 4, BW, D])
    wout_d = din("w_out", [L, D, D])
    fg_d = din("final_g", [1, D])
    ident_d = din("c_ident", [128, 128])
    tri_d = din("c_tri", [128, 128])
    triq_d = din("c_triq", [128, 128])
    eone_d = din("c_eone", [16, 2048])
    pw2_d = din("c_pw2", [128, NIT + 2])
    sel_d = din("c_sel", [128, 16])
    y_d = nc.dram_tensor("y", [T, D], F32, kind="ExternalOutput").ap()
    xmid_d = nc.dram_tensor("xmid", [T, D], F32).ap()
    kT_d = {t: nc.dram_tensor("kT" + t, [8, 128, T], BF16).ap() for t in "as"}
    v_d = {t: nc.dram_tensor("vv" + t, [T, 1024], BF16).ap() for t in "as"}

    with es:
        S = Sched(nc, es)

        def sb(name, shape, dt=F32):
            return es.enter_context(nc.sbuf_tensor(name, shape, dt)), Tk(name)

        def psb(name, shape, dt=F32):
            return es.enter_context(nc.psum_tensor(name, shape, dt)), Tk(name)

        NW = 4
        wt = [sb(f"wt{i}", [128, 4096], BF16) for i in range(NW)]
        xnT, t_xnT = sb("xnT", [128, KC, 512], BF16)
        xall, _ = sb("xall", [128, 4096])
        t_xq = [Tk(f"xq{i}") for i in range(4)]
        scb, t_scb = sb("scb", [128, max(T, 2048)])
        kst, t_kst = sb("kst", [128, 4096], BF16)
        vst, t_vst = sb("vst", [128, 4, 1024], BF16)
        kiT, t_kiT = sb("kiT", [128, T], BF16)
        qiT, t_qiT = sb("qiT", [128, 2, 512], BF16)
        qh = [sb(f"qh{i}", [128, 512], BF16) for i in range(2)]
        sgh = [sb(f"sgh{i}", [128, 512], BF16) for i in range(2)]
        NR = 4
        kring = [sb(f"kr{i}", [128, 1024], BF16) for i in range(NR)]
        vring = [sb(f"vr{i}", [128, 8, 128], BF16) for i in range(NR)]
        pT = [sb(f"pT{i}", [128, 512], BF16) for i in range(3)]
        yT = [sb(f"yT{i}", [128, 8, 512], BF16) for i in range(4)]
        MBW = max(2 * T, 8192)
        MBH = MBW // 2
        mball, _ = sb("mball", [128, MBW], BF16)
        t_mb = [Tk("mb0"), Tk("mb1")]
        Fs = [sb(f"F{i}", [128, 514]) for i in range(5)]
        sgt = [sb(f"sgt{i}", [128, 512], BF16) for i in range(2)]
        mkT, t_mkT = sb("mkT", [128, 8, 256], BF16)
        mv, t_mv = sb("mv", [128, 2, 1024], BF16)
        ident, t_ident = sb("ident", [128, 128], BF16)
        tri, t_tri = sb("tri", [128, 128], BF16)
        triq, t_triq = sb("triq", [128, 128])
        ones, t_ones = sb("ones", [128, 128], BF16)
        eone, t_eone = sb("eone", [16, 2048], BF16)
        pw2, t_pw2 = sb("pw2", [128, NIT + 2])
        sel, t_sel = sb("sel", [128, 16], BF16)
        iwT, t_iwT = sb("iwT", [128, 512], BF16)
        km32, t_km32 = sb("km32", [128, 8, 16])
        kmT, t_kmT = sb("kmT", [128, 8, 16], BF16)
        bsm, t_bsm = sb("bsm", [128, 4, 16])
        m8, t_m8 = sb("m8", [128, 4, 8])
        mbf, t_mbf = sb("mbf", [128, 4, 16], BF16)
        mbT, t_mbT = sb("mbT", [16, 512], BF16)
        wI, t_wI = sb("wI", [128, 4, 4])
        st, t_st = sb("stat", [128, 8])
        epsb, t_eps = sb("epsb", [128, 1])
        bis, t_bis = sb("bis", [128, 8])
        halves, t_halves = sb("halves", [128, NIT + 2])
        nm, t_nm = sb("nm", [128, NIT + 1])
        ssum, t_ssum = sb("ssum", [128, NIT])
        dtmp, t_dtmp = sb("dtmp", [128, NIT])
        cneg, t_cneg = sb("cneg", [128, 1])
        nhh, t_nhh = sb("nhh", [128, NIT + 2])
        cw, t_cw = sb("cw", [128, 8, 3])
        cbias, t_cbias = sb("cbias", [128, 8])
        carry, t_carry = sb("carry", [128, 8, 2])
        sgc, t_sgc = sb("sgc", [128, 512], BF16)

        pj = [psb(f"pj{i}", [128, 512]) for i in range(3)]
        ptr, t_ptr = psb("ptr", [128, 1024], BF16)
        stp = [psb(f"stp{i}", [128, 512]) for i in range(2)]
        ot, t_ot = psb("ot", [128, 512])
        dn, t_dn = psb("dn", [128, 512])
        pjc = [0]

        def next_pj():
            pjc[0] += 1
            return pj[pjc[0] % 3]

        S.dma("pool", ident[:], ident_d, t_ident, writes=[t_ident])
        S.dma("pool", tri[:], tri_d, t_tri, writes=[t_tri])
        S.dma("pool", eone[:], eone_d, t_eone, writes=[t_eone])
        S.dma("sp", triq[:], triq_d, t_triq, writes=[t_triq])
        S.dma("sp", pw2[:], pw2_d, t_pw2, writes=[t_pw2])
        S.dma("pool", sel[:], sel_d, t_sel, writes=[t_sel])
        S.op("dve", lambda e: e.memset(ones[:], 1.0), writes=[t_ones])
        S.op("dve", lambda e: e.memset(epsb[:], EPS), writes=[t_eps])

        wcnt = [0]

        def wload(parts):
            w_sb, w_tk = wt[wcnt[0] % NW]
            wcnt[0] += 1
            for off, nk, ncol, src in parts:
                dst = w_sb[:, off:off + nk * ncol].rearrange("p (k n) -> p k n", k=nk)
                if isinstance(src, list):
                    for lo, sap in src:
                        S.dma("pool", dst[:, :, lo:lo + sap.shape[2]], sap, w_tk, writes=[w_tk])
                else:
                    S.dma("pool", dst, src, w_tk, writes=[w_tk])
            return w_sb, w_tk

        def wview(w_sb, off, nk, ncol):
            return w_sb[:, off:off + nk * ncol].rearrange("p (k n) -> p k n", k=nk)

        def win_cols(l, c0, n, reps=1, stride=0):
            base = win_d[l].rearrange("(k p) n -> p k n", p=128)
            if reps == 1:
                return base[:, :, c0:c0 + n]
            return [(r * n, base[:, :, c0 + r * stride:c0 + r * stride + n]) for r in range(reps)]

        def rmsnorm_rstd(x_ap, x_tks, junk_ap, junk_tks, col):
            S.op("act", lambda e: e.activation(out=junk_ap, in_=x_ap, func=AF.Square, accum_out=st[:, col:col + 1]),
                 reads=x_tks, writes=junk_tks + [t_st])
            S.op("act", lambda e: e.activation(out=st[:, col:col + 1], in_=st[:, col:col + 1], func=AF.Sqrt,
                                               scale=1.0 / D, bias=epsb[:, 0:1]), reads=[t_st, t_eps], writes=[t_st])
            S.op("dve", lambda e: e.reciprocal(st[:, col:col + 1], st[:, col:col + 1]), reads=[t_st], writes=[t_st])

        evc = [0]

        def evac_copy(out_ap, out_tks, in_ap, in_tks):
            evc[0] += 1
            if evc[0] % 2 == 0:
                S.op("act", lambda e: e.activation(out=out_ap, in_=in_ap, func=AF.Copy), reads=in_tks, writes=out_tks)
            else:
                S.op("dve", lambda e: e.tensor_copy(out_ap, in_ap), reads=in_tks, writes=out_tks)

        def norm_T(x_src_ap, g_tk_ready, ncols_tok, tok_off):
            pass

        def fm_chunk(w_view, w_tk, kn, rhs_fn, rhs_tks, ncol_tok):
            ps, tps = next_pj()
            for k in range(kn):
                S.op("pe", lambda e, k=k: e.matmul(ps[:, 0:ncol_tok], w_view[:, k, :], rhs_fn(k),
                                                   start=(k == 0), stop=(k == kn - 1)),
                     reads=[w_tk] + rhs_tks, writes=[tps])
            return ps, tps

        for l in range(L):
            xsrc = x_d if l == 0 else xmid_d
            xreg = [Tk(f"xreg{l}_{g}") for g in range(NG)]
            if l > 0:
                for g in range(NG):
                    xreg[g].w = xreg_prev[g].w
            kreg = {t: [Tk(f"kreg{t}{l}_{g}") for g in range(NG)] for t in "as"}
            vreg = {t: [Tk(f"vreg{t}{l}_{g}") for g in range(NG)] for t in "as"}
            if l > 0:
                for t in "as":
                    for g in range(NG):
                        kreg[t][g].r = list(kreg_prev[t][g].r)
                        kreg[t][g].w = kreg_prev[t][g].w
                        vreg[t][g].r = list(vreg_prev[t][g].r)
                        vreg[t][g].w = vreg_prev[t][g].w

            for k3 in range(3):
                S.dma("sp", cw[:, :, k3], cw_d[l, k3].rearrange("(c p) -> p c", p=128), t_cw, writes=[t_cw],
                      allow_slow_non_contiguous=True)
            S.dma("sp", cbias[:], cb_d[l].rearrange("(c p) -> p c", p=128), t_cbias, writes=[t_cbias],
                  allow_slow_non_contiguous=True)
            S.op("dve", lambda e: e.memset(carry[:], 0.0), writes=[t_carry])
            S.op("dve", lambda e: e.memset(km32[:], 0.0), writes=[t_km32])
            S.op("dve", lambda e: e.memset(kmT[:], 0.0), writes=[t_kmT])

            if DBG == "consts":
                S.drain()
                return nc
            gbc = scb[:, 0:2048]
            S.dma("sp", gbc, mg_d[l:l + 1, :].partition_broadcast(128), t_scb, writes=[t_scb])
            for mt in range(2):
                xt = xall[:, 0:2048]
                S.dma("sp", xt, mem_d[mt * 128:(mt + 1) * 128, :], t_xq[0], writes=[t_xq[0], t_xq[1]])
                xs = kst[:, 0:2048]
                rmsnorm_rstd(xt, [t_xq[0], t_xq[1]], xs, [t_kst], mt)
                S.op("dve", lambda e, mt=mt: e.scalar_tensor_tensor(out=xs, in0=xt, scalar=st[:, mt:mt + 1], in1=gbc,
                                                                    op0=ALU.mult, op1=ALU.mult),
                     reads=[t_xq[0], t_xq[1], t_st, t_scb], writes=[t_kst])
                for k4 in range(4):
                    for kk in range(4):
                        k = k4 * 4 + kk
                        S.op("pe", lambda e, k=k, kk=kk: e.transpose(ptr[:, kk * 128:(kk + 1) * 128],
                                                                     xs[:, k * 128:(k + 1) * 128], ident[:]),
                             reads=[t_kst, t_ident], writes=[t_ptr])
                    evac_copy(xnT[:, k4 * 4:(k4 + 1) * 4, mt * 128:(mt + 1) * 128],
                              [t_xnT], ptr[:, 0:512].rearrange("p (a b) -> p a b", a=4), [t_ptr])
            wkv = wkv_d[l].rearrange("(k p) n -> p k n", p=128)
            for c in range(4):
                w_sb, w_tk = wload([(0, KC, 256, wkv[:, :, c * 256:(c + 1) * 256])])
                wv = wview(w_sb, 0, KC, 256)
                for cc in range(2):
                    ps, tps = fm_chunk(wv[:, :, cc * 128:(cc + 1) * 128], w_tk, KC,
                                       lambda k: xnT[:, k, 0:256], [t_xnT], 256)
                    evac_copy(mkT[:, c * 2 + cc, :], [t_mkT], ps[:, 0:256], [tps])
            for c in range(4):
                w_sb, w_tk = wload([(0, KC, 256, wkv[:, :, 1024 + c * 256:1024 + (c + 1) * 256])])
                wv = wview(w_sb, 0, KC, 256)
                for mt in range(2):
                    ps, tps = next_pj()
                    for k in range(KC):
                        S.op("pe", lambda e, k=k, mt=mt: e.matmul(ps[:, 0:256], xnT[:, k, mt * 128:(mt + 1) * 128],
                                                                  wv[:, k, :], start=(k == 0), stop=(k == KC - 1)),
                             reads=[t_xnT, w_tk], writes=[tps])
                    evac_copy(mv[:, mt, c * 256:(c + 1) * 256], [t_mv], ps[:, 0:256], [tps])

            if DBG == "memkv":
                S.drain()
                return nc
            for g in range(NG):
                tok0 = g * 512
                S.dma("sp", gbc, lng_d[l:l + 1, :].partition_broadcast(128), t_scb, writes=[t_scb])
                for t in range(4):
                    hx = t % 2
                    xt = xall[:, hx * 2048:(hx + 1) * 2048]
                    xtk = [t_xq[2 * hx], t_xq[2 * hx + 1]]
                    S.dma("sp", xt, xsrc[tok0 + t * 128: tok0 + (t + 1) * 128, :], xtk[0], reads=[xreg[g]], writes=xtk)
                    xs = kst[:, 0:2048]
                    rmsnorm_rstd(xt, xtk, xs, [t_kst], t)
                    S.op("dve", lambda e, t=t, xt=xt: e.scalar_tensor_tensor(out=xs, in0=xt, scalar=st[:, t:t + 1],
                                                                             in1=gbc, op0=ALU.mult, op1=ALU.mult),
                         reads=xtk + [t_st, t_scb], writes=[t_kst])
                    for k4 in range(4):
                        for kk in range(4):
                            k = k4 * 4 + kk
                            S.op("pe", lambda e, k=k, kk=kk: e.transpose(ptr[:, kk * 128:(kk + 1) * 128],
                                                                         xs[:, k * 128:(k + 1) * 128], ident[:]),
                                 reads=[t_kst, t_ident], writes=[t_ptr])
                        evac_copy(xnT[:, k4 * 4:(k4 + 1) * 4, t * 128:(t + 1) * 128],
                                  [t_xnT], ptr[:, 0:512].rearrange("p (a b) -> p a b", a=4), [t_ptr])

                if DBG == "stage0":
                    S.drain()
                    return nc
                for typ, ck, cv in (("a", C_AK, C_AV), ("s", C_SK, C_SV)):
                    kst3 = kst[:].rearrange("p (h t) -> p h t", h=8)
                    for c in range(4):
                        w_sb, w_tk = wload([(0, KC, 256, win_cols(l, ck + c * 256, 256))])
                        wv = wview(w_sb, 0, KC, 256)
                        for cc in range(2):
                            h = c * 2 + cc
                            ps, tps = fm_chunk(wv[:, :, cc * 128:(cc + 1) * 128], w_tk, KC,
                                               lambda k: xnT[:, k, :], [t_xnT], 512)
                            S.op("act", lambda e, h=h, ps=ps: e.activation(out=kst3[:, h, :], in_=ps[:], func=AF.Copy),
                                 reads=[tps], writes=[t_kst])
                            if typ == "a":
                                S.op("dve", lambda e, h=h: e.tensor_reduce(
                                    out=km32[:, h, 2 * g:2 * g + 2], in_=kst3[:, h, :].rearrange("p (b t) -> p b t", b=2),
                                    axis=AX.X, op=ALU.add), reads=[t_kst], writes=[t_km32])
                    S.dma("sp", kT_d[typ][:, :, tok0:tok0 + 512].rearrange("h d t -> d h t"), kst3, t_kst,
                          reads=[t_kst], writes=[kreg[typ][g]])
                    if DBG == "kvA":
                        S.drain()
                        return nc
                    if typ == "a":
                        S.op("dve", lambda e: e.tensor_scalar(kmT[:, :, 2 * g:2 * g + 2], km32[:, :, 2 * g:2 * g + 2],
                                                              1.0 / 256.0, None, op0=ALU.mult),
                             reads=[t_km32], writes=[t_kmT])
                    for c in range(4):
                        w_sb, w_tk = wload([(0, KC, 256, win_cols(l, cv + c * 256, 256))])
                        wv = wview(w_sb, 0, KC, 256)
                        for t in range(4):
                            ps, tps = next_pj()
                            for k in range(KC):
                                S.op("pe", lambda e, k=k, t=t, ps=ps: e.matmul(
                                    ps[:, 0:256], xnT[:, k, t * 128:(t + 1) * 128], wv[:, k, :],
                                    start=(k == 0), stop=(k == KC - 1)), reads=[t_xnT, w_tk], writes=[tps])
                            evac_copy(vst[:, t, c * 256:(c + 1) * 256], [t_vst], ps[:, 0:256], [tps])
                    S.dma("sp", v_d[typ][tok0:tok0 + 512, :].rearrange("(t p) c -> p t c", p=128), vst[:], t_vst,
                          reads=[t_vst], writes=[vreg[typ][g]])
                if DBG == "kvC":
                    S.drain()
                    return nc
                w_sb, w_tk = wload([(0, KC, 256, win_cols(l, C_IQ, 256))])
                wv = wview(w_sb, 0, KC, 256)
                for cc in range(2):
                    ps, tps = fm_chunk(wv[:, :, cc * 128:(cc + 1) * 128], w_tk, KC, lambda k: xnT[:, k, :], [t_xnT], 512)
                    evac_copy(qiT[:, cc, :], [t_qiT], ps[:], [tps])
                w_sb, w_tk = wload([(0, KC, 256, win_cols(l, C_IK - 64, 256))])
                wv = wview(w_sb, 0, KC, 256)
                ps, tps = fm_chunk(wv[:, :, 64:192], w_tk, KC, lambda k: xnT[:, k, :], [t_xnT], 512)
                S.op("act", lambda e, ps=ps: e.activation(out=kiT[0:64, tok0:tok0 + 512], in_=ps[0:64, :], func=AF.Copy),
                     reads=[tps], writes=[t_kiT])
                S.op("act", lambda e, ps=ps: e.activation(out=iwT[64:96, :], in_=ps[64:96, :], func=AF.Copy),
                     reads=[tps], writes=[t_iwT])
                ps, tps = fm_chunk(wv[:, :, 0:128], w_tk, KC, lambda k: xnT[:, k, :], [t_xnT], 512)
                S.op("act", lambda e, ps=ps: e.activation(out=kiT[64:128, tok0:tok0 + 512], in_=ps[64:128, :], func=AF.Copy),
                     reads=[tps], writes=[t_kiT])
                ps, tps = next_pj()
                for t in range(4):
                    S.op("pe", lambda e, t=t, ps=ps: e.matmul(ps[:, t * 16:(t + 1) * 16], iwT[64:96, t * 128:(t + 1) * 128],
                                                             sel[64:96, :], start=True, stop=True),
                         reads=[t_iwT, t_sel], writes=[tps])
                S.op("dve", lambda e, ps=ps: e.tensor_scalar(wI[:], ps[:, 0:64].rearrange("p (a b) -> p a b", a=4)[:, :, 0:4],
                                                             (64.0 ** -0.5) * 0.5, None, op0=ALU.mult),
                     reads=[tps], writes=[t_wI])
                if DBG == "kv":
                    S.drain()
                    return nc
                def kv_loader(typ, h, nkt, lookahead=2):
                    nch = (nkt + 7) // 8
                    state = {"issued": 0}

                    def issue(c):
                        slot = (kvc[0] + c) % NR
                        k_sb, k_tk = kring[slot]
                        v_sb, v_tk = vring[slot]
                        k0 = c * 1024
                        n = min(1024, nkt * 128 - k0)
                        gs = sorted(set((k0 + i * 128) // 512 for i in range(n // 128)))
                        S.dma("sp", k_sb[:, 0:n], kT_d[typ][h, :, k0:k0 + n], k_tk,
                              reads=[kreg[typ][gg] for gg in gs], writes=[k_tk])
                        S.dma("sp", v_sb[:, 0:n // 128, :],
                              v_d[typ][k0:k0 + n, h * 128:(h + 1) * 128].rearrange("(a p) d -> p a d", p=128), v_tk,
                              reads=[vreg[typ][gg] for gg in gs], writes=[v_tk])

                    def get(kt):
                        c = kt // 8
                        while state["issued"] < min(nch, c + 1 + lookahead):
                            issue(state["issued"])
                            state["issued"] += 1
                        slot = (kvc[0] + c) % NR
                        k_sb, k_tk = kring[slot]
                        v_sb, v_tk = vring[slot]
                        j = kt % 8
                        return k_sb[:, j * 128:(j + 1) * 128], k_tk, v_sb[:, j, :], v_tk

                    def done():
                        kvc[0] += nch
                    return get, done

                def attention(nkt, qcols, q_ap, q_tk, get_kv, mask_fn, scale):
                    pend = []
                    for kt in range(nkt + 1):
                        if kt < nkt:
                            c0, masks = mask_fn(kt)
                            sp_, tsp = stp[kt % 2]
                            k_ap, k_tk, v_ap, v_tk = get_kv(kt)
                            nm_ = len(masks)
                            S.op("pe", lambda e, sp_=sp_, k_ap=k_ap, c0=c0, nm_=nm_: e.matmul(
                                sp_[:, c0:qcols], k_ap, q_ap[:, c0:qcols], start=True, stop=(nm_ == 0)),
                                reads=[k_tk, q_tk], writes=[tsp])
                            for i, (ml, mr, lo, hi, mtks) in enumerate(masks):
                                S.op("pe", lambda e, sp_=sp_, ml=ml, mr=mr, lo=lo, hi=hi, i=i, nm_=nm_: e.matmul(
                                    sp_[:, lo:hi], ml, mr, start=False, stop=(i == nm_ - 1)),
                                    reads=mtks, writes=[tsp])
                            pend.append((kt, c0, sp_, tsp, v_ap, v_tk))
                        if kt > 0:
                            pk, c0, sp_, tsp, v_ap, v_tk = pend.pop(0)
                            p_sb, p_tk = pT[pk % 3]
                            S.op("act", lambda e, p_sb=p_sb, sp_=sp_, c0=c0: e.activation(
                                out=p_sb[:, c0:qcols], in_=sp_[:, c0:qcols], func=AF.Exp, scale=scale),
                                reads=[tsp], writes=[p_tk])
                            S.op("pe", lambda e, v_ap=v_ap, p_sb=p_sb, c0=c0, pk=pk: e.matmul(
                                ot[:, c0:qcols], v_ap, p_sb[:, c0:qcols], start=(pk == 0), stop=(pk == nkt - 1)),
                                reads=[v_tk, p_tk], writes=[t_ot])
                            S.op("pe", lambda e, p_sb=p_sb, c0=c0, pk=pk: e.matmul(
                                dn[:, c0:qcols], ones[:], p_sb[:, c0:qcols], start=(pk == 0), stop=(pk == nkt - 1)),
                                reads=[t_ones, p_tk], writes=[t_dn])

                def normalize(o_ap, o_tk, qcols, sg_ap, sg_tk, y_ap, y_tk):
                    rden, t_rden = Fs[3]
                    t1, t_t1 = Fs[4]
                    S.op("dve", lambda e: e.reciprocal(rden[:, 0:qcols], dn[:, 0:qcols]), reads=[t_dn], writes=[t_rden])
                    S.op("dve", lambda e: e.tensor_tensor(out=t1[:, 0:qcols], in0=o_ap, in1=rden[:, 0:qcols], op=ALU.mult),
                         reads=[o_tk, t_rden], writes=[t_t1])
                    S.op("dve", lambda e: e.tensor_tensor(out=y_ap, in0=t1[:, 0:qcols], in1=sg_ap, op=ALU.mult),
                         reads=[t_t1, sg_tk], writes=[y_tk])

                for h in range(8):
                    q_sb, q_tk = qh[h % 2]
                    g_sb, g_tk = sgh[h % 2]
                    w_sb, w_tk = wload([(0, KC, 256, win_cols(l, C_AQ + h * 128, 128, reps=2, stride=C_AG - C_AQ))])
                    wv = wview(w_sb, 0, KC, 256)
                    ps, tps = fm_chunk(wv[:, :, 0:128], w_tk, KC, lambda k: xnT[:, k, :], [t_xnT], 512)
                    S.op("act", lambda e, ps=ps, q_sb=q_sb: e.activation(out=q_sb[:], in_=ps[:], func=AF.Copy),
                         reads=[tps], writes=[q_tk])
                    ps, tps = fm_chunk(wv[:, :, 128:256], w_tk, KC, lambda k: xnT[:, k, :], [t_xnT], 512)
                    S.op("act", lambda e, ps=ps, g_sb=g_sb: e.activation(out=g_sb[:], in_=ps[:], func=AF.Silu),
                         reads=[tps], writes=[g_tk])
                    ps, tps = next_pj()
                    for t in range(4):
                        S.op("pe", lambda e, t=t, ps=ps, q_sb=q_sb: e.matmul(
                            ps[:, t * 16:(t + 1) * 16], q_sb[:, t * 128:(t + 1) * 128], kmT[:, h, :], start=True, stop=True),
                            reads=[q_tk, t_kmT], writes=[tps])
                    S.op("dve", lambda e, ps=ps: e.tensor_copy(bsm[:].rearrange("p a b -> p (a b)"), ps[:, 0:64]),
                         reads=[tps], writes=[t_bsm])
                    S.op("dve", lambda e: e.memset(bsm[:, 0:2, 2 * g:16], -BIG), writes=[t_bsm])
                    S.op("dve", lambda e: e.memset(bsm[:, 2:4, 2 * g + 1:16], -BIG), writes=[t_bsm])
                    for t in range(4):
                        S.op("dve", lambda e, t=t: e.max(out=m8[:, t, :], in_=bsm[:, t, :]), reads=[t_bsm], writes=[t_m8])
                    for t in range(4):
                        S.op("dve", lambda e, t=t: e.tensor_scalar(mbf[:, t, :], bsm[:, t, :], m8[:, t, 2:3], NEG,
                                                                  op0=ALU.is_lt, op1=ALU.mult),
                             reads=[t_bsm, t_m8], writes=[t_mbf])
                    for t in range(4):
                        S.op("pe", lambda e, t=t: e.transpose(ptr[0:16, t * 128:(t + 1) * 128], mbf[:, t, :], ident[:]),
                             reads=[t_mbf, t_ident], writes=[t_ptr])
                    S.op("dve", lambda e: e.tensor_copy(mbT[:, :], ptr[0:16, 0:512]), reads=[t_ptr], writes=[t_mbT])

                    def moba_mask(kt):
                        a = kt - 4 * g
                        c0 = 128 * max(a, 0)
                        i = kt // 2
                        masks = []
                        if i < 2 * g:
                            masks.append((eone[:, i * 128:(i + 1) * 128], mbT[:, 0:512], 0, 512, [t_eone, t_mbT]))
                        elif i == 2 * g:
                            masks.append((eone[:, i * 128:(i + 1) * 128], mbT[:, 256:512], 256, 512, [t_eone, t_mbT]))
                            masks.append((ident[:], tri[:], a * 128, (a + 1) * 128, [t_ident, t_tri]))
                        else:
                            masks.append((ident[:], tri[:], a * 128, (a + 1) * 128, [t_ident, t_tri]))
                        return c0, masks

                    get_kv, kv_done = kv_loader("a", h, 4 * g + 4)
                    attention(4 * g + 4, 512, q_sb, q_tk, get_kv, moba_mask, 128.0 ** -0.5)
                    kv_done()
                    normalize(ot[:, 0:512], t_ot, 512, g_sb[:], g_tk, yT[0][0][:, h, :], yT[0][1])

                if DBG == "moba":
                    S.drain()
                    return nc
                for j in range(8):
                    ccs, t_ccs = Fs[0]
                    u, t_u = Fs[1]
                    cacc, t_cacc = Fs[2]
                    w_sb, w_tk = wload([(0, KC, 256, win_cols(l, C_CC + j * 128, 128, reps=2, stride=C_CH - C_CC))])
                    wv = wview(w_sb, 0, KC, 256)
                    ps, tps = fm_chunk(wv[:, :, 0:128], w_tk, KC, lambda k: xnT[:, k, :], [t_xnT], 512)
                    S.op("act", lambda e, ps=ps: e.activation(out=ccs[:, 0:512], in_=ps[:], func=AF.Copy),
                         reads=[tps], writes=[t_ccs])
                    ps, tps = fm_chunk(wv[:, :, 128:256], w_tk, KC, lambda k: xnT[:, k, :], [t_xnT], 512)
                    S.op("dve", lambda e, ps=ps: e.tensor_tensor(out=u[:, 2:514], in0=ps[:], in1=ccs[:, 0:512], op=ALU.mult),
                         reads=[tps, t_ccs], writes=[t_u])
                    S.op("dve", lambda e, j=j: e.tensor_copy(u[:, 0:2], carry[:, j, :]), reads=[t_carry], writes=[t_u])
                    S.op("act", lambda e, j=j: e.activation(out=cacc[:, 0:512], in_=u[:, 2:514], func=AF.Identity,
                                                            scale=cw[:, j, 2:3], bias=cbias[:, j:j + 1]),
                         reads=[t_u, t_cw, t_cbias], writes=[t_cacc])
                    S.op("dve", lambda e, j=j: e.scalar_tensor_tensor(out=cacc[:, 0:512], in0=u[:, 1:513], scalar=cw[:, j, 1:2],
                                                                      in1=cacc[:, 0:512], op0=ALU.mult, op1=ALU.add),
                         reads=[t_u, t_cw, t_cacc], writes=[t_cacc])
                    S.op("dve", lambda e, j=j: e.scalar_tensor_tensor(out=cacc[:, 0:512], in0=u[:, 0:512], scalar=cw[:, j, 0:1],
                                                                      in1=cacc[:, 0:512], op0=ALU.mult, op1=ALU.add),
                         reads=[t_u, t_cw, t_cacc], writes=[t_cacc])
                    S.op("dve", lambda e, j=j: e.tensor_copy(carry[:, j, :], u[:, 512:514]), reads=[t_u], writes=[t_carry])
                    w_sb, w_tk = wload([(0, KC, 256, win_cols(l, C_CB + j * 128, 128, reps=2, stride=C_CG - C_CB))])
                    wv = wview(w_sb, 0, KC, 256)
                    ps, tps = fm_chunk(wv[:, :, 128:256], w_tk, KC, lambda k: xnT[:, k, :], [t_xnT], 512)
                    S.op("act", lambda e, ps=ps: e.activation(out=sgc[:], in_=ps[:], func=AF.Silu), reads=[tps], writes=[t_sgc])
                    ps, tps = fm_chunk(wv[:, :, 0:128], w_tk, KC, lambda k: xnT[:, k, :], [t_xnT], 512)
                    S.op("dve", lambda e, ps=ps: e.tensor_tensor(out=cacc[:, 0:512], in0=ps[:], in1=cacc[:, 0:512], op=ALU.mult),
                         reads=[tps, t_cacc], writes=[t_cacc])
                    S.op("dve", lambda e, j=j: e.tensor_tensor(out=yT[1][0][:, j, :], in0=cacc[:, 0:512], in1=sgc[:], op=ALU.mult),
                         reads=[t_cacc, t_sgc], writes=[yT[1][1]])

                if DBG == "conv":
                    S.drain()
                    return nc
                for hh in range(2):
                    for tt in range(2):
                        t = hh * 2 + tt
                        Q = 4 * g + t
                        nk = (Q + 1) * 128
                        mb_ap = mball[:, tt * MBH: tt * MBH + nk]
                        nchunk = (nk + 511) // 512
                        for c in range(nchunk):
                            wdt = min(512, nk - c * 512)
                            for j in range(4):
                                pb = 64 * (j % 2)
                                ps, tps = next_pj()
                                rl, t_rl = Fs[j % 2]
                                S.op("pe", lambda e, ps=ps, pb=pb, j=j, c=c, wdt=wdt, t=t: e.matmul(
                                    ps[:, 0:wdt], qiT[pb:pb + 64, j // 2, t * 128:(t + 1) * 128],
                                    kiT[pb:pb + 64, c * 512:c * 512 + wdt], start=True, stop=True),
                                    reads=[t_qiT, t_kiT], writes=[tps])
                                S.op("act", lambda e, ps=ps, rl=rl, wdt=wdt: e.activation(out=rl[:, 0:wdt], in_=ps[:, 0:wdt], func=AF.Relu),
                                     reads=[tps], writes=[t_rl])
                                if j == 0:
                                    S.op("dve", lambda e, rl=rl, c=c, wdt=wdt, t=t: e.tensor_scalar(
                                        scb[:, c * 512:c * 512 + wdt], rl[:, 0:wdt], wI[:, t, 0:1], None, op0=ALU.mult),
                                        reads=[t_rl, t_wI], writes=[t_scb])
                                else:
                                    S.op("dve", lambda e, rl=rl, c=c, wdt=wdt, t=t, j=j: e.scalar_tensor_tensor(
                                        out=scb[:, c * 512:c * 512 + wdt], in0=rl[:, 0:wdt], scalar=wI[:, t, j:j + 1],
                                        in1=scb[:, c * 512:c * 512 + wdt], op0=ALU.mult, op1=ALU.add),
                                        reads=[t_rl, t_wI, t_scb], writes=[t_scb])
                        S.op("dve", lambda e, nk=nk: e.tensor_reduce(out=bis[:, 0:1], in_=scb[:, 0:nk], axis=AX.X, op=ALU.min),
                             reads=[t_scb], writes=[t_bis])
                        S.op("dve", lambda e, nk=nk: e.tensor_reduce(out=bis[:, 1:2], in_=scb[:, 0:nk], axis=AX.X, op=ALU.max),
                             reads=[t_scb], writes=[t_bis])
                        S.op("dve", lambda e, Q=Q: e.tensor_tensor(out=scb[:, Q * 128:(Q + 1) * 128], in0=scb[:, Q * 128:(Q + 1) * 128],
                                                                   in1=triq[:], op=ALU.add), reads=[t_scb, t_triq], writes=[t_scb])
                        S.op("dve", lambda e: e.tensor_tensor(out=bis[:, 2:3], in0=bis[:, 1:2], in1=bis[:, 0:1], op=ALU.subtract),
                             reads=[t_bis], writes=[t_bis])
                        S.op("dve", lambda e: e.tensor_scalar(bis[:, 2:3], bis[:, 2:3], 1.0001, 1e-6, op0=ALU.mult, op1=ALU.add),
                             reads=[t_bis], writes=[t_bis])
                        S.op("dve", lambda e: e.tensor_scalar(halves[:], pw2[:], bis[:, 2:3], None, op0=ALU.mult),
                             reads=[t_pw2, t_bis], writes=[t_halves])
                        S.op("dve", lambda e: e.scalar_tensor_tensor(out=nm[:, 0:1], in0=bis[:, 0:1], scalar=-1.0, in1=halves[:, 0:1],
                                                                     op0=ALU.mult, op1=ALU.subtract),
                             reads=[t_bis, t_halves], writes=[t_nm])
                        S.op("dve", lambda e: e.memset(ssum[:], 0.0), writes=[t_ssum])
                        cthr = float(2 * min(TOPK, T // 4) - nk)
                        S.op("dve", lambda e: e.memset(cneg[:], 0.5 - cthr), writes=[t_cneg])
                        S.op("dve", lambda e: e.tensor_scalar(nhh[:], halves[:], -0.5, None, op0=ALU.mult),
                             reads=[t_halves], writes=[t_nhh])
                        for it in range(NIT):
                            S.op("act", lambda e, it=it, nk=nk: e.activation(out=kst[:, 0:nk], in_=scb[:, 0:nk], func=AF.Sign,
                                                                             bias=nm[:, it:it + 1], scale=1.0,
                                                                             accum_out=ssum[:, it:it + 1]),
                                 reads=[t_scb, t_nm], writes=[t_kst, t_ssum])
                            S.op("act", lambda e, it=it: e.activation(out=dtmp[:, it:it + 1], in_=ssum[:, it:it + 1], func=AF.Sign,
                                                                      bias=cneg[:, 0:1], scale=1.0),
                                 reads=[t_ssum, t_cneg], writes=[t_dtmp])
                            S.op("act", lambda e, it=it: e.activation(out=nm[:, it + 1:it + 2], in_=dtmp[:, it:it + 1], func=AF.Identity,
                                                                      scale=nhh[:, it:it + 1], bias=nm[:, it:it + 1]),
                                 reads=[t_dtmp, t_nhh, t_nm], writes=[t_nm])
                        S.op("dve", lambda e: e.scalar_tensor_tensor(out=bis[:, 3:4], in0=nm[:, NIT:NIT + 1], scalar=-1.0,
                                                                     in1=halves[:, NIT + 1:NIT + 2], op0=ALU.mult, op1=ALU.subtract),
                             reads=[t_nm, t_halves], writes=[t_bis])
                        S.op("dve", lambda e, mb_ap=mb_ap, nk=nk: e.tensor_scalar(mb_ap, scb[:, 0:nk], bis[:, 3:4], NEG,
                                                                                 op0=ALU.is_lt, op1=ALU.mult),
                             reads=[t_scb, t_bis], writes=[t_mb[tt]])
                    nkt = 4 * g + 2 * hh + 2
                    for h in range(8):
                        q_sb, q_tk = qh[h % 2]
                        g_sb, g_tk = sgh[h % 2]
                        w_sb, w_tk = wload([(0, KC, 256, win_cols(l, C_SQ + h * 128, 128, reps=2, stride=C_SG - C_SQ))])
                        wv = wview(w_sb, 0, KC, 256)
                        ps, tps = fm_chunk(wv[:, :, 0:128], w_tk, KC, lambda k: xnT[:, k, hh * 256:(hh + 1) * 256], [t_xnT], 256)
                        S.op("act", lambda e, ps=ps, q_sb=q_sb: e.activation(out=q_sb[:, 0:256], in_=ps[:, 0:256], func=AF.Copy),
                             reads=[tps], writes=[q_tk])
                        ps, tps = fm_chunk(wv[:, :, 128:256], w_tk, KC, lambda k: xnT[:, k, hh * 256:(hh + 1) * 256], [t_xnT], 256)
                        S.op("act", lambda e, ps=ps, g_sb=g_sb: e.activation(out=g_sb[:, 0:256], in_=ps[:, 0:256], func=AF.Silu),
                             reads=[tps], writes=[g_tk])

                        def dsa_mask(kt):
                            a = kt - (4 * g + 2 * hh)
                            c0 = 128 * max(a, 0)
                            masks = []
                            for tt in range(2):
                                if kt <= 4 * g + 2 * hh + tt:
                                    masks.append((mball[:, tt * MBH + kt * 128: tt * MBH + (kt + 1) * 128], ident[:],
                                                  tt * 128, (tt + 1) * 128, [t_mb[tt], t_ident]))
                            return c0, masks

                        get_kv, kv_done = kv_loader("s", h, nkt)
                        attention(nkt, 256, q_sb, q_tk, get_kv, dsa_mask, 128.0 ** -0.5)
                        kv_done()
                        normalize(ot[:, 0:256], t_ot, 256, g_sb[:, 0:256], g_tk,
                                  yT[2][0][:, h, hh * 256:(hh + 1) * 256], yT[2][1])

                if DBG == "dsa":
                    S.drain()
                    return nc
                for h in range(4):
                    for dc in range(2):
                        q_sb, q_tk = qh[dc]
                        g_sb, g_tk = sgh[dc]
                        cidx = h * 2 + dc
                        w_sb, w_tk = wload([(0, KC, 256, win_cols(l, C_MQ + cidx * 128, 128, reps=2, stride=C_MG - C_MQ))])
                        wv = wview(w_sb, 0, KC, 256)
                        ps, tps = fm_chunk(wv[:, :, 0:128], w_tk, KC, lambda k: xnT[:, k, :], [t_xnT], 512)
                        S.op("act", lambda e, ps=ps, q_sb=q_sb: e.activation(out=q_sb[:], in_=ps[:], func=AF.Copy),
                             reads=[tps], writes=[q_tk])
                        ps, tps = fm_chunk(wv[:, :, 128:256], w_tk, KC, lambda k: xnT[:, k, :], [t_xnT], 512)
                        S.op("act", lambda e, ps=ps, g_sb=g_sb: e.activation(out=g_sb[:], in_=ps[:], func=AF.Silu),
                             reads=[tps], writes=[g_tk])
                    pts = []
                    for mt in range(2):
                        sp_, tsp = stp[mt]
                        for dc in range(2):
                            S.op("pe", lambda e, sp_=sp_, mt=mt, dc=dc: e.matmul(
                                sp_[:], mkT[:, h * 2 + dc, mt * 128:(mt + 1) * 128], qh[dc][0][:], start=(dc == 0), stop=(dc == 1)),
                                reads=[t_mkT, qh[dc][1]], writes=[tsp])
                        p_sb, p_tk = pT[mt]
                        S.op("act", lambda e, sp_=sp_, p_sb=p_sb: e.activation(out=p_sb[:], in_=sp_[:], func=AF.Exp, scale=256.0 ** -0.5),
                             reads=[tsp], writes=[p_tk])
                        pts.append((p_sb, p_tk))
                    o2, t_o2 = pj[0]
                    for mt in range(2):
                        p_sb, p_tk = pts[mt]
                        S.op("pe", lambda e, mt=mt, p_sb=p_sb: e.matmul(ot[:], mv[:, mt, h * 256:h * 256 + 128], p_sb[:],
                                                                        start=(mt == 0), stop=(mt == 1)),
                             reads=[t_mv, p_tk], writes=[t_ot])
                        S.op("pe", lambda e, mt=mt, p_sb=p_sb: e.matmul(o2[:], mv[:, mt, h * 256 + 128:h * 256 + 256], p_sb[:],
                                                                        start=(mt == 0), stop=(mt == 1)),
                             reads=[t_mv, p_tk], writes=[t_o2])
                        S.op("pe", lambda e, mt=mt, p_sb=p_sb: e.matmul(dn[:], ones[:], p_sb[:], start=(mt == 0), stop=(mt == 1)),
                             reads=[t_ones, p_tk], writes=[t_dn])
                    normalize(ot[:], t_ot, 512, sgh[0][0][:], sgh[0][1], yT[3][0][:, h * 2, :], yT[3][1])
                    normalize(o2[:], t_o2, 512, sgh[1][0][:], sgh[1][1], yT[3][0][:, h * 2 + 1, :], yT[3][1])

                if DBG == "mem":
                    S.drain()
                    return nc
                mrg = mball[:, 0:8192].rearrange("p (c t) -> p c t", c=16)
                t_mrg = t_mb
                for oc2 in range(8):
                    for br in range(4):
                        w_sb, w_tk = wload([(0, KC, 256, win_cols(l, C_R + 2048 * br + 256 * oc2, 256))])
                        wr = wview(w_sb, 0, KC, 256)
                        w_sb2, w_tk2 = wload([(0, 8, 256,
                                               wbr_d[l, br].rearrange("(k p) n -> p k n", p=128)[:, :, oc2 * 256:(oc2 + 1) * 256])])
                        wb = wview(w_sb2, 0, 8, 256)
                        y_sb, y_tk = yT[br]
                        for c2 in range(2):
                            oc = oc2 * 2 + c2
                            acc, t_acc = Fs[c2]
                            tmp, t_tmp = Fs[2 + c2]
                            sg_sb, sg_tk = sgt[c2]
                            ps, tps = fm_chunk(wr[:, :, c2 * 128:(c2 + 1) * 128], w_tk, KC, lambda k: xnT[:, k, :], [t_xnT], 512)
                            S.op("act", lambda e, ps=ps, sg_sb=sg_sb: e.activation(out=sg_sb[:], in_=ps[:], func=AF.Sigmoid),
                                 reads=[tps], writes=[sg_tk])
                            ps, tps = fm_chunk(wb[:, :, c2 * 128:(c2 + 1) * 128], w_tk2, 8, lambda k, y_sb=y_sb: y_sb[:, k, :], [y_tk], 512)
                            if br == 0:
                                S.op("dve", lambda e, ps=ps, sg_sb=sg_sb, acc=acc: e.tensor_tensor(
                                    out=acc[:, 0:512], in0=ps[:], in1=sg_sb[:], op=ALU.mult), reads=[tps, sg_tk], writes=[t_acc])
                            else:
                                S.op("dve", lambda e, ps=ps, sg_sb=sg_sb, tmp=tmp: e.tensor_tensor(
                                    out=tmp[:, 0:512], in0=ps[:], in1=sg_sb[:], op=ALU.mult), reads=[tps, sg_tk], writes=[t_tmp])
                                if br < 3:
                                    S.op("dve", lambda e, tmp=tmp, acc=acc: e.tensor_tensor(
                                        out=acc[:, 0:512], in0=acc[:, 0:512], in1=tmp[:, 0:512], op=ALU.add),
                                        reads=[t_acc, t_tmp], writes=[t_acc])
                                else:
                                    S.op("dve", lambda e, tmp=tmp, acc=acc, oc=oc: e.tensor_tensor(
                                        out=mrg[:, oc, :], in0=acc[:, 0:512], in1=tmp[:, 0:512], op=ALU.add),
                                        reads=[t_acc, t_tmp], writes=[t_mb[0], t_mb[1]])
                if DBG == "merge":
                    S.drain()
                    return nc
                wo = wout_d[l].rearrange("(k p) n -> p k n", p=128)
                for cb in range(8):
                    w_sb, w_tk = wload([(0, KC, 256, wo[:, :, cb * 256:(cb + 1) * 256])])
                    wv = wview(w_sb, 0, KC, 256)
                    xr = xall[:, (cb % 2) * 1024:(cb % 2 + 1) * 1024].rearrange("p (t c) -> p t c", t=4)
                    t_xr = t_xq[cb % 2]
                    xo = xall[:, 2048 + (cb % 2) * 1024: 2048 + (cb % 2 + 1) * 1024].rearrange("p (t c) -> p t c", t=4)
                    t_xo = t_xq[2 + cb % 2]
                    S.dma("sp", xr, xsrc[tok0:tok0 + 512, cb * 256:(cb + 1) * 256].rearrange("(t p) c -> p t c", p=128),
                          t_xr, reads=[xreg[g]], writes=[t_xr])
                    for t in range(4):
                        ps, tps = next_pj()
                        for k in range(KC):
                            S.op("pe", lambda e, k=k, t=t, ps=ps: e.matmul(
                                ps[:, 0:256], mrg[:, k, t * 128:(t + 1) * 128], wv[:, k, :], start=(k == 0), stop=(k == KC - 1)),
                                reads=[t_mb[0], t_mb[1], w_tk], writes=[tps])
                        S.op("dve", lambda e, t=t, ps=ps, xo=xo, xr=xr: e.tensor_tensor(
                            out=xo[:, t, :], in0=ps[:, 0:256], in1=xr[:, t, :], op=ALU.add),
                            reads=[tps, t_xr], writes=[t_xo])
                    S.dma("sp", xmid_d[tok0:tok0 + 512, cb * 256:(cb + 1) * 256].rearrange("(t p) c -> p t c", p=128), xo,
                          t_xo, reads=[t_xo], writes=[xreg[g]] if l > 0 or True else [])
            xreg_prev = xreg
            kreg_prev = kreg
            vreg_prev = vreg

        gbc = scb[:, 0:2048]
        S.dma("sp", gbc, fg_d.partition_broadcast(128), t_scb, writes=[t_scb])
        for tt in range(NT):
            hx = tt % 2
            xt = xall[:, hx * 2048:(hx + 1) * 2048]
            xtk = [t_xq[2 * hx], t_xq[2 * hx + 1]]
            S.dma("sp", xt, xmid_d[tt * 128:(tt + 1) * 128, :], xtk[0], reads=[xreg_prev[tt // 4]], writes=xtk)
            rmsnorm_rstd(xt, xtk, kst[:, 0:2048], [t_kst], tt % 8)
            S.op("dve", lambda e, xt=xt, c=tt % 8: e.scalar_tensor_tensor(out=xt, in0=xt, scalar=st[:, c:c + 1], in1=gbc,
                                                                          op0=ALU.mult, op1=ALU.mult),
                 reads=xtk + [t_st, t_scb], writes=xtk)
            S.dma("sp", y_d[tt * 128:(tt + 1) * 128, :], xt, xtk[1], reads=xtk, writes=[])
        S.final_wait("sp", t_xq)
        S.drain()
    return nc


kvc = [0]
DBG = None


def _consts():
    k = np.arange(128)
    tri = np.where(k[None, :] >= k[:, None], 0.0, NEG).astype(np.float32)
    triq = np.where(k[None, :] <= k[:, None], 0.0, -BIG).astype(np.float32)
    eone = np.zeros((16, 2048), np.float32)
    for i in range(16):
        eone[i, i * 128:(i + 1) * 128] = 1.0
    pw2 = np.zeros((128, NIT + 2), np.float32)
    for i in range(NIT + 1):
        pw2[:, i] = 2.0 ** -(i + 1)
    pw2[:, NIT + 1] = 1.25 * 2.0 ** -(NIT + 1)
    selm = np.zeros((128, 16), np.float32)
    for j in range(4):
        selm[64 + j, j] = 1.0
    return {"c_sel": selm, "c_ident": np.eye(128, dtype=np.float32), "c_tri": tri, "c_triq": triq, "c_eone": eone, "c_pw2": pw2}


_CACHE = {}


def run(inputs, T, L, n_cores, batch_of_core):
    key = (T, L)
    if key not in _CACHE:
        kvc[0] = 0
        _CACHE[key] = build(T, L)
    nc = _CACHE[key]
    cst = _consts()
    f = lambda a: np.ascontiguousarray(np.asarray(a, dtype=np.float32))
    shared = {
        "ln_g": f(inputs["ln_g"]), "w_in": f(inputs["w_in"]), "conv_w": f(inputs["conv_w"]),
        "conv_b": f(inputs["conv_b"]), "mem_ln_g": f(inputs["mem_ln_g"]), "w_mem_kv": f(inputs["w_mem_kv"]),
        "w_branch": f(inputs["w_branch"]), "w_out": f(inputs["w_out"]),
        "final_g": f(inputs["final_g"]).reshape(1, D), **cst,
    }
    in_maps = []
    zshared = None
    for c in range(n_cores):
        b = batch_of_core[c]
        if b is None:
            if zshared is None:
                zshared = {k: (v if k.startswith("c_") else np.zeros_like(v)) for k, v in shared.items()}
                zshared["x"] = np.zeros((T, D), np.float32)
                zshared["mem"] = np.zeros((256, D), np.float32)
            in_maps.append(zshared)
            continue
        m = dict(shared)
        m["x"] = f(inputs["x"][b])
        m["mem"] = f(inputs["mem"][b])
        in_maps.append(m)
    res = run_bass_kernel_spmd(nc, in_maps, core_ids=list(range(n_cores)))
    return [r["y"] for r in res.results]


def kernel(x, mem, ln_g, w_in, conv_w, conv_b, mem_ln_g, w_mem_kv, w_branch, w_out, final_g):
    inputs = dict(x=x, mem=mem, ln_g=ln_g, w_in=w_in, conv_w=conv_w, conv_b=conv_b, mem_ln_g=mem_ln_g,
                  w_mem_kv=w_mem_kv, w_branch=w_branch, w_out=w_out, final_g=final_g)
    B, T, _ = np.asarray(x).shape
    L = np.asarray(ln_g).shape[0]
    owner = {0: 0, 1: 1, 4: 2, 5: 3}
    outs = run(inputs, T, L, 8, [owner.get(c) if owner.get(c, B) < B else None for c in range(8)])
    core_of = {b: c for c, b in owner.items()}
    return np.stack([outs[core_of[b]] for b in range(B)], axis=0).astype(np.float32)
```

```python
import os
import numpy as np
import concourse.bass as bass
import concourse.mybir as mybir
from concourse.bass_utils import run_bass_kernel_spmd
from contextlib import ExitStack

F32 = mybir.dt.float32
BF16 = mybir.dt.bfloat16
AF = mybir.ActivationFunctionType
ALU = mybir.AluOpType
AX = mybir.AxisListType

D = 2048
KC = 16
BW = 1024
INC = 22852
NEG = -30000.0
BIG = 1.0e30
EPS = 1e-6
C_AQ, C_AK, C_AV, C_AG = 0, 1024, 2048, 3072
C_CB, C_CC, C_CH, C_CG = 4096, 5120, 6144, 7168
C_SQ, C_SK, C_SV, C_SG = 8192, 9216, 10240, 11264
C_MQ, C_MG = 12288, 13312
C_IQ, C_IK, C_IW, C_R = 14336, 14592, 14656, 14660
NIT = 16
TOPK = 256


class Tk:
    __slots__ = ("name", "w", "r", "dsem", "dcnt")

    def __init__(self, name):
        self.name = name
        self.w = None
        self.r = []
        self.dsem = None
        self.dcnt = 0


class Sched:
    def __init__(self, nc, es):
        self.nc = nc
        self.es = es
        self.eng = {"pe": nc.tensor, "act": nc.scalar, "dve": nc.vector, "pool": nc.gpsimd, "sp": nc.sync}
        self.sem = {k: es.enter_context(nc.semaphore("sem_" + k)) for k in self.eng}
        self.cnt = {k: 0 for k in self.eng}
        self.waited = {k: {} for k in self.eng}
        self.ninst = 0
        self.owners = []

    def drain(self):
        for k in self.eng:
            if self.cnt[k] > 0 and k != "sp":
                self.eng["sp"].wait_ge(self.sem[k], self.cnt[k])
        for o in self.owners:
            self.eng["sp"].wait_ge(o.dsem, o.dcnt)

    def _deps(self, e, reads, writes):
        deps = {}

        def add(tk, war):
            if tk is None:
                return
            key, sh, val, te = tk
            if te == e and e == "pe":
                return
            if deps.get(key, (None, 0))[1] < val:
                deps[key] = (sh, val)

        for t in reads:
            add(t.w, False)
        for t in writes:
            add(t.w, False)
            for rt in t.r:
                add(rt, True)
        return deps

    def _wait(self, e, deps):
        for key, (sh, val) in deps.items():
            if self.waited[e].get(key, 0) >= val:
                continue
            self.eng[e].wait_ge(sh, val)
            self.waited[e][key] = val

    def _record(self, tk, reads, writes):
        for t in writes:
            t.w = tk
            t.r = []
        for t in reads:
            if t in writes:
                continue
            t.r = [x for x in t.r if x[0] != tk[0]] + [tk]

    def op(self, e, fn, reads=(), writes=()):
        reads = list(reads)
        writes = list(writes)
        self._wait(e, self._deps(e, reads, writes))
        ins = fn(self.eng[e])
        self.cnt[e] += 1
        ins.then_inc(self.sem[e], 1)
        tk = (e, self.sem[e], self.cnt[e], e)
        self._record(tk, reads, writes)
        self.ninst += 1
        return tk

    def dma(self, q, out_ap, in_ap, owner, reads=(), writes=(), **kw):
        reads = list(reads)
        writes = list(writes)
        if owner.dsem is None:
            owner.dsem = self.es.enter_context(self.nc.semaphore("d_" + owner.name))
            self.owners.append(owner)
        self._wait(q, self._deps(q, reads, writes))
        ins = self.eng[q].dma_start(out=out_ap, in_=in_ap, **kw)
        owner.dcnt += 16
        ins.then_inc(owner.dsem, 16)
        tk = ("d_" + owner.name, owner.dsem, owner.dcnt, None)
        self._record(tk, reads, writes)
        self.ninst += 1
        return tk

    def final_wait(self, e, tiles):
        deps = {}
        for t in tiles:
            for tk in [t.w] + t.r:
                if tk is None:
                    continue
                key, sh, val, te = tk
                if deps.get(key, (None, 0))[1] < val:
                    deps[key] = (sh, val)
        self._wait(e, deps)


def build(T, L):
    NG = T // 512
    NT = T // 128
    nc = bass.Bass("TRN2", target_bir_lowering=False)
    es = ExitStack()

    def din(name, shape):
        return nc.dram_tensor(name, shape, F32, kind="ExternalInput").ap()

    x_d = din("x", [T, D])
    mem_d = din("mem", [256, D])
    lng_d = din("ln_g", [L, D])
    win_d = din("w_in", [L, D, INC])
    cw_d = din("conv_w", [L, 3, BW])
    cb_d = din("conv_b", [L, BW])
    mg_d = din("mem_ln_g", [L, D])
    wkv_d = din("w_mem_kv", [L, D, 2048])
    wbr_d = din("w_branch", [L, 4, BW, D])
    wout_d = din("w_out", [L, D, D])
    fg_d = din("final_g", [1, D])
    ident_d = din("c_ident", [128, 128])
    tri_d = din("c_tri", [128, 128])
    triq_d = din("c_triq", [128, 128])
    eone_d = din("c_eone", [16, 2048])
    pw2_d = din("c_pw2", [128, NIT + 2])
    sel_d = din("c_sel", [128, 16])
    y_d = nc.dram_tensor("y", [T, D], F32, kind="ExternalOutput").ap()
    xmid_d = nc.dram_tensor("xmid", [T, D], F32).ap()
    kT_d = {t: nc.dram_tensor("kT" + t, [8, 128, T], BF16).ap() for t in "as"}
    v_d = {t: nc.dram_tensor("vv" + t, [T, 1024], BF16).ap() for t in "as"}

    with es:
        S = Sched(nc, es)

        def sb(name, shape, dt=F32):
            return es.enter_context(nc.sbuf_tensor(name, shape, dt)), Tk(name)

        def psb(name, shape, dt=F32):
            return es.enter_context(nc.psum_tensor(name, shape, dt)), Tk(name)

        NW = 4
        wt = [sb(f"wt{i}", [128, 4096], BF16) for i in range(NW)]
        xnT, t_xnT = sb("xnT", [128, KC, 512], BF16)
        xall, _ = sb("xall", [128, 4096])
        t_xq = [Tk(f"xq{i}") for i in range(4)]
        scb, t_scb = sb("scb", [128, max(T, 2048)])
        kst, t_kst = sb("kst", [128, 4096], BF16)
        vst, t_vst = sb("vst", [128, 4, 1024], BF16)
        kiT, t_kiT = sb("kiT", [128, T], BF16)
        qiT, t_qiT = sb("qiT", [128, 2, 512], BF16)
        qh = [sb(f"qh{i}", [128, 512], BF16) for i in range(2)]
        sgh = [sb(f"sgh{i}", [128, 512], BF16) for i in range(2)]
        NR = 4
        kring = [sb(f"kr{i}", [128, 1024], BF16) for i in range(NR)]
        vring = [sb(f"vr{i}", [128, 8, 128], BF16) for i in range(NR)]
        pT = [sb(f"pT{i}", [128, 512], BF16) for i in range(3)]
        yT = [sb(f"yT{i}", [128, 8, 512], BF16) for i in range(4)]
        MBW = max(2 * T, 8192)
        MBH = MBW // 2
        mball, _ = sb("mball", [128, MBW], BF16)
        t_mb = [Tk("mb0"), Tk("mb1")]
        Fs = [sb(f"F{i}", [128, 514]) for i in range(5)]
        sgt = [sb(f"sgt{i}", [128, 512], BF16) for i in range(2)]
        mkT, t_mkT = sb("mkT", [128, 8, 256], BF16)
        mv, t_mv = sb("mv", [128, 2, 1024], BF16)
        ident, t_ident = sb("ident", [128, 128], BF16)
        tri, t_tri = sb("tri", [128, 128], BF16)
        triq, t_triq = sb("triq", [128, 128])
        ones, t_ones = sb("ones", [128, 128], BF16)
        eone, t_eone = sb("eone", [16, 2048], BF16)
        pw2, t_pw2 = sb("pw2", [128, NIT + 2])
        sel, t_sel = sb("sel", [128, 16], BF16)
        iwT, t_iwT = sb("iwT", [128, 512], BF16)
        km32, t_km32 = sb("km32", [128, 8, 16])
        kmT, t_kmT = sb("kmT", [128, 8, 16], BF16)
        bsm, t_bsm = sb("bsm", [128, 4, 16])
        m8, t_m8 = sb("m8", [128, 4, 8])
        mbf, t_mbf = sb("mbf", [128, 4, 16], BF16)
        mbT, t_mbT = sb("mbT", [16, 512], BF16)
        wI, t_wI = sb("wI", [128, 4, 4])
        st, t_st = sb("stat", [128, 8])
        epsb, t_eps = sb("epsb", [128, 1])
        bis, t_bis = sb("bis", [128, 8])
        halves, t_halves = sb("halves", [128, NIT + 2])
        nm, t_nm = sb("nm", [128, NIT + 1])
        ssum, t_ssum = sb("ssum", [128, NIT])
        dtmp, t_dtmp = sb("dtmp", [128, NIT])
        cneg, t_cneg = sb("cneg", [128, 1])
        nhh, t_nhh = sb("nhh", [128, NIT + 2])
        cw, t_cw = sb("cw", [128, 8, 3])
        cbias, t_cbias = sb("cbias", [128, 8])
        carry, t_carry = sb("carry", [128, 8, 2])
        sgc, t_sgc = sb("sgc", [128, 512], BF16)

        pj = [psb(f"pj{i}", [128, 512]) for i in range(3)]
        ptr, t_ptr = psb("ptr", [128, 1024], BF16)
        stp = [psb(f"stp{i}", [128, 512]) for i in range(2)]
        ot, t_ot = psb("ot", [128, 512])
        dn, t_dn = psb("dn", [128, 512])
        pjc = [0]

        def next_pj():
            pjc[0] += 1
            return pj[pjc[0] % 3]

        S.dma("pool", ident[:], ident_d, t_ident, writes=[t_ident])
        S.dma("pool", tri[:], tri_d, t_tri, writes=[t_tri])
        S.dma("pool", eone[:], eone_d, t_eone, writes=[t_eone])
        S.dma("sp", triq[:], triq_d, t_triq, writes=[t_triq])
        S.dma("sp", pw2[:], pw2_d, t_pw2, writes=[t_pw2])
        S.dma("pool", sel[:], sel_d, t_sel, writes=[t_sel])
        S.op("dve", lambda e: e.memset(ones[:], 1.0), writes=[t_ones])
        S.op("dve", lambda e: e.memset(epsb[:], EPS), writes=[t_eps])

        wcnt = [0]

        def wload(parts):
            w_sb, w_tk = wt[wcnt[0] % NW]
            wcnt[0] += 1
            for off, nk, ncol, src in parts:
                dst = w_sb[:, off:off + nk * ncol].rearrange("p (k n) -> p k n", k=nk)
                if isinstance(src, list):
                    for lo, sap in src:
                        S.dma("pool", dst[:, :, lo:lo + sap.shape[2]], sap, w_tk, writes=[w_tk])
                else:
                    S.dma("pool", dst, src, w_tk, writes=[w_tk])
            return w_sb, w_tk

        def wview(w_sb, off, nk, ncol):
            return w_sb[:, off:off + nk * ncol].rearrange("p (k n) -> p k n", k=nk)

        def win_cols(l, c0, n, reps=1, stride=0):
            base = win_d[l].rearrange("(k p) n -> p k n", p=128)
            if reps == 1:
                return base[:, :, c0:c0 + n]
            return [(r * n, base[:, :, c0 + r * stride:c0 + r * stride + n]) for r in range(reps)]

        def rmsnorm_rstd(x_ap, x_tks, junk_ap, junk_tks, col):
            S.op("act", lambda e: e.activation(out=junk_ap, in_=x_ap, func=AF.Square, accum_out=st[:, col:col + 1]),
                 reads=x_tks, writes=junk_tks + [t_st])
            S.op("act", lambda e: e.activation(out=st[:, col:col + 1], in_=st[:, col:col + 1], func=AF.Sqrt,
                                               scale=1.0 / D, bias=epsb[:, 0:1]), reads=[t_st, t_eps], writes=[t_st])
            S.op("dve", lambda e: e.reciprocal(st[:, col:col + 1], st[:, col:col + 1]), reads=[t_st], writes=[t_st])

        evc = [0]

        def evac_copy(out_ap, out_tks, in_ap, in_tks):
            evc[0] += 1
            if evc[0] % 2 == 0:
                S.op("act", lambda e: e.activation(out=out_ap, in_=in_ap, func=AF.Copy), reads=in_tks, writes=out_tks)
            else:
                S.op("dve", lambda e: e.tensor_copy(out_ap, in_ap), reads=in_tks, writes=out_tks)

        def norm_T(x_src_ap, g_tk_ready, ncols_tok, tok_off):
            pass

        def fm_chunk(w_view, w_tk, kn, rhs_fn, rhs_tks, ncol_tok):
            ps, tps = next_pj()
            for k in range(kn):
                S.op("pe", lambda e, k=k: e.matmul(ps[:, 0:ncol_tok], w_view[:, k, :], rhs_fn(k),
                                                   start=(k == 0), stop=(k == kn - 1)),
                     reads=[w_tk] + rhs_tks, writes=[tps])
            return ps, tps

        for l in range(L):
            xsrc = x_d if l == 0 else xmid_d
            xreg = [Tk(f"xreg{l}_{g}") for g in range(NG)]
            if l > 0:
                for g in range(NG):
                    xreg[g].w = xreg_prev[g].w
            kreg = {t: [Tk(f"kreg{t}{l}_{g}") for g in range(NG)] for t in "as"}
            vreg = {t: [Tk(f"vreg{t}{l}_{g}") for g in range(NG)] for t in "as"}
            if l > 0:
                for t in "as":
                    for g in range(NG):
                        kreg[t][g].r = list(kreg_prev[t][g].r)
                        kreg[t][g].w = kreg_prev[t][g].w
                        vreg[t][g].r = list(vreg_prev[t][g].r)
                        vreg[t][g].w = vreg_prev[t][g].w

            for k3 in range(3):
                S.dma("sp", cw[:, :, k3], cw_d[l, k3].rearrange("(c p) -> p c", p=128), t_cw, writes=[t_cw],
                      allow_slow_non_contiguous=True)
            S.dma("sp", cbias[:], cb_d[l].rearrange("(c p) -> p c", p=128), t_cbias, writes=[t_cbias],
                  allow_slow_non_contiguous=True)
            S.op("dve", lambda e: e.memset(carry[:], 0.0), writes=[t_carry])
            S.op("dve", lambda e: e.memset(km32[:], 0.0), writes=[t_km32])
            S.op("dve", lambda e: e.memset(kmT[:], 0.0), writes=[t_kmT])

            if DBG == "consts":
                S.drain()
                return nc
            gbc = scb[:, 0:2048]
            S.dma("sp", gbc, mg_d[l:l + 1, :].partition_broadcast(128), t_scb, writes=[t_scb])
            for mt in range(2):
                xt = xall[:, 0:2048]
                S.dma("sp", xt, mem_d[mt * 128:(mt + 1) * 128, :], t_xq[0], writes=[t_xq[0], t_xq[1]])
                xs = kst[:, 0:2048]
                rmsnorm_rstd(xt, [t_xq[0], t_xq[1]], xs, [t_kst], mt)
                S.op("dve", lambda e, mt=mt: e.scalar_tensor_tensor(out=xs, in0=xt, scalar=st[:, mt:mt + 1], in1=gbc,
                                                                    op0=ALU.mult, op1=ALU.mult),
                     reads=[t_xq[0], t_xq[1], t_st, t_scb], writes=[t_kst])
                for k4 in range(4):
                    for kk in range(4):
                        k = k4 * 4 + kk
                        S.op("pe", lambda e, k=k, kk=kk: e.transpose(ptr[:, kk * 128:(kk + 1) * 128],
                                                                     xs[:, k * 128:(k + 1) * 128], ident[:]),
                             reads=[t_kst, t_ident], writes=[t_ptr])
                    evac_copy(xnT[:, k4 * 4:(k4 + 1) * 4, mt * 128:(mt + 1) * 128],
                              [t_xnT], ptr[:, 0:512].rearrange("p (a b) -> p a b", a=4), [t_ptr])
            wkv = wkv_d[l].rearrange("(k p) n -> p k n", p=128)
            for c in range(4):
                w_sb, w_tk = wload([(0, KC, 256, wkv[:, :, c * 256:(c + 1) * 256])])
                wv = wview(w_sb, 0, KC, 256)
                for cc in range(2):
                    ps, tps = fm_chunk(wv[:, :, cc * 128:(cc + 1) * 128], w_tk, KC,
                                       lambda k: xnT[:, k, 0:256], [t_xnT], 256)
                    evac_copy(mkT[:, c * 2 + cc, :], [t_mkT], ps[:, 0:256], [tps])
            for c in range(4):
                w_sb, w_tk = wload([(0, KC, 256, wkv[:, :, 1024 + c * 256:1024 + (c + 1) * 256])])
                wv = wview(w_sb, 0, KC, 256)
                for mt in range(2):
                    ps, tps = next_pj()
                    for k in range(KC):
                        S.op("pe", lambda e, k=k, mt=mt: e.matmul(ps[:, 0:256], xnT[:, k, mt * 128:(mt + 1) * 128],
                                                                  wv[:, k, :], start=(k == 0), stop=(k == KC - 1)),
                             reads=[t_xnT, w_tk], writes=[tps])
                    evac_copy(mv[:, mt, c * 256:(c + 1) * 256], [t_mv], ps[:, 0:256], [tps])

            if DBG == "memkv":
                S.drain()
                return nc
            for g in range(NG):
                tok0 = g * 512
                S.dma("sp", gbc, lng_d[l:l + 1, :].partition_broadcast(128), t_scb, writes=[t_scb])
                for t in range(4):
                    hx = t % 2
                    xt = xall[:, hx * 2048:(hx + 1) * 2048]
                    xtk = [t_xq[2 * hx], t_xq[2 * hx + 1]]
                    S.dma("sp", xt, xsrc[tok0 + t * 128: tok0 + (t + 1) * 128, :], xtk[0], reads=[xreg[g]], writes=xtk)
                    xs = kst[:, 0:2048]
                    rmsnorm_rstd(xt, xtk, xs, [t_kst], t)
                    S.op("dve", lambda e, t=t, xt=xt: e.scalar_tensor_tensor(out=xs, in0=xt, scalar=st[:, t:t + 1],
                                                                             in1=gbc, op0=ALU.mult, op1=ALU.mult),
                         reads=xtk + [t_st, t_scb], writes=[t_kst])
                    for k4 in range(4):
                        for kk in range(4):
                            k = k4 * 4 + kk
                            S.op("pe", lambda e, k=k, kk=kk: e.transpose(ptr[:, kk * 128:(kk + 1) * 128],
                                                                         xs[:, k * 128:(k + 1) * 128], ident[:]),
                                 reads=[t_kst, t_ident], writes=[t_ptr])
                        evac_copy(xnT[:, k4 * 4:(k4 + 1) * 4, t * 128:(t + 1) * 128],
                                  [t_xnT], ptr[:, 0:512].rearrange("p (a b) -> p a b", a=4), [t_ptr])

                if DBG == "stage0":
                    S.drain()
                    return nc
                for typ, ck, cv in (("a", C_AK, C_AV), ("s", C_SK, C_SV)):
                    kst3 = kst[:].rearrange("p (h t) -> p h t", h=8)
                    for c in range(4):
                        w_sb, w_tk = wload([(0, KC, 256, win_cols(l, ck + c * 256, 256))])
                        wv = wview(w_sb, 0, KC, 256)
                        for cc in range(2):
                            h = c * 2 + cc
                            ps, tps = fm_chunk(wv[:, :, cc * 128:(cc + 1) * 128], w_tk, KC,
                                               lambda k: xnT[:, k, :], [t_xnT], 512)
                            S.op("act", lambda e, h=h, ps=ps: e.activation(out=kst3[:, h, :], in_=ps[:], func=AF.Copy),
                                 reads=[tps], writes=[t_kst])
                            if typ == "a":
                                S.op("dve", lambda e, h=h: e.tensor_reduce(
                                    out=km32[:, h, 2 * g:2 * g + 2], in_=kst3[:, h, :].rearrange("p (b t) -> p b t", b=2),
                                    axis=AX.X, op=ALU.add), reads=[t_kst], writes=[t_km32])
                    S.dma("sp", kT_d[typ][:, :, tok0:tok0 + 512].rearrange("h d t -> d h t"), kst3, t_kst,
                          reads=[t_kst], writes=[kreg[typ][g]])
                    if DBG == "kvA":
                        S.drain()
                        return nc
                    if typ == "a":
                        S.op("dve", lambda e: e.tensor_scalar(kmT[:, :, 2 * g:2 * g + 2], km32[:, :, 2 * g:2 * g + 2],
                                                              1.0 / 256.0, None, op0=ALU.mult),
                             reads=[t_km32], writes=[t_kmT])
                    for c in range(4):
                        w_sb, w_tk = wload([(0, KC, 256, win_cols(l, cv + c * 256, 256))])
                        wv = wview(w_sb, 0, KC, 256)
                        for t in range(4):
                            ps, tps = next_pj()
                            for k in range(KC):
                                S.op("pe", lambda e, k=k, t=t, ps=ps: e.matmul(
                                    ps[:, 0:256], xnT[:, k, t * 128:(t + 1) * 128], wv[:, k, :],
                                    start=(k == 0), stop=(k == KC - 1)), reads=[t_xnT, w_tk], writes=[tps])
                            evac_copy(vst[:, t, c * 256:(c + 1) * 256], [t_vst], ps[:, 0:256], [tps])
                    S.dma("sp", v_d[typ][tok0:tok0 + 512, :].rearrange("(t p) c -> p t c", p=128), vst[:], t_vst,
                          reads=[t_vst], writes=[vreg[typ][g]])
                if DBG == "kvC":
                    S.drain()
                    return nc
                w_sb, w_tk = wload([(0, KC, 256, win_cols(l, C_IQ, 256))])
                wv = wview(w_sb, 0, KC, 256)
                for cc in range(2):
                    ps, tps = fm_chunk(wv[:, :, cc * 128:(cc + 1) * 128], w_tk, KC, lambda k: xnT[:, k, :], [t_xnT], 512)
                    evac_copy(qiT[:, cc, :], [t_qiT], ps[:], [tps])
                w_sb, w_tk = wload([(0, KC, 256, win_cols(l, C_IK - 64, 256))])
                wv = wview(w_sb, 0, KC, 256)
                ps, tps = fm_chunk(wv[:, :, 64:192], w_tk, KC, lambda k: xnT[:, k, :], [t_xnT], 512)
                S.op("act", lambda e, ps=ps: e.activation(out=kiT[0:64, tok0:tok0 + 512], in_=ps[0:64, :], func=AF.Copy),
                     reads=[tps], writes=[t_kiT])
                S.op("act", lambda e, ps=ps: e.activation(out=iwT[64:96, :], in_=ps[64:96, :], func=AF.Copy),
                     reads=[tps], writes=[t_iwT])
                ps, tps = fm_chunk(wv[:, :, 0:128], w_tk, KC, lambda k: xnT[:, k, :], [t_xnT], 512)
                S.op("act", lambda e, ps=ps: e.activation(out=kiT[64:128, tok0:tok0 + 512], in_=ps[64:128, :], func=AF.Copy),
                     reads=[tps], writes=[t_kiT])
                ps, tps = next_pj()
                for t in range(4):
                    S.op("pe", lambda e, t=t, ps=ps: e.matmul(ps[:, t * 16:(t + 1) * 16], iwT[64:96, t * 128:(t + 1) * 128],
                                                             sel[64:96, :], start=True, stop=True),
                         reads=[t_iwT, t_sel], writes=[tps])
                S.op("dve", lambda e, ps=ps: e.tensor_scalar(wI[:], ps[:, 0:64].rearrange("p (a b) -> p a b", a=4)[:, :, 0:4],
                                                             (64.0 ** -0.5) * 0.5, None, op0=ALU.mult),
                     reads=[tps], writes=[t_wI])
                if DBG == "kv":
                    S.drain()
                    return nc
                def kv_loader(typ, h, nkt, lookahead=2):
                    nch = (nkt + 7) // 8
                    state = {"issued": 0}

                    def issue(c):
                        slot = (kvc[0] + c) % NR
                        k_sb, k_tk = kring[slot]
                        v_sb, v_tk = vring[slot]
                        k0 = c * 1024
                        n = min(1024, nkt * 128 - k0)
                        gs = sorted(set((k0 + i * 128) // 512 for i in range(n // 128)))
                        S.dma("sp", k_sb[:, 0:n], kT_d[typ][h, :, k0:k0 + n], k_tk,
                              reads=[kreg[typ][gg] for gg in gs], writes=[k_tk])
                        S.dma("sp", v_sb[:, 0:n // 128, :],
                              v_d[typ][k0:k0 + n, h * 128:(h + 1) * 128].rearrange("(a p) d -> p a d", p=128), v_tk,
                              reads=[vreg[typ][gg] for gg in gs], writes=[v_tk])

                    def get(kt):
                        c = kt // 8
                        while state["issued"] < min(nch, c + 1 + lookahead):
                            issue(state["issued"])
                            state["issued"] += 1
                        slot = (kvc[0] + c) % NR
                        k_sb, k_tk = kring[slot]
                        v_sb, v_tk = vring[slot]
                        j = kt % 8
                        return k_sb[:, j * 128:(j + 1) * 128], k_tk, v_sb[:, j, :], v_tk

                    def done():
                        kvc[0] += nch
                    return get, done

                def attention(nkt, qcols, q_ap, q_tk, get_kv, mask_fn, scale):
                    pend = []
                    for kt in range(nkt + 1):
                        if kt < nkt:
                            c0, masks = mask_fn(kt)
                            sp_, tsp = stp[kt % 2]
                            k_ap, k_tk, v_ap, v_tk = get_kv(kt)
                            nm_ = len(masks)
                            S.op("pe", lambda e, sp_=sp_, k_ap=k_ap, c0=c0, nm_=nm_: e.matmul(
                                sp_[:, c0:qcols], k_ap, q_ap[:, c0:qcols], start=True, stop=(nm_ == 0)),
                                reads=[k_tk, q_tk], writes=[tsp])
                            for i, (ml, mr, lo, hi, mtks) in enumerate(masks):
                                S.op("pe", lambda e, sp_=sp_, ml=ml, mr=mr, lo=lo, hi=hi, i=i, nm_=nm_: e.matmul(
                                    sp_[:, lo:hi], ml, mr, start=False, stop=(i == nm_ - 1)),
                                    reads=mtks, writes=[tsp])
                            pend.append((kt, c0, sp_, tsp, v_ap, v_tk))
                        if kt > 0:
                            pk, c0, sp_, tsp, v_ap, v_tk = pend.pop(0)
                            p_sb, p_tk = pT[pk % 3]
                            S.op("act", lambda e, p_sb=p_sb, sp_=sp_, c0=c0: e.activation(
                                out=p_sb[:, c0:qcols], in_=sp_[:, c0:qcols], func=AF.Exp, scale=scale),
                                reads=[tsp], writes=[p_tk])
                            S.op("pe", lambda e, v_ap=v_ap, p_sb=p_sb, c0=c0, pk=pk: e.matmul(
                                ot[:, c0:qcols], v_ap, p_sb[:, c0:qcols], start=(pk == 0), stop=(pk == nkt - 1)),
                                reads=[v_tk, p_tk], writes=[t_ot])
                            S.op("pe", lambda e, p_sb=p_sb, c0=c0, pk=pk: e.matmul(
                                dn[:, c0:qcols], ones[:], p_sb[:, c0:qcols], start=(pk == 0), stop=(pk == nkt - 1)),
                                reads=[t_ones, p_tk], writes=[t_dn])

                def normalize(o_ap, o_tk, qcols, sg_ap, sg_tk, y_ap, y_tk):
                    rden, t_rden = Fs[3]
                    t1, t_t1 = Fs[4]
                    S.op("dve", lambda e: e.reciprocal(rden[:, 0:qcols], dn[:, 0:qcols]), reads=[t_dn], writes=[t_rden])
                    S.op("dve", lambda e: e.tensor_tensor(out=t1[:, 0:qcols], in0=o_ap, in1=rden[:, 0:qcols], op=ALU.mult),
                         reads=[o_tk, t_rden], writes=[t_t1])
                    S.op("dve", lambda e: e.tensor_tensor(out=y_ap, in0=t1[:, 0:qcols], in1=sg_ap, op=ALU.mult),
                         reads=[t_t1, sg_tk], writes=[y_tk])

                for h in range(8):
                    q_sb, q_tk = qh[h % 2]
                    g_sb, g_tk = sgh[h % 2]
                    w_sb, w_tk = wload([(0, KC, 256, win_cols(l, C_AQ + h * 128, 128, reps=2, stride=C_AG - C_AQ))])
                    wv = wview(w_sb, 0, KC, 256)
                    ps, tps = fm_chunk(wv[:, :, 0:128], w_tk, KC, lambda k: xnT[:, k, :], [t_xnT], 512)
                    S.op("act", lambda e, ps=ps, q_sb=q_sb: e.activation(out=q_sb[:], in_=ps[:], func=AF.Copy),
                         reads=[tps], writes=[q_tk])
                    ps, tps = fm_chunk(wv[:, :, 128:256], w_tk, KC, lambda k: xnT[:, k, :], [t_xnT], 512)
                    S.op("act", lambda e, ps=ps, g_sb=g_sb: e.activation(out=g_sb[:], in_=ps[:], func=AF.Silu),
                         reads=[tps], writes=[g_tk])
                    ps, tps = next_pj()
                    for t in range(4):
                        S.op("pe", lambda e, t=t, ps=ps, q_sb=q_sb: e.matmul(
                            ps[:, t * 16:(t + 1) * 16], q_sb[:, t * 128:(t + 1) * 128], kmT[:, h, :], start=True, stop=True),
                            reads=[q_tk, t_kmT], writes=[tps])
                    S.op("dve", lambda e, ps=ps: e.tensor_copy(bsm[:].rearrange("p a b -> p (a b)"), ps[:, 0:64]),
                         reads=[tps], writes=[t_bsm])
                    S.op("dve", lambda e: e.memset(bsm[:, 0:2, 2 * g:16], -BIG), writes=[t_bsm])
                    S.op("dve", lambda e: e.memset(bsm[:, 2:4, 2 * g + 1:16], -BIG), writes=[t_bsm])
                    for t in range(4):
                        S.op("dve", lambda e, t=t: e.max(out=m8[:, t, :], in_=bsm[:, t, :]), reads=[t_bsm], writes=[t_m8])
                    for t in range(4):
                        S.op("dve", lambda e, t=t: e.tensor_scalar(mbf[:, t, :], bsm[:, t, :], m8[:, t, 2:3], NEG,
                                                                  op0=ALU.is_lt, op1=ALU.mult),
                             reads=[t_bsm, t_m8], writes=[t_mbf])
                    for t in range(4):
                        S.op("pe", lambda e, t=t: e.transpose(ptr[0:16, t * 128:(t + 1) * 128], mbf[:, t, :], ident[:]),
                             reads=[t_mbf, t_ident], writes=[t_ptr])
                    S.op("dve", lambda e: e.tensor_copy(mbT[:, :], ptr[0:16, 0:512]), reads=[t_ptr], writes=[t_mbT])

                    def moba_mask(kt):
                        a = kt - 4 * g
                        c0 = 128 * max(a, 0)
                        i = kt // 2
                        masks = []
                        if i < 2 * g:
                            masks.append((eone[:, i * 128:(i + 1) * 128], mbT[:, 0:512], 0, 512, [t_eone, t_mbT]))
                        elif i == 2 * g:
                            masks.append((eone[:, i * 128:(i + 1) * 128], mbT[:, 256:512], 256, 512, [t_eone, t_mbT]))
                            masks.append((ident[:], tri[:], a * 128, (a + 1) * 128, [t_ident, t_tri]))
                        else:
                            masks.append((ident[:], tri[:], a * 128, (a + 1) * 128, [t_ident, t_tri]))
                        return c0, masks

                    get_kv, kv_done = kv_loader("a", h, 4 * g + 4)
                    attention(4 * g + 4, 512, q_sb, q_tk, get_kv, moba_mask, 128.0 ** -0.5)
                    kv_done()
                    normalize(ot[:, 0:512], t_ot, 512, g_sb[:], g_tk, yT[0][0][:, h, :], yT[0][1])

                if DBG == "moba":
                    S.drain()
                    return nc
                for j2 in range(4):
                    ctl = [(Fs[0], Fs[2]), (Fs[1], Fs[3])]
                    w_sb, w_tk = wload([(0, KC, 256, win_cols(l, C_CC + j2 * 256, 256))])
                    wv = wview(w_sb, 0, KC, 256)
                    for c2 in range(2):
                        (ccs, t_ccs), (u, t_u) = ctl[c2]
                        ps, tps = fm_chunk(wv[:, :, c2 * 128:(c2 + 1) * 128], w_tk, KC, lambda k: xnT[:, k, :], [t_xnT], 512)
                        S.op("act", lambda e, ps=ps, ccs=ccs: e.activation(out=ccs[:, 0:512], in_=ps[:], func=AF.Copy),
                             reads=[tps], writes=[t_ccs])
                    w_sb, w_tk = wload([(0, KC, 256, win_cols(l, C_CH + j2 * 256, 256))])
                    wv = wview(w_sb, 0, KC, 256)
                    for c2 in range(2):
                        j = j2 * 2 + c2
                        (cacc, t_cacc), (u, t_u) = ctl[c2]
                        ps, tps = fm_chunk(wv[:, :, c2 * 128:(c2 + 1) * 128], w_tk, KC, lambda k: xnT[:, k, :], [t_xnT], 512)
                        S.op("dve", lambda e, ps=ps, u=u, cacc=cacc: e.tensor_tensor(out=u[:, 2:514], in0=ps[:], in1=cacc[:, 0:512], op=ALU.mult),
                             reads=[tps, t_cacc], writes=[t_u])
                        S.op("dve", lambda e, j=j, u=u: e.tensor_copy(u[:, 0:2], carry[:, j, :]), reads=[t_carry], writes=[t_u])
                        S.op("act", lambda e, j=j, u=u, cacc=cacc: e.activation(out=cacc[:, 0:512], in_=u[:, 2:514], func=AF.Identity,
                                                                                scale=cw[:, j, 2:3], bias=cbias[:, j:j + 1]),
                             reads=[t_u, t_cw, t_cbias], writes=[t_cacc])
                        S.op("dve", lambda e, j=j, u=u, cacc=cacc: e.scalar_tensor_tensor(out=cacc[:, 0:512], in0=u[:, 1:513], scalar=cw[:, j, 1:2],
                                                                                          in1=cacc[:, 0:512], op0=ALU.mult, op1=ALU.add),
                             reads=[t_u, t_cw, t_cacc], writes=[t_cacc])
                        S.op("dve", lambda e, j=j, u=u, cacc=cacc: e.scalar_tensor_tensor(out=cacc[:, 0:512], in0=u[:, 0:512], scalar=cw[:, j, 0:1],
                                                                                          in1=cacc[:, 0:512], op0=ALU.mult, op1=ALU.add),
                             reads=[t_u, t_cw, t_cacc], writes=[t_cacc])
                        S.op("dve", lambda e, j=j, u=u: e.tensor_copy(carry[:, j, :], u[:, 512:514]), reads=[t_u], writes=[t_carry])
                    w_sb, w_tk = wload([(0, KC, 256, win_cols(l, C_CB + j2 * 256, 256))])
                    wv = wview(w_sb, 0, KC, 256)
                    for c2 in range(2):
                        (cacc, t_cacc), (u, t_u) = ctl[c2]
                        ps, tps = fm_chunk(wv[:, :, c2 * 128:(c2 + 1) * 128], w_tk, KC, lambda k: xnT[:, k, :], [t_xnT], 512)
                        S.op("dve", lambda e, ps=ps, cacc=cacc: e.tensor_tensor(out=cacc[:, 0:512], in0=ps[:], in1=cacc[:, 0:512], op=ALU.mult),
                             reads=[tps, t_cacc], writes=[t_cacc])
                    w_sb, w_tk = wload([(0, KC, 256, win_cols(l, C_CG + j2 * 256, 256))])
                    wv = wview(w_sb, 0, KC, 256)
                    for c2 in range(2):
                        j = j2 * 2 + c2
                        (cacc, t_cacc), (u, t_u) = ctl[c2]
                        ps, tps = fm_chunk(wv[:, :, c2 * 128:(c2 + 1) * 128], w_tk, KC, lambda k: xnT[:, k, :], [t_xnT], 512)
                        S.op("act", lambda e, ps=ps: e.activation(out=sgc[:], in_=ps[:], func=AF.Silu), reads=[tps], writes=[t_sgc])
                        S.op("dve", lambda e, j=j, cacc=cacc: e.tensor_tensor(out=yT[1][0][:, j, :], in0=cacc[:, 0:512], in1=sgc[:], op=ALU.mult),
                             reads=[t_cacc, t_sgc], writes=[yT[1][1]])
                if DBG == "conv":
                    S.drain()
                    return nc
                for hh in range(2):
                    for tt in range(2):
                        t = hh * 2 + tt
                        Q = 4 * g + t
                        nk = (Q + 1) * 128
                        mb_ap = mball[:, tt * MBH: tt * MBH + nk]
                        nchunk = (nk + 511) // 512
                        for c in range(nchunk):
                            wdt = min(512, nk - c * 512)
                            for j in range(4):
                                pb = 64 * (j % 2)
                                ps, tps = next_pj()
                                rl, t_rl = Fs[j % 2]
                                S.op("pe", lambda e, ps=ps, pb=pb, j=j, c=c, wdt=wdt, t=t: e.matmul(
                                    ps[:, 0:wdt], qiT[pb:pb + 64, j // 2, t * 128:(t + 1) * 128],
                                    kiT[pb:pb + 64, c * 512:c * 512 + wdt], start=True, stop=True),
                                    reads=[t_qiT, t_kiT], writes=[tps])
                                S.op("act", lambda e, ps=ps, rl=rl, wdt=wdt: e.activation(out=rl[:, 0:wdt], in_=ps[:, 0:wdt], func=AF.Relu),
                                     reads=[tps], writes=[t_rl])
                                if j == 0:
                                    S.op("dve", lambda e, rl=rl, c=c, wdt=wdt, t=t: e.tensor_scalar(
                                        scb[:, c * 512:c * 512 + wdt], rl[:, 0:wdt], wI[:, t, 0:1], None, op0=ALU.mult),
                                        reads=[t_rl, t_wI], writes=[t_scb])
                                else:
                                    S.op("dve", lambda e, rl=rl, c=c, wdt=wdt, t=t, j=j: e.scalar_tensor_tensor(
                                        out=scb[:, c * 512:c * 512 + wdt], in0=rl[:, 0:wdt], scalar=wI[:, t, j:j + 1],
                                        in1=scb[:, c * 512:c * 512 + wdt], op0=ALU.mult, op1=ALU.add),
                                        reads=[t_rl, t_wI, t_scb], writes=[t_scb])
                        S.op("dve", lambda e, nk=nk: e.tensor_reduce(out=bis[:, 0:1], in_=scb[:, 0:nk], axis=AX.X, op=ALU.min),
                             reads=[t_scb], writes=[t_bis])
                        S.op("dve", lambda e, nk=nk: e.tensor_reduce(out=bis[:, 1:2], in_=scb[:, 0:nk], axis=AX.X, op=ALU.max),
                             reads=[t_scb], writes=[t_bis])
                        S.op("dve", lambda e, Q=Q: e.tensor_tensor(out=scb[:, Q * 128:(Q + 1) * 128], in0=scb[:, Q * 128:(Q + 1) * 128],
                                                                   in1=triq[:], op=ALU.add), reads=[t_scb, t_triq], writes=[t_scb])
                        S.op("dve", lambda e: e.tensor_tensor(out=bis[:, 2:3], in0=bis[:, 1:2], in1=bis[:, 0:1], op=ALU.subtract),
                             reads=[t_bis], writes=[t_bis])
                        S.op("dve", lambda e: e.tensor_scalar(bis[:, 2:3], bis[:, 2:3], 1.0001, 1e-6, op0=ALU.mult, op1=ALU.add),
                             reads=[t_bis], writes=[t_bis])
                        S.op("dve", lambda e: e.tensor_scalar(halves[:], pw2[:], bis[:, 2:3], None, op0=ALU.mult),
                             reads=[t_pw2, t_bis], writes=[t_halves])
                        S.op("dve", lambda e: e.scalar_tensor_tensor(out=nm[:, 0:1], in0=bis[:, 0:1], scalar=-1.0, in1=halves[:, 0:1],
                                                                     op0=ALU.mult, op1=ALU.subtract),
                             reads=[t_bis, t_halves], writes=[t_nm])
                        S.op("dve", lambda e: e.memset(ssum[:], 0.0), writes=[t_ssum])
                        cthr = float(2 * min(TOPK, T // 4) - nk)
                        S.op("dve", lambda e: e.memset(cneg[:], 0.5 - cthr), writes=[t_cneg])
                        S.op("dve", lambda e: e.tensor_scalar(nhh[:], halves[:], -0.5, None, op0=ALU.mult),
                             reads=[t_halves], writes=[t_nhh])
                        for it in range(NIT):
                            S.op("act", lambda e, it=it, nk=nk: e.activation(out=kst[:, 0:nk], in_=scb[:, 0:nk], func=AF.Sign,
                                                                             bias=nm[:, it:it + 1], scale=1.0,
                                                                             accum_out=ssum[:, it:it + 1]),
                                 reads=[t_scb, t_nm], writes=[t_kst, t_ssum])
                            S.op("act", lambda e, it=it: e.activation(out=dtmp[:, it:it + 1], in_=ssum[:, it:it + 1], func=AF.Sign,
                                                                      bias=cneg[:, 0:1], scale=1.0),
                                 reads=[t_ssum, t_cneg], writes=[t_dtmp])
                            S.op("act", lambda e, it=it: e.activation(out=nm[:, it + 1:it + 2], in_=dtmp[:, it:it + 1], func=AF.Identity,
                                                                      scale=nhh[:, it:it + 1], bias=nm[:, it:it + 1]),
                                 reads=[t_dtmp, t_nhh, t_nm], writes=[t_nm])
                        S.op("dve", lambda e: e.scalar_tensor_tensor(out=bis[:, 3:4], in0=nm[:, NIT:NIT + 1], scalar=-1.0,
                                                                     in1=halves[:, NIT + 1:NIT + 2], op0=ALU.mult, op1=ALU.subtract),
                             reads=[t_nm, t_halves], writes=[t_bis])
                        S.op("dve", lambda e, mb_ap=mb_ap, nk=nk: e.tensor_scalar(mb_ap, scb[:, 0:nk], bis[:, 3:4], NEG,
                                                                                 op0=ALU.is_lt, op1=ALU.mult),
                             reads=[t_scb, t_bis], writes=[t_mb[tt]])
                    nkt = 4 * g + 2 * hh + 2
                    for h in range(8):
                        q_sb, q_tk = qh[h % 2]
                        g_sb, g_tk = sgh[h % 2]
                        w_sb, w_tk = wload([(0, KC, 256, win_cols(l, C_SQ + h * 128, 128, reps=2, stride=C_SG - C_SQ))])
                        wv = wview(w_sb, 0, KC, 256)
                        ps, tps = fm_chunk(wv[:, :, 0:128], w_tk, KC, lambda k: xnT[:, k, hh * 256:(hh + 1) * 256], [t_xnT], 256)
                        S.op("act", lambda e, ps=ps, q_sb=q_sb: e.activation(out=q_sb[:, 0:256], in_=ps[:, 0:256], func=AF.Copy),
                             reads=[tps], writes=[q_tk])
                        ps, tps = fm_chunk(wv[:, :, 128:256], w_tk, KC, lambda k: xnT[:, k, hh * 256:(hh + 1) * 256], [t_xnT], 256)
                        S.op("act", lambda e, ps=ps, g_sb=g_sb: e.activation(out=g_sb[:, 0:256], in_=ps[:, 0:256], func=AF.Silu),
                             reads=[tps], writes=[g_tk])

                        def dsa_mask(kt):
                            a = kt - (4 * g + 2 * hh)
                            c0 = 128 * max(a, 0)
                            masks = []
                            for tt in range(2):
                                if kt <= 4 * g + 2 * hh + tt:
                                    masks.append((mball[:, tt * MBH + kt * 128: tt * MBH + (kt + 1) * 128], ident[:],
                                                  tt * 128, (tt + 1) * 128, [t_mb[tt], t_ident]))
                            return c0, masks

                        get_kv, kv_done = kv_loader("s", h, nkt)
                        attention(nkt, 256, q_sb, q_tk, get_kv, dsa_mask, 128.0 ** -0.5)
                        kv_done()
                        normalize(ot[:, 0:256], t_ot, 256, g_sb[:, 0:256], g_tk,
                                  yT[2][0][:, h, hh * 256:(hh + 1) * 256], yT[2][1])

                if DBG == "dsa":
                    S.drain()
                    return nc
                for h in range(4):
                    for dc in range(2):
                        q_sb, q_tk = qh[dc]
                        g_sb, g_tk = sgh[dc]
                        cidx = h * 2 + dc
                        w_sb, w_tk = wload([(0, KC, 256, win_cols(l, C_MQ + cidx * 128, 128, reps=2, stride=C_MG - C_MQ))])
                        wv = wview(w_sb, 0, KC, 256)
                        ps, tps = fm_chunk(wv[:, :, 0:128], w_tk, KC, lambda k: xnT[:, k, :], [t_xnT], 512)
                        S.op("act", lambda e, ps=ps, q_sb=q_sb: e.activation(out=q_sb[:], in_=ps[:], func=AF.Copy),
                             reads=[tps], writes=[q_tk])
                        ps, tps = fm_chunk(wv[:, :, 128:256], w_tk, KC, lambda k: xnT[:, k, :], [t_xnT], 512)
                        S.op("act", lambda e, ps=ps, g_sb=g_sb: e.activation(out=g_sb[:], in_=ps[:], func=AF.Silu),
                             reads=[tps], writes=[g_tk])
                    pts = []
                    for mt in range(2):
                        sp_, tsp = stp[mt]
                        for dc in range(2):
                            S.op("pe", lambda e, sp_=sp_, mt=mt, dc=dc: e.matmul(
                                sp_[:], mkT[:, h * 2 + dc, mt * 128:(mt + 1) * 128], qh[dc][0][:], start=(dc == 0), stop=(dc == 1)),
                                reads=[t_mkT, qh[dc][1]], writes=[tsp])
                        p_sb, p_tk = pT[mt]
                        S.op("act", lambda e, sp_=sp_, p_sb=p_sb: e.activation(out=p_sb[:], in_=sp_[:], func=AF.Exp, scale=256.0 ** -0.5),
                             reads=[tsp], writes=[p_tk])
                        pts.append((p_sb, p_tk))
                    o2, t_o2 = pj[0]
                    for mt in range(2):
                        p_sb, p_tk = pts[mt]
                        S.op("pe", lambda e, mt=mt, p_sb=p_sb: e.matmul(ot[:], mv[:, mt, h * 256:h * 256 + 128], p_sb[:],
                                                                        start=(mt == 0), stop=(mt == 1)),
                             reads=[t_mv, p_tk], writes=[t_ot])
                        S.op("pe", lambda e, mt=mt, p_sb=p_sb: e.matmul(o2[:], mv[:, mt, h * 256 + 128:h * 256 + 256], p_sb[:],
                                                                        start=(mt == 0), stop=(mt == 1)),
                             reads=[t_mv, p_tk], writes=[t_o2])
                        S.op("pe", lambda e, mt=mt, p_sb=p_sb: e.matmul(dn[:], ones[:], p_sb[:], start=(mt == 0), stop=(mt == 1)),
                             reads=[t_ones, p_tk], writes=[t_dn])
                    normalize(ot[:], t_ot, 512, sgh[0][0][:], sgh[0][1], yT[3][0][:, h * 2, :], yT[3][1])
                    normalize(o2[:], t_o2, 512, sgh[1][0][:], sgh[1][1], yT[3][0][:, h * 2 + 1, :], yT[3][1])

                if DBG == "mem":
                    S.drain()
                    return nc
                mrg = mball[:, 0:8192].rearrange("p (c t) -> p c t", c=16)
                t_mrg = t_mb
                for oc2 in range(8):
                    for br in range(4):
                        w_sb, w_tk = wload([(0, KC, 256, win_cols(l, C_R + 2048 * br + 256 * oc2, 256))])
                        wr = wview(w_sb, 0, KC, 256)
                        w_sb2, w_tk2 = wload([(0, 8, 256,
                                               wbr_d[l, br].rearrange("(k p) n -> p k n", p=128)[:, :, oc2 * 256:(oc2 + 1) * 256])])
                        wb = wview(w_sb2, 0, 8, 256)
                        y_sb, y_tk = yT[br]
                        for c2 in range(2):
                            oc = oc2 * 2 + c2
                            acc, t_acc = Fs[c2]
                            tmp, t_tmp = Fs[2 + c2]
                            sg_sb, sg_tk = sgt[c2]
                            ps, tps = fm_chunk(wr[:, :, c2 * 128:(c2 + 1) * 128], w_tk, KC, lambda k: xnT[:, k, :], [t_xnT], 512)
                            S.op("act", lambda e, ps=ps, sg_sb=sg_sb: e.activation(out=sg_sb[:], in_=ps[:], func=AF.Sigmoid),
                                 reads=[tps], writes=[sg_tk])
                            ps, tps = fm_chunk(wb[:, :, c2 * 128:(c2 + 1) * 128], w_tk2, 8, lambda k, y_sb=y_sb: y_sb[:, k, :], [y_tk], 512)
                            if br == 0:
                                S.op("dve", lambda e, ps=ps, sg_sb=sg_sb, acc=acc: e.tensor_tensor(
                                    out=acc[:, 0:512], in0=ps[:], in1=sg_sb[:], op=ALU.mult), reads=[tps, sg_tk], writes=[t_acc])
                            else:
                                S.op("dve", lambda e, ps=ps, sg_sb=sg_sb, tmp=tmp: e.tensor_tensor(
                                    out=tmp[:, 0:512], in0=ps[:], in1=sg_sb[:], op=ALU.mult), reads=[tps, sg_tk], writes=[t_tmp])
                                if br < 3:
                                    S.op("dve", lambda e, tmp=tmp, acc=acc: e.tensor_tensor(
                                        out=acc[:, 0:512], in0=acc[:, 0:512], in1=tmp[:, 0:512], op=ALU.add),
                                        reads=[t_acc, t_tmp], writes=[t_acc])
                                else:
                                    S.op("dve", lambda e, tmp=tmp, acc=acc, oc=oc: e.tensor_tensor(
                                        out=mrg[:, oc, :], in0=acc[:, 0:512], in1=tmp[:, 0:512], op=ALU.add),
                                        reads=[t_acc, t_tmp], writes=[t_mb[0], t_mb[1]])
                if DBG == "merge":
                    S.drain()
                    return nc
                wo = wout_d[l].rearrange("(k p) n -> p k n", p=128)
                for cb in range(8):
                    w_sb, w_tk = wload([(0, KC, 256, wo[:, :, cb * 256:(cb + 1) * 256])])
                    wv = wview(w_sb, 0, KC, 256)
                    xr = xall[:, (cb % 2) * 1024:(cb % 2 + 1) * 1024].rearrange("p (t c) -> p t c", t=4)
                    t_xr = t_xq[cb % 2]
                    xo = xall[:, 2048 + (cb % 2) * 1024: 2048 + (cb % 2 + 1) * 1024].rearrange("p (t c) -> p t c", t=4)
                    t_xo = t_xq[2 + cb % 2]
                    S.dma("sp", xr, xsrc[tok0:tok0 + 512, cb * 256:(cb + 1) * 256].rearrange("(t p) c -> p t c", p=128),
                          t_xr, reads=[xreg[g]], writes=[t_xr])
                    for t in range(4):
                        ps, tps = next_pj()
                        for k in range(KC):
                            S.op("pe", lambda e, k=k, t=t, ps=ps: e.matmul(
                                ps[:, 0:256], mrg[:, k, t * 128:(t + 1) * 128], wv[:, k, :], start=(k == 0), stop=(k == KC - 1)),
                                reads=[t_mb[0], t_mb[1], w_tk], writes=[tps])
                        S.op("dve", lambda e, t=t, ps=ps, xo=xo, xr=xr: e.tensor_tensor(
                            out=xo[:, t, :], in0=ps[:, 0:256], in1=xr[:, t, :], op=ALU.add),
                            reads=[tps, t_xr], writes=[t_xo])
                    S.dma("sp", xmid_d[tok0:tok0 + 512, cb * 256:(cb + 1) * 256].rearrange("(t p) c -> p t c", p=128), xo,
                          t_xo, reads=[t_xo], writes=[xreg[g]] if l > 0 or True else [])
            xreg_prev = xreg
            kreg_prev = kreg
            vreg_prev = vreg

        gbc = scb[:, 0:2048]
        S.dma("sp", gbc, fg_d.partition_broadcast(128), t_scb, writes=[t_scb])
        for tt in range(NT):
            hx = tt % 2
            xt = xall[:, hx * 2048:(hx + 1) * 2048]
            xtk = [t_xq[2 * hx], t_xq[2 * hx + 1]]
            S.dma("sp", xt, xmid_d[tt * 128:(tt + 1) * 128, :], xtk[0], reads=[xreg_prev[tt // 4]], writes=xtk)
            rmsnorm_rstd(xt, xtk, kst[:, 0:2048], [t_kst], tt % 8)
            S.op("dve", lambda e, xt=xt, c=tt % 8: e.scalar_tensor_tensor(out=xt, in0=xt, scalar=st[:, c:c + 1], in1=gbc,
                                                                          op0=ALU.mult, op1=ALU.mult),
                 reads=xtk + [t_st, t_scb], writes=xtk)
            S.dma("sp", y_d[tt * 128:(tt + 1) * 128, :], xt, xtk[1], reads=xtk, writes=[])
        S.final_wait("sp", t_xq)
        S.drain()
    return nc


kvc = [0]
DBG = None


def _consts():
    k = np.arange(128)
    tri = np.where(k[None, :] >= k[:, None], 0.0, NEG).astype(np.float32)
    triq = np.where(k[None, :] <= k[:, None], 0.0, -BIG).astype(np.float32)
    eone = np.zeros((16, 2048), np.float32)
    for i in range(16):
        eone[i, i * 128:(i + 1) * 128] = 1.0
    pw2 = np.zeros((128, NIT + 2), np.float32)
    for i in range(NIT + 1):
        pw2[:, i] = 2.0 ** -(i + 1)
    pw2[:, NIT + 1] = 1.25 * 2.0 ** -(NIT + 1)
    selm = np.zeros((128, 16), np.float32)
    for j in range(4):
        selm[64 + j, j] = 1.0
    return {"c_sel": selm, "c_ident": np.eye(128, dtype=np.float32), "c_tri": tri, "c_triq": triq, "c_eone": eone, "c_pw2": pw2}


_CACHE = {}


def run(inputs, T, L, n_cores, batch_of_core):
    key = (T, L)
    if key not in _CACHE:
        kvc[0] = 0
        _CACHE[key] = build(T, L)
    nc = _CACHE[key]
    cst = _consts()
    f = lambda a: np.ascontiguousarray(np.asarray(a, dtype=np.float32))
    shared = {
        "ln_g": f(inputs["ln_g"]), "w_in": f(inputs["w_in"]), "conv_w": f(inputs["conv_w"]),
        "conv_b": f(inputs["conv_b"]), "mem_ln_g": f(inputs["mem_ln_g"]), "w_mem_kv": f(inputs["w_mem_kv"]),
        "w_branch": f(inputs["w_branch"]), "w_out": f(inputs["w_out"]),
        "final_g": f(inputs["final_g"]).reshape(1, D), **cst,
    }
    in_maps = []
    zshared = None
    for c in range(n_cores):
        b = batch_of_core[c]
        if b is None:
            if zshared is None:
                zshared = {k: (v if k.startswith("c_") else np.zeros_like(v)) for k, v in shared.items()}
                zshared["x"] = np.zeros((T, D), np.float32)
                zshared["mem"] = np.zeros((256, D), np.float32)
            in_maps.append(zshared)
            continue
        m = dict(shared)
        m["x"] = f(inputs["x"][b])
        m["mem"] = f(inputs["mem"][b])
        in_maps.append(m)
    res = run_bass_kernel_spmd(nc, in_maps, core_ids=list(range(n_cores)))
    return [r["y"] for r in res.results]


def kernel(x, mem, ln_g, w_in, conv_w, conv_b, mem_ln_g, w_mem_kv, w_branch, w_out, final_g):
    inputs = dict(x=x, mem=mem, ln_g=ln_g, w_in=w_in, conv_w=conv_w, conv_b=conv_b, mem_ln_g=mem_ln_g,
                  w_mem_kv=w_mem_kv, w_branch=w_branch, w_out=w_out, final_g=final_g)
    B, T, _ = np.asarray(x).shape
    L = np.asarray(ln_g).shape[0]
    owner = {0: 0, 1: 1, 4: 2, 5: 3}
    outs = run(inputs, T, L, 8, [owner.get(c) if owner.get(c, B) < B else None for c in range(8)])
    core_of = {b: c for c, b in owner.items()}
    return np.stack([outs[core_of[b]] for b in range(B)], axis=0).astype(np.float32)
```
